# Optimizing a Trainium2 kernel written in Bass

```python
import jax, jax.numpy as jnp
from jax import lax
import numpy as np

D_MODEL = 2048
BATCH = 4
SEQ = 4096
DEPTH = 4

GRID_W = 64
CTX_LEN = 256
EPS = 1e-6
N_MOD = 6

LRU_WIDTH = 1024
LRU_BLOCKS = 8
LRU_BLOCK = LRU_WIDTH // LRU_BLOCKS
LRU_C = 8.0
CONV_W = 4
ATT_HEADS = 8
KV_HEADS = 2
GROUP = ATT_HEADS // KV_HEADS
HEAD_DIM = 128
ATT_WIDTH = ATT_HEADS * HEAD_DIM
KV_WIDTH = KV_HEADS * HEAD_DIM
WINDOW = 128
BLOCK_Q = 128
ROPE_PAIRS = HEAD_DIM // 4
ROPE_BASE = 10000.0
EVEN_IN = 2 * LRU_WIDTH + ATT_WIDTH + 2 * KV_WIDTH
EVEN_SPLITS = (LRU_WIDTH, 2 * LRU_WIDTH, 2 * LRU_WIDTH + ATT_WIDTH, 2 * LRU_WIDTH + ATT_WIDTH + KV_WIDTH)
EVEN_OUT = LRU_WIDTH + ATT_WIDTH

M_HEADS = 8
M_DK = 128
M_DV = 256
M_QK = M_HEADS * M_DK
M_V = M_HEADS * M_DV
M_CHUNK = 64
ODD_IN = 2 * M_QK + 2 * M_V + 4 * M_HEADS
ODD_SPLITS = (2 * M_QK, 2 * M_QK + M_V, 2 * M_QK + 2 * M_V)

N_EXPERTS = 16
EC_FACTOR = 2
D_EXPERT = 1536

kernel_name = 'hybrid_rglru_swa_mlstm_ecmoe_dit'


def rmsnorm(x, w):
    xf = x.astype(jnp.float32)
    y = xf * lax.rsqrt(jnp.mean(xf * xf, axis=-1, keepdims=True) + EPS)
    return (y * w.astype(jnp.float32)).astype(x.dtype)


def modulate(h, shift, scale):
    return h * (1 + scale) + shift


def centred_dwconv(x, w, b):
    n = x.shape[1]
    left = CONV_W // 2
    xp = jnp.pad(x, ((0, 0), (left, CONV_W - 1 - left), (0, 0)))
    y = b
    for j in range(CONV_W):
        y = y + xp[:, j:j + n] * w[j]
    return y


def axial_angles(n):
    rows = n // GRID_W
    inv = jnp.power(ROPE_BASE, -jnp.arange(ROPE_PAIRS, dtype=jnp.float32) / ROPE_PAIRS)
    row = jnp.repeat(jnp.arange(rows, dtype=jnp.float32), GRID_W)
    col = (jnp.arange(rows * GRID_W) % GRID_W).astype(jnp.float32)
    return row[:, None] * inv, col[:, None] * inv


def rope_axis(x, ang):
    cos = jnp.cos(ang)[:, None, :].astype(x.dtype)
    sin = jnp.sin(ang)[:, None, :].astype(x.dtype)
    x1, x2 = jnp.split(x, 2, axis=-1)
    return jnp.concatenate([x1 * cos - x2 * sin, x2 * cos + x1 * sin], axis=-1)


def rope_2d(x, row_ang, col_ang):
    xr, xc = jnp.split(x, 2, axis=-1)
    return jnp.concatenate([rope_axis(xr, row_ang), rope_axis(xc, col_ang)], axis=-1)


def softmax_with_sink(logits, sink):
    s = jnp.broadcast_to(sink.reshape(KV_HEADS, GROUP, 1, 1).astype(jnp.float32), logits.shape[:-1] + (1,))
    return jax.nn.softmax(jnp.concatenate([s, logits], axis=-1), axis=-1)[..., 1:]


def banded_window_attention(q, k, v, ck, cv, sink):
    dt = q.dtype
    b_, n = q.shape[:2]
    m = ck.shape[1]
    nb = n // BLOCK_Q
    qb = q.reshape(b_, nb, BLOCK_Q, KV_HEADS, GROUP, HEAD_DIM)

    def band(t):
        tp = jnp.pad(t, ((0, 0), (BLOCK_Q, BLOCK_Q), (0, 0), (0, 0)))
        return jnp.concatenate([tp[:, j * BLOCK_Q:j * BLOCK_Q + n].reshape(b_, nb, BLOCK_Q, KV_HEADS, HEAD_DIM)
                                for j in range(3)], axis=2)

    kb, vb = band(k), band(v)
    scale = HEAD_DIM ** -0.5
    s_band = jnp.einsum('bnqkgd,bnjkd->bnkgqj', qb, kb).astype(jnp.float32) * scale
    s_ctx = jnp.einsum('bnqkgd,bmkd->bnkgqm', qb, ck).astype(jnp.float32) * scale
    qpos = jnp.arange(n).reshape(nb, BLOCK_Q)
    kpos = (jnp.arange(nb) * BLOCK_Q)[:, None] - BLOCK_Q + jnp.arange(3 * BLOCK_Q)[None, :]
    valid = ((jnp.abs(kpos[:, None, :] - qpos[:, :, None]) <= WINDOW)
             & (kpos >= 0)[:, None, :] & (kpos < n)[:, None, :])
    s_band = jnp.where(valid[None, :, None, None], s_band, -jnp.inf)
    p = softmax_with_sink(jnp.concatenate([s_ctx, s_band], axis=-1), sink).astype(dt)
    out = (jnp.einsum('bnkgqm,bmkd->bnqkgd', p[..., :m], cv)
           + jnp.einsum('bnkgqj,bnjkd->bnqkgd', p[..., m:], vb))
    return out.reshape(b_, n, ATT_WIDTH)


def context_attention(q, k, v, sink):
    dt = q.dtype
    b_, m = q.shape[:2]
    qg = q.reshape(b_, m, KV_HEADS, GROUP, HEAD_DIM)
    s = jnp.einsum('bmkgd,bjkd->bkgmj', qg, k).astype(jnp.float32) * HEAD_DIM ** -0.5
    p = softmax_with_sink(s, sink).astype(dt)
    return jnp.einsum('bkgmj,bjkd->bmkgd', p, v).reshape(b_, m, ATT_WIDTH)


def linear_scan(a, b, h0):
    b = b.at[:, 0].add(a[:, 0] * h0)

    def combine(left, right):
        al, bl = left
        ar, br = right
        return al * ar, ar * bl + br

    _, h = lax.associative_scan(combine, (a, b), axis=1)
    return h


def rglru_scan(x, ra_w, ra_b, ix_w, ix_b, lam, h0):
    b_, n, _ = x.shape
    xh = x.reshape(b_, n, LRU_BLOCKS, LRU_BLOCK)
    r = jax.nn.sigmoid(jnp.einsum('bnhi,hij->bnhj', xh, ra_w) + ra_b).reshape(b_, n, LRU_WIDTH)
    i = jax.nn.sigmoid(jnp.einsum('bnhi,hij->bnhj', xh, ix_w) + ix_b).reshape(b_, n, LRU_WIDTH)
    log_a = -LRU_C * r * jax.nn.softplus(-lam.astype(jnp.float32))
    a = jnp.exp(log_a)
    inp = jnp.sqrt(-jnp.expm1(2.0 * log_a)) * (i * x)
    return linear_scan(a, inp, h0)


def even_mixer(hc, hl, w_in, conv_w, conv_b, ra_w, ra_b, ix_w, ix_b, lam, sink, w_out,
               row_ang, col_ang, with_ctx_out):
    dt = hl.dtype

    def project(h):
        b_, n = h.shape[:2]
        xa, ya, q, k, v = jnp.split(h @ w_in, EVEN_SPLITS, axis=-1)
        xa = centred_dwconv(xa, conv_w, conv_b).astype(jnp.float32)
        return (xa, ya, q.reshape(b_, n, ATT_HEADS, HEAD_DIM),
                k.reshape(b_, n, KV_HEADS, HEAD_DIM), v.reshape(b_, n, KV_HEADS, HEAD_DIM))

    cxa, cya, cq, ck, cv = project(hc)
    lxa, lya, lq, lk, lv = project(hl)
    zero = jnp.zeros((hl.shape[0], LRU_WIDTH), jnp.float32)

    def lru(xa, d, h0):
        return rglru_scan(xa, ra_w[d], ra_b[d], ix_w[d], ix_b[d], lam[d], h0)

    hf_c = lru(cxa, 0, zero)
    hf_l = lru(lxa, 0, hf_c[:, -1])
    hb_c = lru(cxa[:, ::-1], 1, zero)
    hb_l = lru(lxa[:, ::-1], 1, hb_c[:, -1])[:, ::-1]
    lat_a = ((hf_l + hb_l) * jax.nn.gelu(lya)).astype(dt)
    lat_b = banded_window_attention(rope_2d(lq, row_ang, col_ang), rope_2d(lk, row_ang, col_ang),
                                    lv, ck, cv, sink)
    lat = jnp.concatenate([lat_a, lat_b], axis=-1) @ w_out
    if not with_ctx_out:
        return None, lat
    ctx_a = ((hf_c + hb_c[:, ::-1]) * jax.nn.gelu(cya)).astype(dt)
    ctx_b = context_attention(cq, ck, cv, sink)
    return jnp.concatenate([ctx_a, ctx_b], axis=-1) @ w_out, lat


def mlstm_state_update(state, k, v, li, b):
    c0, n0, m0 = state
    b_last = b[..., -1]
    log_u = b_last[..., None] - b + li
    m_new = jnp.maximum(b_last + m0, jnp.max(log_u, axis=-1))
    u = jnp.exp(log_u - m_new[..., None])
    decay = jnp.exp(b_last + m0 - m_new)
    uk = u[..., None] * k
    c_new = decay[..., None, None] * c0 + jnp.einsum('bhsd,bhsv->bhdv', uk, v)
    n_new = decay[..., None] * n0 + jnp.sum(uk, axis=2)
    return (c_new, n_new, m_new)


def mlstm_chunk_step(state, xs):
    c0, n0, m0 = state
    q, k, v, li, lf = xs
    b = jnp.cumsum(lf, axis=-1)
    length = q.shape[2]
    causal = jnp.tril(jnp.ones((length, length), dtype=bool))
    log_w = jnp.where(causal, b[..., :, None] - b[..., None, :] + li[..., None, :], -jnp.inf)
    log_inter = b + m0[..., None]
    m = jnp.maximum(log_inter, jnp.max(log_w, axis=-1))
    w = jnp.exp(log_w - m[..., None])
    inter = jnp.exp(log_inter - m)
    s = jnp.einsum('bhtd,bhsd->bhts', q, k) * w
    num = jnp.einsum('bhts,bhsv->bhtv', s, v) + inter[..., None] * jnp.einsum('bhtd,bhdv->bhtv', q, c0)
    den = jnp.sum(s, axis=-1) + inter * jnp.einsum('bhtd,bhd->bht', q, n0)
    h = num / jnp.maximum(jnp.abs(den), jnp.exp(-m))[..., None]
    return mlstm_state_update(state, k, v, li, b), h


def mlstm_chunkwise(state0, q, k, v, li, lf):
    b_, nh, n, _ = q.shape
    nc = n // M_CHUNK

    def chunks(t):
        return jnp.moveaxis(t.reshape((b_, nh, nc, M_CHUNK) + t.shape[3:]), 2, 0)

    state, h = lax.scan(mlstm_chunk_step, state0, (chunks(q), chunks(k), chunks(v), chunks(li), chunks(lf)))
    return state, jnp.moveaxis(h, 0, 2).reshape(b_, nh, n, M_DV)


def mlstm_direction(c_in, l_in, d, with_ctx_out):
    def orient(t):
        return jnp.flip(t, axis=2) if d == 1 else t

    def select(inp):
        q, k, v, li, lf = inp
        return tuple(orient(t) for t in (q, k, v, li[:, d], lf[:, d]))

    cq, ck, cv, cli, clf = select(c_in)
    lq, lk, lv, lli, llf = select(l_in)
    b_ = cq.shape[0]
    state0 = (jnp.zeros((b_, M_HEADS, M_DK, M_DV), jnp.float32),
              jnp.zeros((b_, M_HEADS, M_DK), jnp.float32),
              jnp.zeros((b_, M_HEADS), jnp.float32))
    if with_ctx_out:
        state_c, hc = mlstm_chunkwise(state0, cq, ck, cv, cli, clf)
        hc = orient(hc)
    else:
        state_c = mlstm_state_update(state0, ck, cv, cli, jnp.cumsum(clf, axis=-1))
        hc = None
    _, hl = mlstm_chunkwise(state_c, lq, lk, lv, lli, llf)
    return hc, orient(hl)


def mlstm_output(h, o, hnorm_w, w_out):
    b_, _, n, _ = h.shape
    h = h.transpose(0, 2, 1, 3)
    h = h * lax.rsqrt(jnp.mean(h * h, axis=-1, keepdims=True) + EPS)
    h = h.reshape(b_, n, M_V) * hnorm_w.astype(jnp.float32) * jax.nn.sigmoid(o.astype(jnp.float32))
    return h.astype(o.dtype) @ w_out


def odd_mixer(hc, hl, w_in, conv_w, conv_b, gate_b, hnorm_w, w_out, with_ctx_out):
    def project(h):
        b_, n = h.shape[:2]
        qk, v, o, g = jnp.split(h @ w_in, ODD_SPLITS, axis=-1)
        q, k = jnp.split(jax.nn.silu(centred_dwconv(qk, conv_w, conv_b)), 2, axis=-1)

        def heads(t, dd):
            return t.reshape(b_, n, M_HEADS, dd).transpose(0, 2, 1, 3).astype(jnp.float32)

        g = (g.reshape(b_, n, 4, M_HEADS).astype(jnp.float32) + gate_b).transpose(0, 2, 3, 1)
        return (heads(q, M_DK) * M_DK ** -0.5, heads(k, M_DK), heads(v, M_DV),
                g[:, 0::2], jax.nn.log_sigmoid(g[:, 1::2])), o

    c_in, co = project(hc)
    l_in, lo = project(hl)
    hf_c, hf_l = mlstm_direction(c_in, l_in, 0, with_ctx_out)
    hb_c, hb_l = mlstm_direction(c_in, l_in, 1, with_ctx_out)
    lat = mlstm_output(hf_l + hb_l, lo, hnorm_w, w_out)
    if not with_ctx_out:
        return None, lat
    return mlstm_output(hf_c + hb_c, co, hnorm_w, w_out), lat


def expert_choice_ffn(h, w_router, w_gate, w_up, w_down):
    b_, n, d = h.shape
    cap = EC_FACTOR * n // N_EXPERTS
    aff = jax.nn.softmax((h @ w_router).astype(jnp.float32), axis=-1)
    g, idx = lax.top_k(jnp.swapaxes(aff, 1, 2), cap)
    xs = jax.vmap(lambda hb, ib: hb[ib])(h, idx)
    hid = jax.nn.silu(jnp.einsum('becd,edf->becf', xs, w_gate)) * jnp.einsum('becd,edf->becf', xs, w_up)
    y = jnp.einsum('becf,efd->becd', hid, w_down) * g[..., None].astype(h.dtype)
    return jax.vmap(lambda yb, ib: jnp.zeros((n, d), h.dtype).at[ib.reshape(-1)].add(yb.reshape(-1, d)))(y, idx)


def setup_inputs(seed: int = 0) -> dict:
    key = jax.random.key(seed)
    keys = jax.random.split(key, 32)
    D = D_MODEL
    ne = (DEPTH + 1) // 2
    no = DEPTH // 2

    def nrm(i, shape, scale):
        return jax.random.normal(keys[i], shape, jnp.float32) * scale

    lam_u = jax.random.uniform(keys[15], (ne, 2, LRU_WIDTH), jnp.float32, 0.9, 0.999)
    lam_a = lam_u ** (1.0 / LRU_C)
    gate_i = nrm(21, (no, 2, M_HEADS), 0.1)
    gate_f = jax.random.uniform(keys[22], (no, 2, M_HEADS), jnp.float32, 3.0, 6.0)
    return {
        'x': nrm(0, (BATCH, SEQ, D), 1.0),
        'c': nrm(1, (BATCH, D), 1.0),
        'ctx': nrm(2, (BATCH, CTX_LEN, D), 1.0),
        'c_ctx': nrm(3, (D,), 1.0),
        'ada_w': nrm(4, (DEPTH, D, N_MOD * D), 0.5 * D ** -0.5),
        'ada_b': nrm(5, (DEPTH, N_MOD * D), 0.02),
        'norm_mix_w': 1.0 + nrm(6, (DEPTH, D), 0.02),
        'norm_ffn_w': 1.0 + nrm(7, (DEPTH, D), 0.02),
        'ev_w_in': nrm(8, (ne, D, EVEN_IN), D ** -0.5),
        'ev_conv_w': nrm(9, (ne, CONV_W, LRU_WIDTH), CONV_W ** -0.5),
        'ev_conv_b': nrm(10, (ne, LRU_WIDTH), 0.02),
        'ev_ra_w': nrm(11, (ne, 2, LRU_BLOCKS, LRU_BLOCK, LRU_BLOCK), LRU_BLOCK ** -0.5),
        'ev_ra_b': nrm(12, (ne, 2, LRU_BLOCKS, LRU_BLOCK), 0.1),
        'ev_ix_w': nrm(13, (ne, 2, LRU_BLOCKS, LRU_BLOCK, LRU_BLOCK), LRU_BLOCK ** -0.5),
        'ev_ix_b': nrm(14, (ne, 2, LRU_BLOCKS, LRU_BLOCK), 0.1),
        'ev_lambda': jnp.log(lam_a) - jnp.log1p(-lam_a),
        'ev_sink': nrm(16, (ne, ATT_HEADS), 0.5),
        'ev_w_out': nrm(17, (ne, EVEN_OUT, D), EVEN_OUT ** -0.5),
        'od_w_in': nrm(18, (no, D, ODD_IN), D ** -0.5),
        'od_conv_w': nrm(19, (no, CONV_W, 2 * M_QK), CONV_W ** -0.5),
        'od_conv_b': nrm(20, (no, 2 * M_QK), 0.02),
        'od_gate_b': jnp.stack([gate_i, gate_f], axis=2).reshape(no, 4, M_HEADS),
        'od_hnorm_w': 1.0 + nrm(23, (no, M_V), 0.02),
        'od_w_out': nrm(24, (no, M_V, D), M_V ** -0.5),
        'moe_router': nrm(25, (DEPTH, D, N_EXPERTS), D ** -0.5),
        'moe_w_gate': nrm(26, (DEPTH, N_EXPERTS, D, D_EXPERT), D ** -0.5),
        'moe_w_up': nrm(27, (DEPTH, N_EXPERTS, D, D_EXPERT), D ** -0.5),
        'moe_w_down': nrm(28, (DEPTH, N_EXPERTS, D_EXPERT, D), D_EXPERT ** -0.5),
        'final_norm_w': 1.0 + nrm(29, (D,), 0.02),
    }


def reference(x, c, ctx, c_ctx, ada_w, ada_b, norm_mix_w, norm_ffn_w,
              ev_w_in, ev_conv_w, ev_conv_b, ev_ra_w, ev_ra_b, ev_ix_w, ev_ix_b, ev_lambda, ev_sink, ev_w_out,
              od_w_in, od_conv_w, od_conv_b, od_gate_b, od_hnorm_w, od_w_out,
              moe_router, moe_w_gate, moe_w_up, moe_w_down, final_norm_w):
    n = x.shape[1]
    row_ang, col_ang = axial_angles(n)
    xl, xc = x, ctx
    silu_c = jax.nn.silu(c)
    silu_cc = jax.nn.silu(c_ctx)
    for layer in range(DEPTH):
        last = layer == DEPTH - 1
        mod_l = jnp.split((silu_c @ ada_w[layer] + ada_b[layer])[:, None, :], N_MOD, axis=-1)
        mod_c = jnp.split(silu_cc @ ada_w[layer] + ada_b[layer], N_MOD, axis=-1)
        hl = modulate(rmsnorm(xl, norm_mix_w[layer]), mod_l[0], mod_l[1])
        hc = modulate(rmsnorm(xc, norm_mix_w[layer]), mod_c[0], mod_c[1])
        if layer % 2 == 0:
            e = layer // 2
            oc, ol = even_mixer(hc, hl, ev_w_in[e], ev_conv_w[e], ev_conv_b[e], ev_ra_w[e], ev_ra_b[e],
                                ev_ix_w[e], ev_ix_b[e], ev_lambda[e], ev_sink[e], ev_w_out[e],
                                row_ang, col_ang, not last)
        else:
            o = layer // 2
            oc, ol = odd_mixer(hc, hl, od_w_in[o], od_conv_w[o], od_conv_b[o], od_gate_b[o],
                               od_hnorm_w[o], od_w_out[o], not last)
        xl = xl + mod_l[2] * ol
        xl = xl + mod_l[5] * expert_choice_ffn(
            modulate(rmsnorm(xl, norm_ffn_w[layer]), mod_l[3], mod_l[4]),
            moe_router[layer], moe_w_gate[layer], moe_w_up[layer], moe_w_down[layer])
        if not last:
            xc = xc + mod_c[2] * oc
            xc = xc + mod_c[5] * expert_choice_ffn(
                modulate(rmsnorm(xc, norm_ffn_w[layer]), mod_c[3], mod_c[4]),
                moe_router[layer], moe_w_gate[layer], moe_w_up[layer], moe_w_down[layer])
    return rmsnorm(xl, final_norm_w)
```

```python
import numpy as np
import concourse.bass as bass
import concourse.mybir as mybir
from concourse.bass_utils import run_bass_kernel_spmd
from contextlib import ExitStack

F32 = mybir.dt.float32
BF16 = mybir.dt.bfloat16
U32 = mybir.dt.uint32
I32 = mybir.dt.int32
ALU = mybir.AluOpType
AF = mybir.ActivationFunctionType
AX = mybir.AxisListType

D = 2048
KC = 16
CTX = 256
SEQ = 4096
T = CTX + SEQ
DEPTH = 4
NE = 16
DEXP = 1536
EPS = 1e-6
NQ = 6
import os
DBG = set(os.environ.get('K_DBG', '').split(','))
SAME_SYNC = True


class Buf:
    __slots__ = ("w", "r", "excl")

    def __init__(self):
        self.w = {}
        self.r = {}
        self.excl = False


class TT:
    def __init__(self, h):
        self.h = h
        self.buf = Buf()
        self.sub = {}

    def __getitem__(self, idx):
        return self.h[idx]

    def b(self, key=None):
        if key is None:
            return self.buf
        s = self.sub.get(key)
        if s is None:
            s = self.sub[key] = Buf()
        return s


def _bufs(lst):
    out = []
    for x in lst:
        if isinstance(x, TT):
            out.append(x.buf)
        elif isinstance(x, Buf):
            out.append(x)
        elif isinstance(x, (list, tuple)):
            out.extend(_bufs(x))
        else:
            raise TypeError(type(x))
    return out


class Sched:
    def __init__(self, nc, es):
        self.nc = nc
        self.es = es
        self.engs = {"pe": nc.tensor, "act": nc.scalar, "dve": nc.vector, "pool": nc.gpsimd, "sp": nc.sync}
        self.semh = {}
        self.ecnt = {}
        for k in ("pe", "act", "dve", "pool"):
            self.semh["E_" + k] = es.enter_context(nc.semaphore("sem_" + k))
            self.ecnt[k] = 0
        self.rings = {}
        for q in ("sp", "act", "pool"):
            self.rings[q] = {"n": 0, "val": [0] * NQ}
            for i in range(NQ):
                self.semh[f"D_{q}_{i}"] = es.enter_context(nc.semaphore(f"dq_{q}_{i}"))
        self.waited = {k: {} for k in self.engs}
        self.n_ops = 0

    def _deps(self, reads, writes):
        deps = {}
        for b in reads:
            for k, v in b.w.items():
                if deps.get(k, 0) < v:
                    deps[k] = v
        for b in writes:
            for k, v in b.w.items():
                if deps.get(k, 0) < v:
                    deps[k] = v
            for k, v in b.r.items():
                if deps.get(k, 0) < v:
                    deps[k] = v
        return deps

    def _wait(self, engname, deps, skip=None):
        e = self.engs[engname]
        wd = self.waited[engname]
        for k, v in deps.items():
            if k == skip:
                continue
            if wd.get(k, 0) < v:
                e.wait_ge(self.semh[k], v)
                wd[k] = v

    def _mark(self, key, v, reads, writes):
        for b in writes:
            b.w = {key: v}
            b.r = {}
        for b in reads:
            if b.r.get(key, 0) < v:
                b.r[key] = v

    def op(self, engname, fn, reads=(), writes=()):
        reads = _bufs(reads)
        writes = _bufs(writes)
        for b in reads:
            if b.excl and b not in writes:
                writes.append(b)
        deps = self._deps(reads, writes)
        key = "E_" + engname
        skip = key if (engname == "pe" or not SAME_SYNC) else None
        self._wait(engname, deps, skip)
        ins = fn(self.engs[engname])
        self.ecnt[engname] += 1
        v = self.ecnt[engname]
        ins.then_inc(self.semh[key], 1)
        self._mark(key, v, [b for b in reads if b not in writes], writes)
        self.n_ops += 1
        return ins

    def dma(self, q, out, in_, reads=(), writes=(), fn=None, **kw):
        reads = _bufs(reads)
        writes = _bufs(writes)
        ring = self.rings[q]
        i = ring["n"] % NQ
        ring["n"] += 1
        key = f"D_{q}_{i}"
        deps = self._deps(reads, writes)
        prev = ring["val"][i]
        if prev and deps.get(key, 0) < prev:
            deps[key] = prev
        self._wait(q, deps)
        e = self.engs[q]
        if fn is not None:
            ins = fn(e)
        else:
            ins = e.dma_start(out=out, in_=in_, **kw)
        ins.then_inc(self.semh[key], 16)
        v = prev + 16
        ring["val"][i] = v
        self._mark(key, v, [b for b in reads if b not in writes], writes)
        self.n_ops += 1
        return ins

    def barrier(self):
        deps = {}
        for k in ("pe", "act", "dve", "pool"):
            if self.ecnt[k]:
                deps["E_" + k] = self.ecnt[k]
        for q, ring in self.rings.items():
            for i in range(NQ):
                if ring["val"][i]:
                    deps[f"D_{q}_{i}"] = ring["val"][i]
        for e in self.engs:
            self._wait(e, deps)


def _vec_layout():
    off = {}
    n = 0

    def add(name, cols):
        nonlocal n
        off[name] = n
        n += cols

    add("c", 16)
    add("cctx", 16)
    for l in range(DEPTH):
        add(f"ada_b{l}", 96)
        add(f"nmix{l}", 16)
        add(f"nffn{l}", 16)
    add("fnorm", 16)
    for e in range(2):
        add(f"ev_cw{e}", 32)
        add(f"ev_cb{e}", 8)
        add(f"ev_rab{e}", 16)
        add(f"ev_ixb{e}", 16)
        add(f"ev_lam{e}", 16)
        add(f"ev_sink{e}", 8)
    for o in range(2):
        add(f"od_cw{o}", 64)
        add(f"od_cb{o}", 16)
        add(f"od_gb{o}", 32)
    return off, n


VOFF, NV = _vec_layout()


def _pm(v):
    v = np.asarray(v, np.float32).reshape(-1, 128)
    return np.ascontiguousarray(v.T)


def pack_vecs(inp, b):
    V = np.zeros((128, NV), np.float32)

    def put(name, arr):
        arr = np.asarray(arr, np.float32)
        V[: arr.shape[0], VOFF[name]: VOFF[name] + arr.shape[1]] = arr

    put("c", _pm(inp["c"][b]))
    put("cctx", _pm(inp["c_ctx"]))
    for l in range(DEPTH):
        put(f"ada_b{l}", _pm(inp["ada_b"][l]))
        put(f"nmix{l}", _pm(inp["norm_mix_w"][l]))
        put(f"nffn{l}", _pm(inp["norm_ffn_w"][l]))
    put("fnorm", _pm(inp["final_norm_w"]))
    for e in range(2):
        put(f"ev_cw{e}", np.concatenate([_pm(inp["ev_conv_w"][e, j]) for j in range(4)], axis=1))
        put(f"ev_cb{e}", _pm(inp["ev_conv_b"][e]))
        put(f"ev_rab{e}", np.concatenate([_pm(inp["ev_ra_b"][e, d].reshape(-1)) for d in range(2)], axis=1))
        put(f"ev_ixb{e}", np.concatenate([_pm(inp["ev_ix_b"][e, d].reshape(-1)) for d in range(2)], axis=1))
        put(f"ev_lam{e}", np.concatenate([_pm(inp["ev_lambda"][e, d]) for d in range(2)], axis=1))
        put(f"ev_sink{e}", np.broadcast_to(np.asarray(inp["ev_sink"][e], np.float32)[None, :], (128, 8)))
    for o in range(2):
        put(f"od_cw{o}", np.concatenate([_pm(inp["od_conv_w"][o, j]) for j in range(4)], axis=1))
        put(f"od_cb{o}", _pm(inp["od_conv_b"][o]))
        put(f"od_gb{o}", np.broadcast_to(np.asarray(inp["od_gate_b"][o], np.float32).reshape(1, 32), (128, 32)))
    return V


def make_consts():
    import ml_dtypes
    bf = ml_dtypes.bfloat16
    c = {}
    c["ident_f"] = np.eye(128, dtype=np.float32)
    cb = np.zeros((128, 6, 128), np.float32)
    cb[:, 0, :] = np.eye(128)
    cb[:, 1, :] = 1.0
    j = np.arange(128)[:, None]
    i = np.arange(128)[None, :]
    cb[:, 2, :] = (j >= i)
    cb[:, 3, :] = (j <= i)
    R = np.zeros((128, 128), np.float32)
    for d in range(128):
        p = d + 32 if (d % 64) < 32 else d - 32
        R[p, d] = 1.0
    cb[:, 4, :] = R
    c["cb"] = cb.astype(bf)
    pairs = 32
    inv = np.power(10000.0, -np.arange(pairs, dtype=np.float32) / pairs).astype(np.float32)
    t = np.arange(SEQ)
    row = (t // 64).astype(np.float32)
    col = (t % 64).astype(np.float32)
    ra = (row[:, None] * inv).astype(np.float32)
    ca = (col[:, None] * inv).astype(np.float32)
    cosT = np.zeros((128, SEQ), np.float32)
    sinT = np.zeros((128, SEQ), np.float32)
    cosT[0:32] = np.cos(ra).T
    cosT[32:64] = np.cos(ra).T
    cosT[64:96] = np.cos(ca).T
    cosT[96:128] = np.cos(ca).T
    sinT[0:32] = -np.sin(ra).T
    sinT[32:64] = np.sin(ra).T
    sinT[64:96] = -np.sin(ca).T
    sinT[96:128] = np.sin(ca).T
    c["cosT"] = cosT
    c["sinT"] = sinT
    cf = np.zeros((128, 5, 128), np.float32)
    cf[:, 0, :] = (j <= i)
    cf[:, 1, :] = (j >= i)
    cf[:, 2, :] = np.where(j <= i, 0.0, -30000.0)
    cf[:, 3, :] = np.where(j >= i, 0.0, -30000.0)
    cf[:, 4, :] = 1.0
    c["cf"] = cf
    return c


TBLOCKS = [(0, 256)] + [(256 + 512 * i, 512) for i in range(8)]


class Prog:
    def __init__(self, dbg_in=(), dbg_out=()):
        self.nc = bass.Bass("TRN2", target_bir_lowering=False)
        self.es = ExitStack()
        self.S = Sched(self.nc, self.es)
        self.dbg_in = set(dbg_in)
        self.dbg_out = set(dbg_out)
        self.ext = {}
        nc = self.nc
        self.ps = [TT(self.es.enter_context(nc.psum_tensor(f"ps{i}", [128, 512], F32))) for i in range(8)]
        for p_ in self.ps:
            p_.buf.excl = True
        self.psi = 0
        self.uid = 0

    def dram_in(self, name, shape, dtype=F32):
        t = TT(self.nc.dram_tensor(name, list(shape), dtype, kind="ExternalInput").ap())
        self.ext[name] = t
        return t

    def dram_out(self, name, shape, dtype=F32):
        t = TT(self.nc.dram_tensor(name, list(shape), dtype, kind="ExternalOutput").ap())
        self.ext[name] = t
        return t

    def dram(self, name, shape, dtype):
        kind = "ExternalInput" if name in self.dbg_in else ("ExternalOutput" if name in self.dbg_out else "Internal")
        t = TT(self.nc.dram_tensor(name, list(shape), dtype, kind=kind).ap())
        self.ext[name] = t
        return t

    def sb(self, es, name, shape, dtype):
        self.uid += 1
        return TT(es.enter_context(self.nc.sbuf_tensor(f"{name}_{self.uid}", list(shape), dtype)))

    def psum(self):
        p = self.ps[self.psi % 8]
        self.psi += 1
        return p

    def setup(self):
        S = self.S
        es = self.es
        self.vecs_d = self.dram_in("vecs", [128, NV])
        self.cb_d = self.dram_in("cb", [128, 6, 128], BF16)
        self.identf_d = self.dram_in("ident_f", [128, 128])
        self.V = self.sb(es, "V", [128, NV], F32)
        self.CB = self.sb(es, "CB", [128, 6, 128], BF16)
        self.IDF = self.sb(es, "IDF", [128, 128], F32)
        self.MOD = self.sb(es, "MOD", [128, DEPTH, 96, 2], F32)
        self.epsT = self.sb(es, "epsT", [128, 1], F32)
        S.dma("sp", self.V[:], self.vecs_d[:], reads=[self.vecs_d], writes=[self.V])
        S.dma("sp", self.CB[:], self.cb_d[:], reads=[self.cb_d], writes=[self.CB])
        S.dma("sp", self.IDF[:], self.identf_d[:], reads=[self.identf_d], writes=[self.IDF])
        S.op("dve", lambda e: e.memset(self.epsT[:], EPS), writes=[self.epsT])
        self.IDB = self.CB[:, 0, :]
        self.ONESB = self.CB[:, 1, :]

    def vcol(self, name, c0=0, n=1):
        o = VOFF[name] + c0
        return self.V[:, o:o + n]

    def phase_mod(self, ada_w, layers):
        S = self.S
        with ExitStack() as es:
            s2 = self.sb(es, "s2", [128, 16, 2], F32)
            S.op("act", lambda e: e.activation(out=s2[:, :, 0], in_=self.vcol("c", 0, 16), func=AF.Silu),
                 reads=[self.V], writes=[s2])
            S.op("act", lambda e: e.activation(out=s2[:, :, 1], in_=self.vcol("cctx", 0, 16), func=AF.Silu),
                 reads=[self.V], writes=[s2])
            wt = [self.sb(es, f"adaw{i}", [128, 16, 1024], F32) for i in range(2)]
            it = 0
            for l in layers:
                W = ada_w[l]
                acc = self.psum()
                for g in range(12):
                    w = wt[it % 2]
                    it += 1
                    for kc in range(16):
                        S.dma("sp", w[:, kc, :], W[kc * 128:(kc + 1) * 128, g * 1024:(g + 1) * 1024],
                              reads=[W], writes=[w])
                    for sub in range(8):
                        col = (g * 8 + sub) * 2
                        for kc in range(16):
                            S.op("pe", lambda e, kc=kc, sub=sub, col=col, w=w: e.matmul(
                                acc[:, col:col + 2], lhsT=w[:, kc, sub * 128:(sub + 1) * 128], rhs=s2[:, kc, :],
                                start=(kc == 0), stop=(kc == 15)), reads=[w, s2], writes=[acc])
                accv = acc[:, 0:192].rearrange("p (n w) -> p n w", w=2)
                for wh in range(2):
                    S.op("dve", lambda e, wh=wh, l=l, accv=accv: e.tensor_tensor(
                        out=self.MOD[:, l, :, wh], in0=accv[:, :, wh], in1=self.vcol(f"ada_b{l}", 0, 96), op=ALU.add),
                        reads=[acc, self.V], writes=[self.MOD])
            S.barrier()

    def mod(self, l, j, kc, wh):
        return self.MOD[:, l, j * 16 + kc, wh:wh + 1]

    def phase_norm(self, XT, l, jshift, nwname, consume, es, want_f32=False):
        S = self.S
        A = self.sb(es, "A", [128, 16, 2], F32)
        for wh in range(2):
            S.op("dve", lambda e, wh=wh: e.scalar_tensor_tensor(
                out=A[:, :, wh], in0=self.MOD[:, l, (jshift + 1) * 16:(jshift + 2) * 16, wh], scalar=1.0,
                in1=self.vcol(nwname, 0, 16), op0=ALU.add, op1=ALU.mult), reads=[self.MOD, self.V], writes=[A])
        xts = [self.sb(es, f"xt{i}", [128, 16, 512], F32) for i in range(2)]
        sqs = [self.sb(es, f"sq{i}", [128, 16, 512], BF16) for i in range(1)]
        rstd = [self.sb(es, f"rstd{i}", [128, 512], F32) for i in range(2)]
        hbs = [self.sb(es, f"hb{i}", [128, 16, 512], BF16) for i in range(2)]
        XTv = XT.h.rearrange("(kc p) t -> p kc t", p=128)
        for tb, (t0, tn) in enumerate(TBLOCKS):
            wh = 1 if tb == 0 else 0
            xt = xts[tb % 2]
            sq = sqs[0]
            rs = rstd[tb % 2]
            hb = hbs[tb % 2]
            S.dma("sp", xt[:, :, :tn], XTv[:, :, t0:t0 + tn], reads=[XT.b((kc, tb)) for kc in range(16)], writes=[xt])
            S.op("act", lambda e: e.activation(out=sq[:, :, :tn], in_=xt[:, :, :tn], func=AF.Square),
                 reads=[xt], writes=[sq])
            acc = self.psum()
            for kc in range(16):
                S.op("pe", lambda e, kc=kc: e.matmul(acc[:, :tn], lhsT=self.ONESB, rhs=sq[:, kc, :tn],
                                                      start=(kc == 0), stop=(kc == 15)),
                     reads=[sq, self.CB], writes=[acc])
            S.op("act", lambda e: e.activation(out=rs[:, :tn], in_=acc[:, :tn], func=AF.Sqrt,
                                               bias=self.epsT[:, 0:1], scale=1.0 / D),
                 reads=[acc, self.epsT], writes=[rs])
            S.op("dve", lambda e: e.reciprocal(out=rs[:, :tn], in_=rs[:, :tn]), reads=[rs], writes=[rs])
            hf = xt if want_f32 else None
            for kc in range(16):
                S.op("dve", lambda e, kc=kc: e.scalar_tensor_tensor(
                    out=xt[:, kc, :tn], in0=xt[:, kc, :tn], scalar=A[:, kc, wh:wh + 1], in1=rs[:, :tn],
                    op0=ALU.mult, op1=ALU.mult), reads=[xt, A, rs], writes=[xt])
                S.op("act", lambda e, kc=kc: e.activation(
                    out=hb[:, kc, :tn], in_=xt[:, kc, :tn], func=AF.Identity,
                    bias=self.mod(l, jshift, kc, wh), scale=1.0), reads=[xt, self.MOD], writes=[hb])
                if want_f32:
                    S.op("pool", lambda e, kc=kc: e.tensor_scalar(
                        out=xt[:, kc, :tn], in0=xt[:, kc, :tn], scalar1=self.mod(l, jshift, kc, wh), scalar2=None,
                        op0=ALU.add), reads=[self.MOD], writes=[xt])
            consume(tb, t0, tn, hb, hf)

    def linear(self, es, AT, W, Wap, K, c0, c1, mode, epilogue, tblocks=None):
        S = self.S
        KCn = K // 128
        wts = [self.sb(es, f"lw{i}", [128, KCn, 512], BF16) for i in range(2)]
        ats = [self.sb(es, f"la{i}", [128, KCn, 512], BF16) for i in range(2)]
        ATv = AT.h.rearrange("(kc p) t -> p kc t", p=128)
        Wv = Wap.rearrange("(kc p) n -> p kc n", p=128)
        tbl = list(enumerate(TBLOCKS)) if tblocks is None else tblocks
        wi = 0
        ai = 0
        for cs in range(c0, c1, 512):
            ncol = min(512, c1 - cs)
            wt = wts[wi % 2]
            wi += 1
            for kc in range(KCn):
                S.dma("pool", wt[:, kc, :ncol], Wv[:, kc, cs:cs + ncol], reads=[W], writes=[wt])
            for tb, (t0, tn) in tbl:
                at = ats[ai % 2]
                ai += 1
                S.dma("sp", at[:, :, :tn], ATv[:, :, t0:t0 + tn], reads=[AT.b(tb)], writes=[at])
                if mode == "fm":
                    for sub in range(ncol // 128):
                        ps = self.psum()
                        for kc in range(KCn):
                            S.op("pe", lambda e, kc=kc, sub=sub, ps=ps, wt=wt, at=at, tn=tn: e.matmul(
                                ps[:, :tn], lhsT=wt[:, kc, sub * 128:(sub + 1) * 128], rhs=at[:, kc, :tn],
                                start=(kc == 0), stop=(kc == KCn - 1)), reads=[wt, at], writes=[ps])
                        epilogue(ps, cs + sub * 128, tb, t0, tn)
                else:
                    for s_ in range(tn // 128):
                        ps = self.psum()
                        for kc in range(KCn):
                            S.op("pe", lambda e, kc=kc, s_=s_, ps=ps, wt=wt, at=at, ncol=ncol: e.matmul(
                                ps[:, :ncol], lhsT=at[:, kc, s_ * 128:(s_ + 1) * 128], rhs=wt[:, kc, :ncol],
                                start=(kc == 0), stop=(kc == KCn - 1)), reads=[wt, at], writes=[ps])
                        epilogue(ps, cs, ncol, tb, t0 + s_ * 128)

    def even_proj(self, l, e, w_in, HT, XA, YG, QK, VV, cos_d, sin_d):
        S = self.S
        with ExitStack() as es:
            cosS = self.sb(es, "cosS", [128, SEQ], F32)
            sinS = self.sb(es, "sinS", [128, SEQ], F32)
            S.dma("sp", cosS[:], cos_d[:], reads=[cos_d], writes=[cosS])
            S.dma("sp", sinS[:], sin_d[:], reads=[sin_d], writes=[sinS])
            st_f = [self.sb(es, f"stf{i}", [128, 512], F32) for i in range(2)]
            st_g = [self.sb(es, f"stg{i}", [128, 512], F32) for i in range(2)]
            st_b = [self.sb(es, f"stb{i}", [128, 512], BF16) for i in range(2)]
            st_o = [self.sb(es, f"sto{i}", [128, 512], BF16) for i in range(2)]
            cnt = [0]
            R = self.CB[:, 4, :]

            def epi(ps, col, tb, t0, tn):
                i = cnt[0] % 2
                cnt[0] += 1
                sf, sg, sbb, so = st_f[i], st_g[i], st_b[i], st_o[i]
                if 'noepi' in DBG:
                    S.op("act", lambda e_: e_.copy(out=sf[:, :tn], in_=ps[:, :tn]), reads=[ps], writes=[sf])
                    return
                if ('onlyxa' in DBG and col >= 1024) or ('onlyya' in DBG and not (1024 <= col < 2048)) or ('onlyqk' in DBG and col < 2048):
                    S.op("act", lambda e_: e_.copy(out=sf[:, :tn], in_=ps[:, :tn]), reads=[ps], writes=[sf])
                    return
                if col < 1024:
                    S.op("act", lambda e_: e_.copy(out=sf[:, :tn], in_=ps[:, :tn]), reads=[ps], writes=[sf])
                    S.dma("sp", XA[col:col + 128, t0:t0 + tn], sf[:, :tn], reads=[sf], writes=[XA.b((col // 128, tb))])
                elif col < 2048:
                    c = col - 1024
                    S.op("act", lambda e_: e_.activation(out=sf[:, :tn], in_=ps[:, :tn], func=AF.Square),
                         reads=[ps], writes=[sf])
                    S.op("dve", lambda e_: e_.tensor_scalar(out=sf[:, :tn], in0=sf[:, :tn], scalar1=0.044715,
                                                            scalar2=1.0, op0=ALU.mult, op1=ALU.add),
                         reads=[sf], writes=[sf])
                    S.op("dve", lambda e_: e_.tensor_tensor(out=sf[:, :tn], in0=ps[:, :tn], in1=sf[:, :tn], op=ALU.mult),
                         reads=[ps, sf], writes=[sf])
                    S.op("act", lambda e_: e_.activation(out=sg[:, :tn], in_=sf[:, :tn], func=AF.Sigmoid,
                                                         scale=1.5957691216057308), reads=[sf], writes=[sg])
                    S.op("dve", lambda e_: e_.tensor_tensor(out=so[:, :tn], in0=ps[:, :tn], in1=sg[:, :tn], op=ALU.mult),
                         reads=[ps, sg], writes=[so])
                    S.dma("sp", YG[c:c + 128, t0:t0 + tn], so[:, :tn], reads=[so], writes=[YG.b((c // 128, tb))])
                else:
                    c = col - 2048
                    if tb == 0:
                        S.op("act", lambda e_: e_.copy(out=so[:, :tn], in_=ps[:, :tn]), reads=[ps], writes=[so])
                    else:
                        p0 = t0 - CTX
                        S.op("act", lambda e_: e_.copy(out=sbb[:, :tn], in_=ps[:, :tn]), reads=[ps], writes=[sbb])
                        ps2 = self.psum()
                        S.op("pe", lambda e_: e_.matmul(ps2[:, :tn], lhsT=R, rhs=sbb[:, :tn], start=True, stop=True),
                             reads=[sbb, self.CB], writes=[ps2])
                        S.op("dve", lambda e_: e_.tensor_tensor(out=sf[:, :tn], in0=ps[:, :tn], in1=cosS[:, p0:p0 + tn],
                                                                op=ALU.mult), reads=[ps, cosS], writes=[sf])
                        S.op("dve", lambda e_: e_.tensor_tensor(out=sg[:, :tn], in0=ps2[:, :tn], in1=sinS[:, p0:p0 + tn],
                                                                op=ALU.mult), reads=[ps2, sinS], writes=[sg])
                        S.op("pool", lambda e_: e_.tensor_tensor(out=so[:, :tn], in0=sf[:, :tn], in1=sg[:, :tn], op=ALU.add),
                             reads=[sf, sg], writes=[so])
                    S.dma("sp", QK[c:c + 128, t0:t0 + tn], so[:, :tn], reads=[so], writes=[QK.b((c // 128, tb))])

            self.linear(es, HT, w_in, w_in[:], D, 0, 3328, "fm", epi)

            def epi_v(ps, cs, ncol, tb, tok0):
                i = cnt[0] % 2
                cnt[0] += 1
                so = st_o[i]
                S.op("act", lambda e_: e_.copy(out=so[:, :ncol], in_=ps[:, :ncol]), reads=[ps], writes=[so])
                S.dma("sp", VV[tok0:tok0 + 128, :], so[:, :ncol], reads=[so], writes=[VV.b(tok0 // 128)])

            if 'nov' not in DBG:
                self.linear(es, HT, w_in, w_in[:], D, 3328, 3584, "tm", epi_v)
            S.barrier()

    def even_lru(self, e, raw_d, ixw_d, XA, YG, MIXT):
        S = self.S
        with ExitStack() as es:
            cn = self.sb(es, "cn", [128, 16], F32)
            cn2 = self.sb(es, "cn2", [128, 16], F32)
            S.op("act", lambda e_: e_.activation(out=cn[:], in_=self.vcol(f"ev_lam{e}", 0, 16), func=AF.Exp, scale=-1.0),
                 reads=[self.V], writes=[cn])
            S.op("act", lambda e_: e_.activation(out=cn[:], in_=cn[:], func=AF.Ln, bias=1.0, scale=1.0),
                 reads=[cn], writes=[cn])
            S.op("dve", lambda e_: e_.tensor_scalar(out=cn2[:], in0=cn[:], scalar1=-16.0, scalar2=None, op0=ALU.mult),
                 reads=[cn], writes=[cn2])
            S.op("dve", lambda e_: e_.tensor_scalar(out=cn[:], in0=cn[:], scalar1=-8.0, scalar2=None, op0=ALU.mult),
                 reads=[cn], writes=[cn])
            gw = self.sb(es, "gw", [128, 2, 2, 8, 128], BF16)
            for gi, wd in enumerate((raw_d, ixw_d)):
                for d in range(2):
                    S.dma("pool", gw[:, gi, d, :, :], wd[e, d].rearrange("h i j -> i h j"), reads=[wd], writes=[gw])
            B = [self.sb(es, f"lru{i}", [128, T], F32) for i in range(7)]
            xab = self.sb(es, "xab", [128, T], BF16)
            ygt = self.sb(es, "ygt", [128, T], BF16)
            outb = self.sb(es, "outb", [128, T], BF16)
            xp, xa, Rb, Ib, Ab, HF, HB = B
            segs = [(0, CTX), (CTX, T)]
            for h in range(8):
                S.dma("sp", xp[:], XA[h * 128:(h + 1) * 128, :], reads=[XA.b((h, tb)) for tb in range(9)], writes=[xp])
                S.dma("sp", ygt[:], YG[h * 128:(h + 1) * 128, :], reads=[YG.b((h, tb)) for tb in range(9)], writes=[ygt])
                cw = lambda j: self.vcol(f"ev_cw{e}", j * 8 + h, 1)
                for (a0, a1) in segs:
                    S.op("dve", lambda e_, a0=a0, a1=a1: e_.tensor_scalar(
                        out=xa[:, a0:a1], in0=xp[:, a0:a1], scalar1=cw(2), scalar2=self.vcol(f"ev_cb{e}", h, 1),
                        op0=ALU.mult, op1=ALU.add), reads=[xp, self.V], writes=[xa])
                    for j, off in ((0, -2), (1, -1), (3, 1)):
                        lo = max(a0, a0 - off)
                        hi = min(a1, a1 - off)
                        S.op("dve", lambda e_, j=j, off=off, lo=lo, hi=hi: e_.scalar_tensor_tensor(
                            out=xa[:, lo:hi], in0=xp[:, lo + off:hi + off], scalar=cw(j), in1=xa[:, lo:hi],
                            op0=ALU.mult, op1=ALU.add), reads=[xp, self.V], writes=[xa])
                S.op("pool", lambda e_: e_.tensor_copy(out=xab[:], in_=xa[:]), reads=[xa], writes=[xab])
                for d in range(2):
                    col = d * 8 + h
                    for gi, dst, bname in ((0, Rb, f"ev_rab{e}"), (1, Ib, f"ev_ixb{e}")):
                        for (t0, tn) in TBLOCKS:
                            ps = self.psum()
                            S.op("pe", lambda e_, ps=ps, gi=gi, t0=t0, tn=tn: e_.matmul(
                                ps[:, :tn], lhsT=gw[:, gi, d, h, :], rhs=xab[:, t0:t0 + tn], start=True, stop=True),
                                reads=[gw, xab], writes=[ps])
                            S.op("act", lambda e_, ps=ps, dst=dst, t0=t0, tn=tn, bname=bname: e_.activation(
                                out=dst[:, t0:t0 + tn], in_=ps[:, :tn], func=AF.Sigmoid,
                                bias=self.vcol(bname, col, 1), scale=1.0), reads=[ps, self.V], writes=[dst])
                    S.op("act", lambda e_: e_.activation(out=Ab[:], in_=Rb[:], func=AF.Exp, scale=cn[:, col:col + 1]),
                         reads=[Rb, cn], writes=[Ab])
                    S.op("act", lambda e_: e_.activation(out=xp[:], in_=Rb[:], func=AF.Exp, scale=cn2[:, col:col + 1]),
                         reads=[Rb, cn2], writes=[xp])
                    S.op("dve", lambda e_: e_.tensor_scalar(out=xp[:], in0=xp[:], scalar1=-1.0, scalar2=1.0,
                                                            op0=ALU.mult, op1=ALU.add), reads=[xp], writes=[xp])
                    S.op("act", lambda e_: e_.activation(out=xp[:], in_=xp[:], func=AF.Sqrt), reads=[xp], writes=[xp])
                    S.op("pool", lambda e_: e_.tensor_tensor(out=Ib[:], in0=Ib[:], in1=xa[:], op=ALU.mult),
                         reads=[xa], writes=[Ib])
                    S.op("pool", lambda e_: e_.tensor_tensor(out=Ib[:], in0=Ib[:], in1=xp[:], op=ALU.mult),
                         reads=[xp], writes=[Ib])
                    if d == 0:
                        S.op("dve", lambda e_: e_.tensor_tensor_scan(
                            out=HF[:], data0=Ab[:], data1=Ib[:], initial=0.0, op0=ALU.mult, op1=ALU.add),
                            reads=[Ab, Ib], writes=[HF])
                    else:
                        S.op("dve", lambda e_: e_.tensor_tensor_scan(
                            out=HB[:, 0:CTX][:, ::-1], data0=Ab[:, 0:CTX][:, ::-1], data1=Ib[:, 0:CTX][:, ::-1],
                            initial=0.0, op0=ALU.mult, op1=ALU.add), reads=[Ab, Ib], writes=[HB])
                        S.op("dve", lambda e_: e_.tensor_tensor_scan(
                            out=HB[:, CTX:T][:, ::-1], data0=Ab[:, CTX:T][:, ::-1], data1=Ib[:, CTX:T][:, ::-1],
                            initial=HB[:, 0:1], op0=ALU.mult, op1=ALU.add), reads=[Ab, Ib], writes=[HB])
                S.op("pool", lambda e_: e_.tensor_tensor(out=HF[:], in0=HF[:], in1=HB[:], op=ALU.add),
                     reads=[HB], writes=[HF])
                S.op("pool", lambda e_: e_.tensor_tensor(out=outb[:], in0=HF[:], in1=ygt[:], op=ALU.mult),
                     reads=[HF, ygt], writes=[outb])
                S.dma("sp", MIXT[h * 128:(h + 1) * 128, :], outb[:], reads=[outb],
                      writes=[MIXT.b((h, tb)) for tb in range(9)])
            S.barrier()

    def even_attn(self, e, QK, VV, MIXT):
        S = self.S
        scale = 128.0 ** -0.5
        with ExitStack() as es:
            SE = self.sb(es, "SE", [128, 8], F32)
            S.op("act", lambda e_: e_.activation(out=SE[:], in_=self.vcol(f"ev_sink{e}", 0, 8), func=AF.Exp),
                 reads=[self.V], writes=[SE])
            MK = self.sb(es, "MK", [128, 2, 4, 128], BF16)
            for mi in range(2):
                for g in range(4):
                    S.op("pool", lambda e_, mi=mi, g=g: e_.tensor_copy(out=MK[:, mi, g, :], in_=self.CB[:, 2 + mi, :]),
                         reads=[self.CB], writes=[MK])
            KT = self.sb(es, "KT", [128, T], BF16)
            Vt = self.sb(es, "Vt", [128, 34, 128], BF16)
            QT = self.sb(es, "QT", [128, 4, T], BF16)
            OUT = self.sb(es, "OUT", [128, 4, T], BF16)
            ESB = [self.sb(es, f"esb{i}", [128, 5, 512], BF16) for i in range(2)]
            TMP = [self.sb(es, f"atmp{i}", [128, 512], F32) for i in range(2)]
            allqk = lambda c: [QK.b((c, tb)) for tb in range(9)]
            for kv in range(2):
                S.dma("sp", KT[:], QK[(8 + kv) * 128:(9 + kv) * 128, :], reads=allqk(8 + kv), writes=[KT])
                S.dma("sp", Vt[:], VV[:, kv * 128:(kv + 1) * 128].rearrange("(n p) d -> p n d", p=128),
                      reads=[VV.b(i) for i in range(34)], writes=[Vt])
                for g in range(4):
                    S.dma("sp", QT[:, g, :], QK[(kv * 4 + g) * 128:(kv * 4 + g + 1) * 128, :],
                          reads=allqk(kv * 4 + g), writes=[QT])
                for qb in range(34):
                    t0 = qb * 128
                    if qb < 2:
                        kts = [(0, None), (1, None)]
                    else:
                        kts = [(0, None), (1, None)]
                        if qb - 2 >= 1:
                            kts.append((qb - 1, 0))
                        kts.append((qb, None))
                        if qb - 2 <= 30:
                            kts.append((qb + 1, 1))
                    esb = ESB[qb % 2]
                    tmp = TMP[qb % 2]
                    nk = len(kts)
                    for i, (kt, mk) in enumerate(kts):
                        ps = self.psum()
                        S.op("pe", lambda e_, ps=ps, kt=kt: e_.matmul(
                            ps[:, 0:512].rearrange("p (g q) -> p g q", g=4), lhsT=KT[:, kt * 128:(kt + 1) * 128],
                            rhs=QT[:, :, t0:t0 + 128], start=True, stop=True), reads=[KT, QT], writes=[ps])
                        S.op("act", lambda e_, ps=ps, i=i: e_.activation(out=esb[:, i, :], in_=ps[:, 0:512], func=AF.Exp,
                                                                       scale=scale), reads=[ps], writes=[esb])
                        if mk is not None:
                            S.op("pool", lambda e_, i=i, mk=mk: e_.tensor_tensor(
                                out=esb[:, i, :], in0=esb[:, i, :], in1=MK[:, mk].rearrange("p g q -> p (g q)"),
                                op=ALU.mult), reads=[MK], writes=[esb])
                    psd = self.psum()
                    for i in range(nk):
                        S.op("pe", lambda e_, i=i: e_.matmul(psd[:, 0:512], lhsT=self.ONESB, rhs=esb[:, i, :],
                                                             start=(i == 0), stop=(i == nk - 1)),
                             reads=[esb, self.CB], writes=[psd])
                    pso = self.psum()
                    for i, (kt, mk) in enumerate(kts):
                        S.op("pe", lambda e_, i=i, kt=kt: e_.matmul(pso[:, 0:512], lhsT=Vt[:, kt, :], rhs=esb[:, i, :],
                                                                     start=(i == 0), stop=(i == nk - 1)),
                             reads=[esb, Vt], writes=[pso])
                    for g in range(4):
                        S.op("dve", lambda e_, g=g: e_.tensor_scalar(
                            out=tmp[:, g * 128:(g + 1) * 128], in0=psd[:, g * 128:(g + 1) * 128],
                            scalar1=SE[:, kv * 4 + g:kv * 4 + g + 1], scalar2=None, op0=ALU.add),
                            reads=[psd, SE], writes=[tmp])
                    S.op("dve", lambda e_: e_.reciprocal(out=tmp[:], in_=tmp[:]), reads=[tmp], writes=[tmp])
                    S.op("dve", lambda e_: e_.tensor_tensor(
                        out=OUT[:, :, t0:t0 + 128], in0=pso[:, 0:512].rearrange("p (g q) -> p g q", g=4),
                        in1=tmp[:].rearrange("p (g q) -> p g q", g=4), op=ALU.mult), reads=[pso, tmp], writes=[OUT])
                for g in range(4):
                    hh = 8 + kv * 4 + g
                    S.dma("sp", MIXT[hh * 128:(hh + 1) * 128, :], OUT[:, g, :], reads=[OUT],
                          writes=[MIXT.b((hh, tb)) for tb in range(9)])
            S.barrier()

    def out_proj_residual(self, l, jgate, MIXT, w_out, XT):
        S = self.S
        with ExitStack() as es:
            xr = [self.sb(es, f"xr{i}", [128, 512], F32) for i in range(3)]
            cnt = [0]

            def epi(ps, col, tb, t0, tn):
                x = xr[cnt[0] % 3]
                cnt[0] += 1
                kc = col // 128
                wh = 1 if tb == 0 else 0
                S.dma("sp", x[:, :tn], XT[col:col + 128, t0:t0 + tn], reads=[XT.b((kc, tb))], writes=[x])
                S.op("dve", lambda e_: e_.scalar_tensor_tensor(
                    out=x[:, :tn], in0=ps[:, :tn], scalar=self.mod(l, jgate, kc, wh), in1=x[:, :tn],
                    op0=ALU.mult, op1=ALU.add), reads=[ps, self.MOD], writes=[x])
                S.dma("sp", XT[col:col + 128, t0:t0 + tn], x[:, :tn], reads=[x], writes=[XT.b((kc, tb))])

            for tb in range(9):
                agg = MIXT.b(tb)
                for c in range(16):
                    sb_ = MIXT.b((c, tb))
                    for k, v in sb_.w.items():
                        if agg.w.get(k, 0) < v:
                            agg.w[k] = v
            self.linear(es, MIXT, w_out, w_out[:], D, 0, D, "fm", epi)
            S.barrier()

    def build(self, layers=range(DEPTH), do_mixer=True, do_moe=True, do_final=True, stop=99):
        S = self.S
        self.setup()
        xT_in = self.dram_in("xT", [D, T])
        XT = self.dram("XT", [D, T], F32)
        for kc in range(16):
            S.dma("sp", XT[kc * 128:(kc + 1) * 128, :], xT_in[kc * 128:(kc + 1) * 128, :], reads=[xT_in], writes=[XT])
        S.barrier()
        OUT = self.dram_out("out", [SEQ, D]) if do_final else None
        ada_w = {l: self.dram_in(f"ada_w{l}", [D, 6 * D]) for l in layers}
        self.cos_d = self.dram_in("cosT", [128, SEQ])
        self.sin_d = self.dram_in("sinT", [128, SEQ])
        HT = self.dram("HT", [D, T], BF16)
        MIXT = self.dram("MIXT", [D, T], BF16)
        self.XT, self.HT, self.MIXT = XT, HT, MIXT
        evs = sorted({l // 2 for l in layers if l % 2 == 0})
        ods = sorted({l // 2 for l in layers if l % 2 == 1})
        if do_mixer and evs:
            ev_w_in = {e: self.dram_in(f"ev_w_in{e}", [D, 3584]) for e in evs}
            ev_w_out = {e: self.dram_in(f"ev_w_out{e}", [D, D]) for e in evs}
            ev_ra = self.dram_in("ev_ra_w", [2, 2, 8, 128, 128])
            ev_ix = self.dram_in("ev_ix_w", [2, 2, 8, 128, 128])
            XA = self.dram("XA", [1024, T], F32)
            YG = self.dram("YG", [1024, T], BF16)
            QK = self.dram("QK", [1280, T], BF16)
            VV = self.dram("VV", [T, 256], BF16)
        if do_mixer and ods:
            self.odd_decl(ods)
        if do_moe:
            self.moe_decl(layers)
        if stop >= 1:
            self.phase_mod(ada_w, layers)
        for l in layers:
            if stop < 2:
                break
            if do_mixer:
                with ExitStack() as es:
                    def consume(tb, t0, tn, hb, hf):
                        S.dma("sp", HT.h.rearrange("(kc p) t -> p kc t", p=128)[:, :, t0:t0 + tn], hb[:, :, :tn],
                              reads=[hb], writes=[HT.b(tb)])
                    self.phase_norm(XT, l, 0, f"nmix{l}", consume, es)
                    S.barrier()
                if l % 2 == 0:
                    e = l // 2
                    if stop >= 3:
                        self.even_proj(l, e, ev_w_in[e], HT, XA, YG, QK, VV, self.cos_d, self.sin_d)
                    if stop >= 4:
                        self.even_lru(e, ev_ra, ev_ix, XA, YG, MIXT)
                    if stop >= 5:
                        self.even_attn(e, QK, VV, MIXT)
                    if stop >= 6:
                        self.out_proj_residual(l, 2, MIXT, ev_w_out[e], XT)
                else:
                    self.odd_mixer(l, l // 2)
            if do_moe:
                self.moe_layer(l)
        if do_final:
            self.final_norm(XT, OUT)
        S.barrier()
        self.es.close()
        return self.nc

    def moe_decl(self, layers):
        self.moe_r = {l: self.dram_in(f"moe_r{l}", [D, NE]) for l in layers}
        self.moe_w = {}
        for l in layers:
            for hh in range(2):
                self.moe_w[(l, "g", hh)] = self.dram_in(f"wg{l}_{hh}", [8, D, DEXP])
                self.moe_w[(l, "u", hh)] = self.dram_in(f"wu{l}_{hh}", [8, D, DEXP])
                self.moe_w[(l, "d", hh)] = self.dram_in(f"wd{l}_{hh}", [8, DEXP, D])
        self.Hrow = self.dram("Hrow", [T, D], BF16)
        self.Yacc = self.dram("Yacc", [T, D], F32)

    def moe_layer(self, l):
        S = self.S
        XT, Hrow, Yacc = self.XT, self.Hrow, self.Yacc
        with ExitStack() as eso:
            AFFT = self.sb(eso, "AFFT", [16, T], F32)
            IDXT = self.sb(eso, "IDXT", [128, 5, 16], U32)
            GT = self.sb(eso, "GT", [128, 5, 16], F32)
            zt = self.sb(eso, "zt", [128, 2048], F32)
            S.op("pool", lambda e_: e_.memset(zt[:], 0.0), writes=[zt])
            for i in range(34):
                S.dma("sp", Yacc[i * 128:(i + 1) * 128, :], zt[:], reads=[zt], writes=[Yacc.b(i)])
            with ExitStack() as es:
                WR = self.sb(es, "WR", [128, 16, 16], F32)
                S.dma("sp", WR[:], self.moe_r[l].h.rearrange("(kc p) e -> p kc e", p=128), reads=[self.moe_r[l]], writes=[WR])
                hrow = [self.sb(es, f"hrow{i}", [128, 2048], BF16) for i in range(2)]
                sm = [self.sb(es, f"smx{i}", [128, 4], F32) for i in range(2)]
                ex = [self.sb(es, f"ex{i}", [128, 16], F32) for i in range(2)]
                cnt = [0]

                def consume(tb, t0, tn, hb, hf):
                    for s_ in range(tn // 128):
                        i = cnt[0] % 2
                        cnt[0] += 1
                        tok0 = t0 + s_ * 128
                        ps = self.psum()
                        for kc in range(16):
                            S.op("pe", lambda e_, kc=kc, ps=ps: e_.matmul(
                                ps[:, 0:16], lhsT=hf[:, kc, s_ * 128:(s_ + 1) * 128], rhs=WR[:, kc, :],
                                start=(kc == 0), stop=(kc == 15)), reads=[hf, WR], writes=[ps])
                        m, x_ = sm[i], ex[i]
                        S.op("dve", lambda e_, ps=ps: e_.tensor_reduce(out=m[:, 0:1], in_=ps[:, 0:16], axis=AX.X, op=ALU.max,
                                                                     negate=True), reads=[ps], writes=[m])
                        S.op("act", lambda e_, ps=ps: e_.activation(out=x_[:], in_=ps[:, 0:16], func=AF.Exp, bias=m[:, 0:1],
                                                                    scale=1.0, accum_out=m[:, 1:2]), reads=[ps, m], writes=[x_, m])
                        S.op("dve", lambda e_: e_.reciprocal(out=m[:, 2:3], in_=m[:, 1:2]), reads=[m], writes=[m])
                        S.op("dve", lambda e_: e_.tensor_scalar(out=x_[:], in0=x_[:], scalar1=m[:, 2:3], scalar2=None,
                                                                op0=ALU.mult), reads=[m], writes=[x_])
                        ps2 = self.psum()
                        S.op("pe", lambda e_, ps2=ps2: e_.transpose(out=ps2[0:16, 0:128], in_=x_[:], identity=self.IDF[:]),
                             reads=[x_, self.IDF], writes=[ps2])
                        S.op("act", lambda e_, ps2=ps2: e_.copy(out=AFFT[:, tok0:tok0 + 128], in_=ps2[0:16, 0:128]),
                             reads=[ps2], writes=[AFFT])
                        hr = hrow[i]
                        for kg in range(4):
                            ps3 = self.psum()
                            for kk in range(4):
                                kc = kg * 4 + kk
                                S.op("pe", lambda e_, kc=kc, kk=kk, ps3=ps3: e_.transpose(
                                    out=ps3[:, kk * 128:(kk + 1) * 128], in_=hf[:, kc, s_ * 128:(s_ + 1) * 128],
                                    identity=self.IDF[:]), reads=[hf, self.IDF], writes=[ps3])
                            eng = "act" if kg % 2 == 0 else "dve"
                            if eng == "act":
                                S.op("act", lambda e_, ps3=ps3, kg=kg: e_.copy(out=hr[:, kg * 512:(kg + 1) * 512], in_=ps3[:, 0:512]),
                                     reads=[ps3], writes=[hr])
                            else:
                                S.op("dve", lambda e_, ps3=ps3, kg=kg: e_.tensor_copy(out=hr[:, kg * 512:(kg + 1) * 512], in_=ps3[:, 0:512]),
                                     reads=[ps3], writes=[hr])
                        S.dma("sp", Hrow[tok0:tok0 + 128, :], hr[:], reads=[hr], writes=[Hrow.b(tok0 // 128)])

                self.phase_norm(XT, l, 3, f"nffn{l}", consume, es, want_f32=True)
                S.barrier()
            with ExitStack() as es:
                work = self.sb(es, "work", [16, SEQ], F32)
                VAL = self.sb(es, "VAL", [16, 544], F32)
                IDXu = self.sb(es, "IDXu", [16, 544], U32)
                IDXf = self.sb(es, "IDXf", [16, 544], F32)
                for (c0, n, cap, s0, toff) in ((0, CTX, 32, 512, 0), (CTX, SEQ, 512, 0, CTX)):
                    S.op("act", lambda e_: e_.copy(out=work[:, :n], in_=AFFT[:, c0:c0 + n]), reads=[AFFT], writes=[work])
                    for it in range(cap // 8):
                        sl = slice(s0 + it * 8, s0 + it * 8 + 8)
                        S.op("dve", lambda e_, sl=sl: e_.max(out=VAL[:, sl], in_=work[:, :n]), reads=[work], writes=[VAL])
                        S.op("dve", lambda e_, sl=sl: e_.max_index(out=IDXu[:, sl], in_max=VAL[:, sl], in_values=work[:, :n]),
                             reads=[work, VAL], writes=[IDXu])
                        S.op("dve", lambda e_, sl=sl: e_.match_replace(out=work[:, :n], in_to_replace=VAL[:, sl],
                                                                       in_values=work[:, :n], imm_value=-1.0),
                             reads=[VAL], writes=[work])
                    S.op("dve", lambda e_: e_.tensor_copy(out=IDXf[:, s0:s0 + cap], in_=IDXu[:, s0:s0 + cap]),
                         reads=[IDXu], writes=[IDXf])
                    if toff:
                        S.op("dve", lambda e_: e_.tensor_scalar(out=IDXf[:, s0:s0 + cap], in0=IDXf[:, s0:s0 + cap],
                                                                scalar1=float(toff), scalar2=None, op0=ALU.add),
                             reads=[IDXf], writes=[IDXf])
                S.op("pool", lambda e_: e_.memset(IDXT[:], 0), writes=[IDXT])
                S.op("pool", lambda e_: e_.memset(GT[:], 0.0), writes=[GT])
                for st in range(5):
                    ns = 128 if st < 4 else 32
                    for src, dst in ((IDXf, IDXT), (VAL, GT)):
                        ps = self.psum()
                        S.op("pe", lambda e_, ps=ps, src=src: e_.transpose(
                            out=ps[0:ns, 0:16], in_=src[:, st * 128:st * 128 + ns], identity=self.IDF[0:16, 0:16]),
                            reads=[src, self.IDF], writes=[ps])
                        S.op("dve", lambda e_, ps=ps, dst=dst: e_.tensor_copy(out=dst[0:ns, st, :], in_=ps[0:ns, 0:16]),
                             reads=[ps], writes=[dst])
                S.barrier()
            with ExitStack() as es:
                FG = 256
                NG = DEXP // FG
                wgt = [self.sb(es, f"wgt{i}", [128, 16, FG], BF16) for i in range(2)]
                wut = [self.sb(es, f"wut{i}", [128, 16, FG], BF16) for i in range(2)]
                wdt = [self.sb(es, f"wdt{i}", [128, FG // 128, D], BF16) for i in range(2)]
                xs = [self.sb(es, f"xs{i}", [128, D], BF16) for i in range(2)]
                xsT = self.sb(es, "xsT", [128, 16, 544], BF16)
                hidT = self.sb(es, "hidT", [128, FG // 128, 544], BF16)
                sg = [self.sb(es, f"sg{i}", [128, 512], F32) for i in range(2)]
                yacc = self.sb(es, "yacc", [128, 5, D], F32)
                wi = 0
                xi = 0
                si = 0
                for ex_ in range(NE):
                    Wg = self.moe_w[(l, "g", ex_ // 8)]
                    Wu = self.moe_w[(l, "u", ex_ // 8)]
                    Wd = self.moe_w[(l, "d", ex_ // 8)]
                    el = ex_ % 8
                    for st in range(5):
                        ns = 128 if st < 4 else 32
                        x = xs[xi % 2]
                        xi += 1
                        S.dma("pool", None, None, reads=[Hrow.b(i) for i in range(34)] + [IDXT], writes=[x],
                              fn=lambda e_, x=x, ns=ns, st=st: e_.indirect_dma_start(
                                  out=x[0:ns, :], out_offset=None, in_=Hrow[:, :],
                                  in_offset=bass.IndirectOffsetOnAxis(ap=IDXT[0:ns, st, ex_:ex_ + 1], axis=0)))
                        for kg in range(2):
                            ps = self.psum()
                            psb = ps.h.bitcast(BF16)
                            for kk in range(8):
                                kc = kg * 8 + kk
                                S.op("pe", lambda e_, kc=kc, kk=kk, psb=psb, x=x, ns=ns: e_.transpose(
                                    out=psb[:, kk * 128:kk * 128 + ns], in_=x[0:ns, kc * 128:(kc + 1) * 128],
                                    identity=self.IDB[0:ns, 0:ns]), reads=[x, self.CB], writes=[ps])
                            S.op("act" if kg == 0 else "dve",
                                 (lambda e_, psb=psb, kg=kg, st=st, ns=ns: e_.copy(
                                     out=xsT[:, kg * 8:(kg + 1) * 8, st * 128:st * 128 + ns],
                                     in_=psb[:, 0:1024].rearrange("p (k s) -> p k s", k=8)[:, :, 0:ns])) if kg == 0 else
                                 (lambda e_, psb=psb, kg=kg, st=st, ns=ns: e_.tensor_copy(
                                     out=xsT[:, kg * 8:(kg + 1) * 8, st * 128:st * 128 + ns],
                                     in_=psb[:, 0:1024].rearrange("p (k s) -> p k s", k=8)[:, :, 0:ns])),
                                 reads=[ps], writes=[xsT])
                    for g in range(NG):
                        wg_, wu_, wd_ = wgt[wi % 2], wut[wi % 2], wdt[wi % 2]
                        wi += 1
                        f0 = g * FG
                        S.dma("pool", wg_[:], Wg[el, :, f0:f0 + FG].rearrange("(kc p) f -> p kc f", p=128), reads=[Wg], writes=[wg_])
                        S.dma("pool", wu_[:], Wu[el, :, f0:f0 + FG].rearrange("(kc p) f -> p kc f", p=128), reads=[Wu], writes=[wu_])
                        S.dma("pool", wd_[:], Wd[el, f0:f0 + FG, :].rearrange("(fc p) d -> p fc d", p=128), reads=[Wd], writes=[wd_])
                        for fc in range(FG // 128):
                            for (c0, ns) in ((0, 512), (512, 32)):
                                psg = self.psum()
                                psu = self.psum()
                                for kc in range(16):
                                    S.op("pe", lambda e_, kc=kc, psg=psg, c0=c0, ns=ns, fc=fc, wg_=wg_: e_.matmul(
                                        psg[:, 0:ns], lhsT=wg_[:, kc, fc * 128:(fc + 1) * 128], rhs=xsT[:, kc, c0:c0 + ns],
                                        start=(kc == 0), stop=(kc == 15)), reads=[wg_, xsT], writes=[psg])
                                for kc in range(16):
                                    S.op("pe", lambda e_, kc=kc, psu=psu, c0=c0, ns=ns, fc=fc, wu_=wu_: e_.matmul(
                                        psu[:, 0:ns], lhsT=wu_[:, kc, fc * 128:(fc + 1) * 128], rhs=xsT[:, kc, c0:c0 + ns],
                                        start=(kc == 0), stop=(kc == 15)), reads=[wu_, xsT], writes=[psu])
                                s_ = sg[si % 2]
                                si += 1
                                S.op("act", lambda e_, psg=psg, s_=s_, ns=ns: e_.activation(out=s_[:, 0:ns], in_=psg[:, 0:ns], func=AF.Silu),
                                     reads=[psg], writes=[s_])
                                S.op("dve", lambda e_, psu=psu, s_=s_, ns=ns, c0=c0, fc=fc: e_.tensor_tensor(
                                    out=hidT[:, fc, c0:c0 + ns], in0=psu[:, 0:ns], in1=s_[:, 0:ns], op=ALU.mult),
                                    reads=[psu, s_], writes=[hidT])
                        for st in range(5):
                            ns = 128 if st < 4 else 32
                            for dc in range(4):
                                psd = self.psum()
                                nf = FG // 128
                                for fc in range(nf):
                                    S.op("pe", lambda e_, fc=fc, psd=psd, st=st, ns=ns, dc=dc, wd_=wd_: e_.matmul(
                                        psd[0:ns, 0:512], lhsT=hidT[:, fc, st * 128:st * 128 + ns],
                                        rhs=wd_[:, fc, dc * 512:(dc + 1) * 512], start=(fc == 0), stop=(fc == nf - 1)),
                                        reads=[hidT, wd_], writes=[psd])
                                gsc = GT[0:ns, st, ex_:ex_ + 1]
                                if g == 0:
                                    S.op("act", lambda e_, psd=psd, st=st, ns=ns, dc=dc, gsc=gsc: e_.activation(
                                        out=yacc[0:ns, st, dc * 512:(dc + 1) * 512], in_=psd[0:ns, 0:512], func=AF.Copy, scale=gsc),
                                        reads=[psd, GT], writes=[yacc])
                                else:
                                    S.op("dve", lambda e_, psd=psd, st=st, ns=ns, dc=dc, gsc=gsc: e_.scalar_tensor_tensor(
                                        out=yacc[0:ns, st, dc * 512:(dc + 1) * 512], in0=psd[0:ns, 0:512], scalar=gsc,
                                        in1=yacc[0:ns, st, dc * 512:(dc + 1) * 512], op0=ALU.mult, op1=ALU.add),
                                        reads=[psd, GT], writes=[yacc])
                    for st in range(5):
                        ns = 128 if st < 4 else 32
                        S.dma("pool", None, None, reads=[yacc, IDXT], writes=[Yacc],
                              fn=lambda e_, ns=ns, st=st: e_.indirect_dma_start(
                                  out=Yacc[:, :], out_offset=bass.IndirectOffsetOnAxis(ap=IDXT[0:ns, st, ex_:ex_ + 1], axis=0),
                                  in_=yacc[0:ns, st, :], in_offset=None, compute_op=ALU.add))
                S.barrier()
            with ExitStack() as es:
                yt = [self.sb(es, f"yt{i}", [128, D], F32) for i in range(2)]
                xr = [self.sb(es, f"mxr{i}", [128, 16, 128], F32) for i in range(2)]
                XTv = XT.h.rearrange("(kc p) t -> p kc t", p=128)
                for i in range(34):
                    y = yt[i % 2]
                    x = xr[i % 2]
                    wh = 1 if i < 2 else 0
                    S.dma("sp", y[:], Yacc[i * 128:(i + 1) * 128, :], reads=[Yacc], writes=[y])
                    S.dma("sp", x[:], XTv[:, :, i * 128:(i + 1) * 128], reads=[XT.b(("m5", i))], writes=[x])
                    for kg in range(4):
                        ps = self.psum()
                        for kk in range(4):
                            kc = kg * 4 + kk
                            S.op("pe", lambda e_, kc=kc, kk=kk, ps=ps, y=y: e_.transpose(
                                out=ps[:, kk * 128:(kk + 1) * 128], in_=y[:, kc * 128:(kc + 1) * 128], identity=self.IDF[:]),
                                reads=[y, self.IDF], writes=[ps])
                        for kk in range(4):
                            kc = kg * 4 + kk
                            S.op("dve", lambda e_, kc=kc, kk=kk, ps=ps, x=x: e_.scalar_tensor_tensor(
                                out=x[:, kc, :], in0=ps[:, kk * 128:(kk + 1) * 128], scalar=self.mod(l, 5, kc, wh),
                                in1=x[:, kc, :], op0=ALU.mult, op1=ALU.add), reads=[ps, self.MOD], writes=[x])
                    S.dma("sp", XTv[:, :, i * 128:(i + 1) * 128], x[:], reads=[x], writes=[XT.b(("m5", i))])
                S.barrier()

    def final_norm(self, XT, OUT):
        S = self.S
        with ExitStack() as es:
            xts = [self.sb(es, f"fx{i}", [128, 16, 512], F32) for i in range(2)]
            sq = self.sb(es, "fsq", [128, 16, 512], BF16)
            rs = self.sb(es, "frs", [128, 512], F32)
            ot = [self.sb(es, f"fo{i}", [128, D], F32) for i in range(2)]
            XTv = XT.h.rearrange("(kc p) t -> p kc t", p=128)
            oi = 0
            for tb, (t0, tn) in list(enumerate(TBLOCKS))[1:]:
                xt = xts[tb % 2]
                S.dma("sp", xt[:], XTv[:, :, t0:t0 + tn], reads=[XT], writes=[xt])
                S.op("act", lambda e: e.activation(out=sq[:], in_=xt[:], func=AF.Square), reads=[xt], writes=[sq])
                acc = self.psum()
                for kc in range(16):
                    S.op("pe", lambda e: e.matmul(acc[:, :], lhsT=self.ONESB, rhs=sq[:, kc, :], start=(kc == 0), stop=(kc == 15)),
                         reads=[sq, self.CB], writes=[acc])
                S.op("act", lambda e: e.activation(out=rs[:], in_=acc[:, :], func=AF.Sqrt, bias=self.epsT[:, 0:1], scale=1.0 / D),
                     reads=[acc, self.epsT], writes=[rs])
                S.op("dve", lambda e: e.reciprocal(out=rs[:], in_=rs[:]), reads=[rs], writes=[rs])
                for kc in range(16):
                    S.op("dve", lambda e: e.scalar_tensor_tensor(out=xt[:, kc, :], in0=xt[:, kc, :], scalar=self.vcol("fnorm", kc, 1),
                                                                 in1=rs[:], op0=ALU.mult, op1=ALU.mult), reads=[rs, self.V], writes=[xt])
                for s_ in range(4):
                    o = ot[oi % 2]
                    oi += 1
                    for kg in range(4):
                        ps = self.psum()
                        for kk in range(4):
                            kc = kg * 4 + kk
                            S.op("pe", lambda e: e.transpose(out=ps[:, kk * 128:(kk + 1) * 128], in_=xt[:, kc, s_ * 128:(s_ + 1) * 128],
                                                             identity=self.IDF[:]), reads=[xt, self.IDF], writes=[ps])
                        if kg % 2 == 0:
                            S.op("act", lambda e: e.copy(out=o[:, kg * 512:(kg + 1) * 512], in_=ps[:, 0:512]), reads=[ps], writes=[o])
                        else:
                            S.op("dve", lambda e: e.tensor_copy(out=o[:, kg * 512:(kg + 1) * 512], in_=ps[:, 0:512]), reads=[ps], writes=[o])
                    r0 = t0 - CTX + s_ * 128
                    S.dma("sp", OUT[r0:r0 + 128, :], o[:], reads=[o], writes=[OUT.b(r0)])
            S.barrier()

    def odd_decl(self, ods):
        self.od_w_in = {o: self.dram_in(f"od_w_in{o}", [D, 6176]) for o in ods}
        self.od_w_out = {o: self.dram_in(f"od_w_out{o}", [D, D]) for o in ods}
        self.hnorm_d = {o: self.dram_in(f"hnorm_bc{o}", [128, D]) for o in ods}
        self.cf_d = self.dram_in("cf", [128, 5, 128])
        self.QKP = self.dram("QKP", [D, T], F32)
        self.QS = self.dram("QS", [D, T], BF16)
        self.VM = self.dram("VM", [T, D], BF16)
        self.OS = self.dram("OS", [T, D], BF16)
        self.GG = self.dram("GG", [T, 32], F32)
        self.HS = self.dram("HS", [T, D], F32)

    def odd_mixer(self, l, o, stop=99):
        S = self.S
        HT, MIXT, XT = self.HT, self.MIXT, self.XT
        w_in = self.od_w_in[o]
        QKP, QS, VM, OS, GG, HS = self.QKP, self.QS, self.VM, self.OS, self.GG, self.HS
        with ExitStack() as es:
            st_f = [self.sb(es, f"ostf{i}", [128, 512], F32) for i in range(2)]
            st_b = [self.sb(es, f"ostb{i}", [128, 512], BF16) for i in range(2)]
            cnt = [0]

            def epi_qk(ps, col, tb, t0, tn):
                sf = st_f[cnt[0] % 2]
                cnt[0] += 1
                S.op("act", lambda e: e.copy(out=sf[:, :tn], in_=ps[:, :tn]), reads=[ps], writes=[sf])
                S.dma("sp", QKP[col:col + 128, t0:t0 + tn], sf[:, :tn], reads=[sf], writes=[QKP.b((col, tb))])

            self.linear(es, HT, w_in, w_in[:], D, 0, 2048, "fm", epi_qk)

            def epi_tm(ps, cs, ncol, tb, tok0):
                i = cnt[0] % 2
                cnt[0] += 1
                if cs < 4096:
                    sb_ = st_b[i]
                    S.op("act", lambda e: e.copy(out=sb_[:, :ncol], in_=ps[:, :ncol]), reads=[ps], writes=[sb_])
                    S.dma("sp", VM[tok0:tok0 + 128, cs - 2048:cs - 2048 + ncol], sb_[:, :ncol], reads=[sb_], writes=[VM.b((cs, tok0))])
                elif cs < 6144:
                    sb_ = st_b[i]
                    S.op("act", lambda e: e.activation(out=sb_[:, :ncol], in_=ps[:, :ncol], func=AF.Sigmoid), reads=[ps], writes=[sb_])
                    S.dma("sp", OS[tok0:tok0 + 128, cs - 4096:cs - 4096 + ncol], sb_[:, :ncol], reads=[sb_], writes=[OS.b((cs, tok0))])
                else:
                    sf = st_f[i]
                    S.op("act", lambda e: e.copy(out=sf[:, :ncol], in_=ps[:, :ncol]), reads=[ps], writes=[sf])
                    S.dma("sp", GG[tok0:tok0 + 128, :], sf[:, :ncol], reads=[sf], writes=[GG.b(tok0)])

            self.linear(es, HT, w_in, w_in[:], D, 2048, 6176, "tm", epi_tm)
            S.barrier()
        if stop < 2:
            return
        with ExitStack() as es:
            xp = [self.sb(es, f"oxp{i}", [128, T], F32) for i in range(2)]
            xa = [self.sb(es, f"oxa{i}", [128, T], F32) for i in range(2)]
            xo = [self.sb(es, f"oxo{i}", [128, T], BF16) for i in range(2)]
            segs = [(0, CTX), (CTX, T)]
            for c in range(16):
                p_, a_, o_ = xp[c % 2], xa[c % 2], xo[c % 2]
                S.dma("sp", p_[:], QKP[c * 128:(c + 1) * 128, :], reads=[QKP], writes=[p_])
                cw = lambda j: self.vcol(f"od_cw{o}", j * 16 + c, 1)
                for (a0, a1) in segs:
                    S.op("dve", lambda e: e.tensor_scalar(out=a_[:, a0:a1], in0=p_[:, a0:a1], scalar1=cw(2),
                                                          scalar2=self.vcol(f"od_cb{o}", c, 1), op0=ALU.mult, op1=ALU.add),
                         reads=[p_, self.V], writes=[a_])
                    for j, off in ((0, -2), (1, -1), (3, 1)):
                        lo = max(a0, a0 - off)
                        hi = min(a1, a1 - off)
                        S.op("dve", lambda e: e.scalar_tensor_tensor(out=a_[:, lo:hi], in0=p_[:, lo + off:hi + off], scalar=cw(j),
                                                                     in1=a_[:, lo:hi], op0=ALU.mult, op1=ALU.add),
                             reads=[p_, self.V], writes=[a_])
                if c < 8:
                    S.op("act", lambda e: e.activation(out=a_[:], in_=a_[:], func=AF.Silu), reads=[a_], writes=[a_])
                    S.op("pool", lambda e: e.tensor_scalar(out=o_[:], in0=a_[:], scalar1=128.0 ** -0.5, scalar2=None, op0=ALU.mult),
                         reads=[a_], writes=[o_])
                else:
                    S.op("act", lambda e: e.activation(out=o_[:], in_=a_[:], func=AF.Silu), reads=[a_], writes=[o_])
                S.dma("sp", QS[c * 128:(c + 1) * 128, :], o_[:], reads=[o_], writes=[QS.b(c)])
            S.barrier()
        if stop < 3:
            return
        with ExitStack() as es:
            CF = self.sb(es, "CF", [128, 5, 128], F32)
            S.dma("sp", CF[:], self.cf_d[:], reads=[self.cf_d], writes=[CF])
            NCH = 34
            GGt = self.sb(es, "GGt", [128, NCH, 32], F32)
            S.dma("sp", GGt[:], GG.h.rearrange("(n p) g -> p n g", p=128), reads=[GG], writes=[GGt])
            gbb = self.vcol(f"od_gb{o}", 0, 32)
            for n in range(NCH):
                S.op("pool", lambda e: e.tensor_tensor(out=GGt[:, n, :], in0=GGt[:, n, :], in1=gbb, op=ALU.add),
                     reads=[self.V], writes=[GGt])
            GG4 = GGt[:].rearrange("p n (ty h) -> p n ty h", ty=4)
            LF = self.sb(es, "LF", [128, NCH, 2, 8], F32)
            for d in range(2):
                S.op("act", lambda e: e.activation(out=LF[:, :, d, :], in_=GG4[:, :, 2 * d + 1, :], func=AF.Exp, scale=-1.0),
                     reads=[GGt], writes=[LF])
            S.op("act", lambda e: e.activation(out=LF[:], in_=LF[:], func=AF.Ln, bias=1.0, scale=1.0), reads=[LF], writes=[LF])
            S.op("dve", lambda e: e.tensor_scalar(out=LF[:], in0=LF[:], scalar1=-1.0, scalar2=None, op0=ALU.mult),
                 reads=[LF], writes=[LF])
            Bt = self.sb(es, "Bt", [128, NCH, 2, 8], F32)
            Ct = self.sb(es, "Ct", [128, NCH, 2, 8], F32)
            EBt = self.sb(es, "EBt", [128, NCH, 2, 8], F32)
            for n in range(NCH):
                ps = self.psum()
                for d in range(2):
                    S.op("pe", lambda e: e.matmul(ps[:, d * 8:(d + 1) * 8], lhsT=CF[:, d, :], rhs=LF[:, n, d, :], start=True, stop=True),
                         reads=[CF, LF], writes=[ps])
                S.op("dve", lambda e: e.tensor_copy(out=Bt[:, n].rearrange("p d h -> p (d h)"), in_=ps[:, 0:16]),
                     reads=[ps], writes=[Bt])
            for d in range(2):
                S.op("dve", lambda e: e.tensor_tensor(out=Ct[:, :, d, :], in0=GG4[:, :, 2 * d, :], in1=Bt[:, :, d, :], op=ALU.subtract),
                     reads=[GGt, Bt], writes=[Ct])
            S.op("act", lambda e: e.activation(out=EBt[:], in_=Bt[:], func=AF.Exp), reads=[Bt], writes=[EBt])
            qT = self.sb(es, "qT", [128, T], BF16)
            kT = self.sb(es, "kT", [128, T], BF16)
            Va = self.sb(es, "Va", [128, NCH, 257], BF16)
            Hs = self.sb(es, "Hs", [128, NCH, 256], F32)
            ktm = self.sb(es, "ktm", [128, NCH, 128], BF16)
            Cf = [self.sb(es, f"Cf{d}", [128, 257], F32) for d in range(2)]
            Cb = [self.sb(es, f"Cb{d}", [128, 257], BF16) for d in range(2)]
            LFB = [self.sb(es, f"LFB{i}", [128, 128], F32) for i in range(2)]
            BBm = [self.sb(es, f"BBm{i}", [128, 128], F32) for i in range(2)]
            Dm = [self.sb(es, f"Dm{i}", [128, 128], F32) for i in range(2)]
            SD = [self.sb(es, f"SD{i}", [128, 128], BF16) for i in range(2)]
            ku = [self.sb(es, f"ku{i}", [128, 128], BF16) for i in range(2)]
            tin = [self.sb(es, f"tin{i}", [128, 257], F32) for i in range(2)]
            tot = [self.sb(es, f"tot{i}", [128, 257], F32) for i in range(2)]
            sc = [self.sb(es, f"sc{i}", [128, 8], F32) for i in range(2)]
            order_f = list(range(NCH))
            order_b = [1, 0] + list(range(NCH - 1, 1, -1))
            ui = 0
            for hd in range(8):
                S.dma("sp", qT[:], QS[hd * 128:(hd + 1) * 128, :], reads=[QS], writes=[qT])
                S.dma("sp", kT[:], QS[1024 + hd * 128:1024 + (hd + 1) * 128, :], reads=[QS], writes=[kT])
                S.dma("sp", Va[:, :, 0:256], VM[:, hd * 256:(hd + 1) * 256].rearrange("(n p) d -> p n d", p=128), reads=[VM], writes=[Va])
                S.op("pool", lambda e: e.memset(Va[:, :, 256:257], 1.0), writes=[Va])
                S.op("pool", lambda e: e.memset(Hs[:], 0.0), writes=[Hs])
                for n0 in range(0, NCH, 8):
                    nn = min(8, NCH - n0)
                    ps = self.psum()
                    psb = ps.h.bitcast(BF16)
                    for j in range(nn):
                        S.op("pe", lambda e: e.transpose(out=psb[:, j * 128:(j + 1) * 128], in_=kT[:, (n0 + j) * 128:(n0 + j + 1) * 128],
                                                         identity=self.IDB), reads=[kT, self.CB], writes=[ps])
                    S.op("act", lambda e: e.copy(out=ktm[:, n0:n0 + nn, :].rearrange("p n d -> p (n d)"), in_=psb[:, 0:nn * 128]),
                         reads=[ps], writes=[ktm])
                for d in range(2):
                    S.op("pool", lambda e: e.memset(Cf[d][:], 0.0), writes=[Cf[d]])
                    S.op("pool", lambda e: e.memset(Cb[d][:], 0.0), writes=[Cb[d]])
                for step in range(NCH):
                    for d in range(2):
                        n = order_f[step] if d == 0 else order_b[step]
                        i = ui % 2
                        ui += 1
                        tsl = slice(n * 128, (n + 1) * 128)
                        last = 127 if d == 0 else 0
                        lfb, bbm, dm, sd, ku_, tin_, tot_, sc_ = LFB[i], BBm[i], Dm[i], SD[i], ku[i], tin[i], tot[i], sc[i]
                        S.op("pool", lambda e: e.tensor_scalar(out=lfb[:], in0=CF[:, 4, :], scalar1=LF[:, n, d, hd:hd + 1], scalar2=None,
                                                               op0=ALU.mult), reads=[CF, LF], writes=[lfb])
                        psB = self.psum()
                        S.op("pe", lambda e: e.matmul(psB[:, 0:128], lhsT=lfb[:], rhs=CF[:, d, :], start=True, stop=True),
                             reads=[lfb, CF], writes=[psB])
                        S.op("dve", lambda e: e.tensor_tensor(out=bbm[:], in0=psB[:, 0:128], in1=CF[:, 2 + d, :], op=ALU.add),
                             reads=[psB, CF], writes=[bbm])
                        S.op("act", lambda e: e.copy(out=sc_[:, 0:1], in_=psB[:, last:last + 1]), reads=[psB], writes=[sc_])
                        S.op("act", lambda e: e.activation(out=dm[:], in_=bbm[:], func=AF.Exp, bias=Ct[:, n, d, hd:hd + 1], scale=1.0),
                             reads=[bbm, Ct], writes=[dm])
                        S.op("act", lambda e: e.activation(out=sc_[:, 1:2], in_=Ct[:, n, d, hd:hd + 1], func=AF.Exp, bias=sc_[:, 0:1], scale=1.0),
                             reads=[Ct, sc_], writes=[sc_])
                        S.op("act", lambda e: e.activation(out=sc_[:, 2:3], in_=sc_[:, 0:1], func=AF.Exp), reads=[sc_], writes=[sc_])
                        psS = self.psum()
                        S.op("pe", lambda e: e.matmul(psS[:, 0:128], lhsT=kT[:, tsl], rhs=qT[:, tsl], start=True, stop=True),
                             reads=[kT, qT], writes=[psS])
                        S.op("dve", lambda e: e.tensor_tensor(out=sd[:], in0=psS[:, 0:128], in1=dm[:], op=ALU.mult),
                             reads=[psS, dm], writes=[sd])
                        psI = self.psum()
                        S.op("pe", lambda e: e.matmul(psI[:, 0:257], lhsT=sd[:], rhs=Va[:, n, :], start=True, stop=True),
                             reads=[sd, Va], writes=[psI])
                        psC = self.psum()
                        S.op("pe", lambda e: e.matmul(psC[:, 0:257], lhsT=qT[:, tsl], rhs=Cb[d][:], start=True, stop=True),
                             reads=[qT, Cb[d]], writes=[psC])
                        S.op("act", lambda e: e.activation(out=tin_[:], in_=psC[:, 0:257], func=AF.Copy, scale=EBt[:, n, d, hd:hd + 1]),
                             reads=[psC, EBt], writes=[tin_])
                        S.op("dve", lambda e: e.tensor_tensor(out=tot_[:], in0=psI[:, 0:257], in1=tin_[:], op=ALU.add),
                             reads=[psI, tin_], writes=[tot_])
                        S.op("act", lambda e: e.activation(out=sc_[:, 3:4], in_=tot_[:, 256:257], func=AF.Abs), reads=[tot_], writes=[sc_])
                        S.op("dve", lambda e: e.tensor_scalar(out=sc_[:, 3:4], in0=sc_[:, 3:4], scalar1=1.0, scalar2=None,
                                                              op0=ALU.max), reads=[sc_], writes=[sc_])
                        S.op("dve", lambda e: e.reciprocal(out=sc_[:, 4:5], in_=sc_[:, 3:4]), reads=[sc_], writes=[sc_])
                        S.op("dve", lambda e: e.scalar_tensor_tensor(out=Hs[:, n, :], in0=tot_[:, 0:256], scalar=sc_[:, 4:5], in1=Hs[:, n, :],
                                                                     op0=ALU.mult, op1=ALU.add), reads=[tot_, sc_], writes=[Hs])
                        S.op("pool", lambda e: e.tensor_scalar(out=ku_[:], in0=ktm[:, n, :], scalar1=sc_[:, 1:2], scalar2=None, op0=ALU.mult),
                             reads=[ktm, sc_], writes=[ku_])
                        psU = self.psum()
                        S.op("pe", lambda e: e.matmul(psU[:, 0:257], lhsT=ku_[:], rhs=Va[:, n, :], start=True, stop=True),
                             reads=[ku_, Va], writes=[psU])
                        S.op("dve", lambda e: e.scalar_tensor_tensor(out=Cf[d][:], in0=Cf[d][:], scalar=sc_[:, 2:3], in1=psU[:, 0:257],
                                                                     op0=ALU.mult, op1=ALU.add), reads=[psU, sc_], writes=[Cf[d]])
                        S.op("act", lambda e: e.copy(out=Cb[d][:], in_=Cf[d][:]), reads=[Cf[d]], writes=[Cb[d]])
                S.dma("sp", HS[:, hd * 256:(hd + 1) * 256].rearrange("(n p) d -> p n d", p=128), Hs[:], reads=[Hs], writes=[HS.b(hd)])
            S.barrier()
        if stop < 4:
            return
        with ExitStack() as es:
            hn = self.sb(es, "hn", [128, D], F32)
            S.dma("sp", hn[:], self.hnorm_d[o][:], reads=[self.hnorm_d[o]], writes=[hn])
            hsT = [self.sb(es, f"hsT{i}", [128, D], F32) for i in range(2)]
            osT = [self.sb(es, f"osT{i}", [128, D], BF16) for i in range(2)]
            w2 = [self.sb(es, f"w2{i}", [128, D], F32) for i in range(2)]
            hb_ = [self.sb(es, f"hbo{i}", [128, D], BF16) for i in range(2)]
            junk = self.sb(es, "junk", [128, 256], F32)
            ss = [self.sb(es, f"oss{i}", [128, 8], F32) for i in range(2)]
            mo = [self.sb(es, f"mo{i}", [128, 16, 128], BF16) for i in range(2)]
            MIXv = MIXT.h.rearrange("(kc p) t -> p kc t", p=128)
            for i in range(34):
                h_, o_, w_, hb2, s_, m_ = hsT[i % 2], osT[i % 2], w2[i % 2], hb_[i % 2], ss[i % 2], mo[i % 2]
                S.dma("sp", h_[:], HS[i * 128:(i + 1) * 128, :], reads=[HS], writes=[h_])
                S.dma("sp", o_[:], OS[i * 128:(i + 1) * 128, :], reads=[OS], writes=[o_])
                S.op("pool", lambda e: e.tensor_tensor(out=w_[:], in0=o_[:], in1=hn[:], op=ALU.mult), reads=[o_, hn], writes=[w_])
                for hd in range(8):
                    S.op("act", lambda e: e.activation(out=junk[:], in_=h_[:, hd * 256:(hd + 1) * 256], func=AF.Square,
                                                       accum_out=s_[:, hd:hd + 1]), reads=[h_], writes=[junk, s_])
                S.op("act", lambda e: e.activation(out=s_[:], in_=s_[:], func=AF.Sqrt, bias=self.epsT[:, 0:1], scale=1.0 / 256),
                     reads=[s_, self.epsT], writes=[s_])
                S.op("dve", lambda e: e.reciprocal(out=s_[:], in_=s_[:]), reads=[s_], writes=[s_])
                for hd in range(8):
                    S.op("dve", lambda e: e.scalar_tensor_tensor(out=hb2[:, hd * 256:(hd + 1) * 256], in0=h_[:, hd * 256:(hd + 1) * 256],
                                                                 scalar=s_[:, hd:hd + 1], in1=w_[:, hd * 256:(hd + 1) * 256],
                                                                 op0=ALU.mult, op1=ALU.mult), reads=[h_, s_, w_], writes=[hb2])
                for kg in range(2):
                    ps = self.psum()
                    psb = ps.h.bitcast(BF16)
                    for kk in range(8):
                        kc = kg * 8 + kk
                        S.op("pe", lambda e: e.transpose(out=psb[:, kk * 128:(kk + 1) * 128], in_=hb2[:, kc * 128:(kc + 1) * 128],
                                                         identity=self.IDB), reads=[hb2, self.CB], writes=[ps])
                    if kg == 0:
                        S.op("act", lambda e: e.copy(out=m_[:, 0:8, :].rearrange("p k t -> p (k t)"), in_=psb[:, 0:1024]), reads=[ps], writes=[m_])
                    else:
                        S.op("dve", lambda e: e.tensor_copy(out=m_[:, 8:16, :].rearrange("p k t -> p (k t)"), in_=psb[:, 0:1024]), reads=[ps], writes=[m_])
                S.dma("sp", MIXv[:, :, i * 128:(i + 1) * 128], m_[:], reads=[m_], writes=[MIXT.b(("o4", i))])
            S.barrier()
        if stop < 5:
            return
        self.out_proj_residual(l, 2, MIXT, self.od_w_out[o], XT)


N_CORES = 4


def kernel(**inp):
    inp = {k: np.asarray(v) for k, v in inp.items()}
    P = Prog()
    nc = P.build()
    cs = make_consts()
    shared = {"cb": cs["cb"], "ident_f": cs["ident_f"], "cosT": cs["cosT"], "sinT": cs["sinT"], "cf": cs["cf"],
              "ev_ra_w": np.ascontiguousarray(inp["ev_ra_w"], dtype=np.float32),
              "ev_ix_w": np.ascontiguousarray(inp["ev_ix_w"], dtype=np.float32)}
    for l in range(DEPTH):
        shared[f"ada_w{l}"] = np.ascontiguousarray(inp["ada_w"][l], dtype=np.float32)
        shared[f"moe_r{l}"] = np.ascontiguousarray(inp["moe_router"][l], dtype=np.float32)
        for hh in range(2):
            shared[f"wg{l}_{hh}"] = np.ascontiguousarray(inp["moe_w_gate"][l, hh * 8:(hh + 1) * 8], dtype=np.float32)
            shared[f"wu{l}_{hh}"] = np.ascontiguousarray(inp["moe_w_up"][l, hh * 8:(hh + 1) * 8], dtype=np.float32)
            shared[f"wd{l}_{hh}"] = np.ascontiguousarray(inp["moe_w_down"][l, hh * 8:(hh + 1) * 8], dtype=np.float32)
    for e in range(2):
        shared[f"ev_w_in{e}"] = np.ascontiguousarray(inp["ev_w_in"][e], dtype=np.float32)
        shared[f"ev_w_out{e}"] = np.ascontiguousarray(inp["ev_w_out"][e], dtype=np.float32)
        shared[f"od_w_in{e}"] = np.ascontiguousarray(inp["od_w_in"][e], dtype=np.float32)
        shared[f"od_w_out{e}"] = np.ascontiguousarray(inp["od_w_out"][e], dtype=np.float32)
        shared[f"hnorm_bc{e}"] = np.ascontiguousarray(
            np.broadcast_to(np.asarray(inp["od_hnorm_w"][e], np.float32)[None, :], (128, D)))
    in_maps = []
    for b in range(N_CORES):
        m = dict(shared)
        m["xT"] = np.ascontiguousarray(np.concatenate([inp["ctx"][b], inp["x"][b]], axis=0).T.astype(np.float32))
        m["vecs"] = pack_vecs(inp, b)
        in_maps.append(m)
    res = run_bass_kernel_spmd(nc, in_maps, core_ids=list(range(N_CORES)))
    return np.stack([np.asarray(res.results[b]["out"], dtype=np.float32) for b in range(N_CORES)], axis=0)
```

```python
import numpy as np
import concourse.bass as bass
import concourse.mybir as mybir
from concourse.bass_utils import run_bass_kernel_spmd
from contextlib import ExitStack

F32 = mybir.dt.float32
BF16 = mybir.dt.bfloat16
U32 = mybir.dt.uint32
I32 = mybir.dt.int32
ALU = mybir.AluOpType
AF = mybir.ActivationFunctionType
AX = mybir.AxisListType

D = 2048
KC = 16
CTX = 256
SEQ = 4096
T = CTX + SEQ
DEPTH = 4
NE = 16
DEXP = 1536
EPS = 1e-6
NQ = 6
import os
DBG = set(os.environ.get('K_DBG', '').split(','))
SAME_SYNC = os.environ.get("K_SAME", "1") == "1"


class Buf:
    __slots__ = ("w", "r", "excl")

    def __init__(self):
        self.w = {}
        self.r = {}
        self.excl = False


class TT:
    def __init__(self, h):
        self.h = h
        self.buf = Buf()
        self.sub = {}

    def __getitem__(self, idx):
        return self.h[idx]

    def b(self, key=None):
        if key is None:
            return self.buf
        s = self.sub.get(key)
        if s is None:
            s = self.sub[key] = Buf()
        return s


def _bufs(lst):
    out = []
    for x in lst:
        if isinstance(x, TT):
            out.append(x.buf)
        elif isinstance(x, Buf):
            out.append(x)
        elif isinstance(x, (list, tuple)):
            out.extend(_bufs(x))
        else:
            raise TypeError(type(x))
    return out


class Sched:
    def __init__(self, nc, es):
        self.nc = nc
        self.es = es
        self.engs = {"pe": nc.tensor, "act": nc.scalar, "dve": nc.vector, "pool": nc.gpsimd, "sp": nc.sync}
        self.semh = {}
        self.ecnt = {}
        for k in ("pe", "act", "dve", "pool"):
            self.semh["E_" + k] = es.enter_context(nc.semaphore("sem_" + k))
            self.ecnt[k] = 0
        self.rings = {}
        for q in ("sp", "act", "pool"):
            self.rings[q] = {"n": 0, "val": [0] * NQ}
            for i in range(NQ):
                self.semh[f"D_{q}_{i}"] = es.enter_context(nc.semaphore(f"dq_{q}_{i}"))
        self.waited = {k: {} for k in self.engs}
        self.n_ops = 0

    def _deps(self, reads, writes):
        deps = {}
        for b in reads:
            for k, v in b.w.items():
                if deps.get(k, 0) < v:
                    deps[k] = v
        for b in writes:
            for k, v in b.w.items():
                if deps.get(k, 0) < v:
                    deps[k] = v
            for k, v in b.r.items():
                if deps.get(k, 0) < v:
                    deps[k] = v
        return deps

    def _wait(self, engname, deps, skip=None):
        e = self.engs[engname]
        wd = self.waited[engname]
        for k, v in deps.items():
            if k == skip:
                continue
            if wd.get(k, 0) < v:
                e.wait_ge(self.semh[k], v)
                wd[k] = v

    def _mark(self, key, v, reads, writes):
        for b in writes:
            b.w = {key: v}
            b.r = {}
        for b in reads:
            if b.r.get(key, 0) < v:
                b.r[key] = v

    def op(self, engname, fn, reads=(), writes=()):
        reads = _bufs(reads)
        writes = _bufs(writes)
        for b in reads:
            if b.excl and b not in writes:
                writes.append(b)
        deps = self._deps(reads, writes)
        key = "E_" + engname
        skip = key if (engname == "pe" or not SAME_SYNC) else None
        self._wait(engname, deps, skip)
        ins = fn(self.engs[engname])
        self.ecnt[engname] += 1
        v = self.ecnt[engname]
        ins.then_inc(self.semh[key], 1)
        self._mark(key, v, [b for b in reads if b not in writes], writes)
        self.n_ops += 1
        return ins

    def dma(self, q, out, in_, reads=(), writes=(), fn=None, **kw):
        reads = _bufs(reads)
        writes = _bufs(writes)
        ring = self.rings[q]
        i = ring["n"] % NQ
        ring["n"] += 1
        key = f"D_{q}_{i}"
        deps = self._deps(reads, writes)
        prev = ring["val"][i]
        if prev and deps.get(key, 0) < prev:
            deps[key] = prev
        self._wait(q, deps)
        e = self.engs[q]
        if fn is not None:
            ins = fn(e)
        else:
            ins = e.dma_start(out=out, in_=in_, **kw)
        ins.then_inc(self.semh[key], 16)
        v = prev + 16
        ring["val"][i] = v
        self._mark(key, v, [b for b in reads if b not in writes], writes)
        self.n_ops += 1
        return ins

    def barrier(self):
        deps = {}
        for k in ("pe", "act", "dve", "pool"):
            if self.ecnt[k]:
                deps["E_" + k] = self.ecnt[k]
        for q, ring in self.rings.items():
            for i in range(NQ):
                if ring["val"][i]:
                    deps[f"D_{q}_{i}"] = ring["val"][i]
        for e in self.engs:
            self._wait(e, deps)


def _vec_layout():
    off = {}
    n = 0

    def add(name, cols):
        nonlocal n
        off[name] = n
        n += cols

    add("c", 16)
    add("cctx", 16)
    for l in range(DEPTH):
        add(f"ada_b{l}", 96)
        add(f"nmix{l}", 16)
        add(f"nffn{l}", 16)
    add("fnorm", 16)
    for e in range(2):
        add(f"ev_cw{e}", 32)
        add(f"ev_cb{e}", 8)
        add(f"ev_rab{e}", 16)
        add(f"ev_ixb{e}", 16)
        add(f"ev_lam{e}", 16)
        add(f"ev_sink{e}", 8)
    for o in range(2):
        add(f"od_cw{o}", 64)
        add(f"od_cb{o}", 16)
        add(f"od_gb{o}", 32)
    return off, n


VOFF, NV = _vec_layout()


def _pm(v):
    v = np.asarray(v, np.float32).reshape(-1, 128)
    return np.ascontiguousarray(v.T)


def pack_vecs(inp, b):
    V = np.zeros((128, NV), np.float32)

    def put(name, arr):
        arr = np.asarray(arr, np.float32)
        V[: arr.shape[0], VOFF[name]: VOFF[name] + arr.shape[1]] = arr

    put("c", _pm(inp["c"][b]))
    put("cctx", _pm(inp["c_ctx"]))
    for l in range(DEPTH):
        put(f"ada_b{l}", _pm(inp["ada_b"][l]))
        put(f"nmix{l}", _pm(inp["norm_mix_w"][l]))
        put(f"nffn{l}", _pm(inp["norm_ffn_w"][l]))
    put("fnorm", _pm(inp["final_norm_w"]))
    for e in range(2):
        put(f"ev_cw{e}", np.concatenate([_pm(inp["ev_conv_w"][e, j]) for j in range(4)], axis=1))
        put(f"ev_cb{e}", _pm(inp["ev_conv_b"][e]))
        put(f"ev_rab{e}", np.concatenate([_pm(inp["ev_ra_b"][e, d].reshape(-1)) for d in range(2)], axis=1))
        put(f"ev_ixb{e}", np.concatenate([_pm(inp["ev_ix_b"][e, d].reshape(-1)) for d in range(2)], axis=1))
        put(f"ev_lam{e}", np.concatenate([_pm(inp["ev_lambda"][e, d]) for d in range(2)], axis=1))
        put(f"ev_sink{e}", np.broadcast_to(np.asarray(inp["ev_sink"][e], np.float32)[None, :], (128, 8)))
    for o in range(2):
        put(f"od_cw{o}", np.concatenate([_pm(inp["od_conv_w"][o, j]) for j in range(4)], axis=1))
        put(f"od_cb{o}", _pm(inp["od_conv_b"][o]))
        put(f"od_gb{o}", np.broadcast_to(np.asarray(inp["od_gate_b"][o], np.float32).reshape(1, 32), (128, 32)))
    return V


def make_consts():
    import ml_dtypes
    bf = ml_dtypes.bfloat16
    c = {}
    c["ident_f"] = np.eye(128, dtype=np.float32)
    cb = np.zeros((128, 6, 128), np.float32)
    cb[:, 0, :] = np.eye(128)
    cb[:, 1, :] = 1.0
    j = np.arange(128)[:, None]
    i = np.arange(128)[None, :]
    cb[:, 2, :] = (j >= i)
    cb[:, 3, :] = (j <= i)
    R = np.zeros((128, 128), np.float32)
    for d in range(128):
        p = d + 32 if (d % 64) < 32 else d - 32
        R[p, d] = 1.0
    cb[:, 4, :] = R
    c["cb"] = cb.astype(bf)
    pairs = 32
    inv = np.power(10000.0, -np.arange(pairs, dtype=np.float32) / pairs).astype(np.float32)
    t = np.arange(SEQ)
    row = (t // 64).astype(np.float32)
    col = (t % 64).astype(np.float32)
    ra = (row[:, None] * inv).astype(np.float32)
    ca = (col[:, None] * inv).astype(np.float32)
    cosT = np.zeros((128, SEQ), np.float32)
    sinT = np.zeros((128, SEQ), np.float32)
    cosT[0:32] = np.cos(ra).T
    cosT[32:64] = np.cos(ra).T
    cosT[64:96] = np.cos(ca).T
    cosT[96:128] = np.cos(ca).T
    sinT[0:32] = -np.sin(ra).T
    sinT[32:64] = np.sin(ra).T
    sinT[64:96] = -np.sin(ca).T
    sinT[96:128] = np.sin(ca).T
    c["cosT"] = cosT
    c["sinT"] = sinT
    cf = np.zeros((128, 5, 128), np.float32)
    cf[:, 0, :] = (j <= i)
    cf[:, 1, :] = (j >= i)
    cf[:, 2, :] = np.where(j <= i, 0.0, -30000.0)
    cf[:, 3, :] = np.where(j >= i, 0.0, -30000.0)
    cf[:, 4, :] = 1.0
    c["cf"] = cf
    return c


TBLOCKS = [(0, 256)] + [(256 + 512 * i, 512) for i in range(8)]


class Prog:
    def __init__(self, dbg_in=(), dbg_out=()):
        self.nc = bass.Bass("TRN2", target_bir_lowering=False)
        self.es = ExitStack()
        self.S = Sched(self.nc, self.es)
        self.dbg_in = set(dbg_in)
        self.dbg_out = set(dbg_out)
        self.ext = {}
        nc = self.nc
        self.ps = [TT(self.es.enter_context(nc.psum_tensor(f"ps{i}", [128, 512], F32))) for i in range(8)]
        for p_ in self.ps:
            p_.buf.excl = True
        self.psi = 0
        self.uid = 0

    def dram_in(self, name, shape, dtype=F32):
        t = TT(self.nc.dram_tensor(name, list(shape), dtype, kind="ExternalInput").ap())
        self.ext[name] = t
        return t

    def dram_out(self, name, shape, dtype=F32):
        t = TT(self.nc.dram_tensor(name, list(shape), dtype, kind="ExternalOutput").ap())
        self.ext[name] = t
        return t

    def dram(self, name, shape, dtype):
        kind = "ExternalInput" if name in self.dbg_in else ("ExternalOutput" if name in self.dbg_out else "Internal")
        t = TT(self.nc.dram_tensor(name, list(shape), dtype, kind=kind).ap())
        self.ext[name] = t
        return t

    def sb(self, es, name, shape, dtype):
        self.uid += 1
        return TT(es.enter_context(self.nc.sbuf_tensor(f"{name}_{self.uid}", list(shape), dtype)))

    def psum(self):
        p = self.ps[self.psi % 8]
        self.psi += 1
        return p

    def setup(self):
        S = self.S
        es = self.es
        self.vecs_d = self.dram_in("vecs", [128, NV])
        self.cb_d = self.dram_in("cb", [128, 6, 128], BF16)
        self.identf_d = self.dram_in("ident_f", [128, 128])
        self.V = self.sb(es, "V", [128, NV], F32)
        self.CB = self.sb(es, "CB", [128, 6, 128], BF16)
        self.IDF = self.sb(es, "IDF", [128, 128], F32)
        self.MOD = self.sb(es, "MOD", [128, DEPTH, 96, 2], F32)
        self.epsT = self.sb(es, "epsT", [128, 1], F32)
        S.dma("sp", self.V[:], self.vecs_d[:], reads=[self.vecs_d], writes=[self.V])
        S.dma("sp", self.CB[:], self.cb_d[:], reads=[self.cb_d], writes=[self.CB])
        S.dma("sp", self.IDF[:], self.identf_d[:], reads=[self.identf_d], writes=[self.IDF])
        S.op("dve", lambda e: e.memset(self.epsT[:], EPS), writes=[self.epsT])
        self.IDB = self.CB[:, 0, :]
        self.ONESB = self.CB[:, 1, :]

    def vcol(self, name, c0=0, n=1):
        o = VOFF[name] + c0
        return self.V[:, o:o + n]

    def phase_mod(self, ada_w, layers):
        S = self.S
        with ExitStack() as es:
            s2 = self.sb(es, "s2", [128, 16, 2], F32)
            S.op("act", lambda e: e.activation(out=s2[:, :, 0], in_=self.vcol("c", 0, 16), func=AF.Silu),
                 reads=[self.V], writes=[s2])
            S.op("act", lambda e: e.activation(out=s2[:, :, 1], in_=self.vcol("cctx", 0, 16), func=AF.Silu),
                 reads=[self.V], writes=[s2])
            wt = [self.sb(es, f"adaw{i}", [128, 16, 1024], F32) for i in range(2)]
            it = 0
            for l in layers:
                W = ada_w[l]
                acc = self.psum()
                for g in range(12):
                    w = wt[it % 2]
                    it += 1
                    for kc in range(16):
                        S.dma("sp", w[:, kc, :], W[kc * 128:(kc + 1) * 128, g * 1024:(g + 1) * 1024],
                              reads=[W], writes=[w])
                    for sub in range(8):
                        col = (g * 8 + sub) * 2
                        for kc in range(16):
                            S.op("pe", lambda e, kc=kc, sub=sub, col=col, w=w: e.matmul(
                                acc[:, col:col + 2], lhsT=w[:, kc, sub * 128:(sub + 1) * 128], rhs=s2[:, kc, :],
                                start=(kc == 0), stop=(kc == 15)), reads=[w, s2], writes=[acc])
                accv = acc[:, 0:192].rearrange("p (n w) -> p n w", w=2)
                for wh in range(2):
                    S.op("dve", lambda e, wh=wh, l=l, accv=accv: e.tensor_tensor(
                        out=self.MOD[:, l, :, wh], in0=accv[:, :, wh], in1=self.vcol(f"ada_b{l}", 0, 96), op=ALU.add),
                        reads=[acc, self.V], writes=[self.MOD])
            S.barrier()

    def mod(self, l, j, kc, wh):
        return self.MOD[:, l, j * 16 + kc, wh:wh + 1]

    def phase_norm(self, XT, l, jshift, nwname, consume, es, want_f32=False):
        S = self.S
        A = self.sb(es, "A", [128, 16, 2], F32)
        for wh in range(2):
            S.op("dve", lambda e, wh=wh: e.scalar_tensor_tensor(
                out=A[:, :, wh], in0=self.MOD[:, l, (jshift + 1) * 16:(jshift + 2) * 16, wh], scalar=1.0,
                in1=self.vcol(nwname, 0, 16), op0=ALU.add, op1=ALU.mult), reads=[self.MOD, self.V], writes=[A])
        xts = [self.sb(es, f"xt{i}", [128, 16, 512], F32) for i in range(2)]
        sqs = [self.sb(es, f"sq{i}", [128, 16, 512], BF16) for i in range(1)]
        rstd = [self.sb(es, f"rstd{i}", [128, 512], F32) for i in range(2)]
        hbs = [self.sb(es, f"hb{i}", [128, 16, 512], BF16) for i in range(2)]
        XTv = XT.h.rearrange("(kc p) t -> p kc t", p=128)
        for tb, (t0, tn) in enumerate(TBLOCKS):
            wh = 1 if tb == 0 else 0
            xt = xts[tb % 2]
            sq = sqs[0]
            rs = rstd[tb % 2]
            hb = hbs[tb % 2]
            xk = [xt.b(kc) for kc in range(16)]
            hk = [hb.b(kc) for kc in range(16)]
            for whole, parts in ((xt, xk), (hb, hk)):
                for pb in parts:
                    for k_, v_ in whole.buf.w.items():
                        if pb.w.get(k_, 0) < v_:
                            pb.w[k_] = v_
                    for k_, v_ in whole.buf.r.items():
                        if pb.r.get(k_, 0) < v_:
                            pb.r[k_] = v_
            S.dma("sp", xt[:, :, :tn], XTv[:, :, t0:t0 + tn], reads=[XT.b((kc, tb)) for kc in range(16)], writes=[xt] + xk)
            S.op("act", lambda e: e.activation(out=sq[:, :, :tn], in_=xt[:, :, :tn], func=AF.Square),
                 reads=[xt] + xk, writes=[sq])
            acc = self.psum()
            for kc in range(16):
                S.op("pe", lambda e, kc=kc: e.matmul(acc[:, :tn], lhsT=self.ONESB, rhs=sq[:, kc, :tn],
                                                      start=(kc == 0), stop=(kc == 15)),
                     reads=[sq, self.CB], writes=[acc])
            S.op("act", lambda e: e.activation(out=rs[:, :tn], in_=acc[:, :tn], func=AF.Sqrt,
                                               bias=self.epsT[:, 0:1], scale=1.0 / D),
                 reads=[acc, self.epsT], writes=[rs])
            S.op("dve", lambda e: e.reciprocal(out=rs[:, :tn], in_=rs[:, :tn]), reads=[rs], writes=[rs])
            hf = xt if want_f32 else None
            for kc in range(16):
                S.op("dve", lambda e, kc=kc: e.scalar_tensor_tensor(
                    out=xt[:, kc, :tn], in0=xt[:, kc, :tn], scalar=A[:, kc, wh:wh + 1], in1=rs[:, :tn],
                    op0=ALU.mult, op1=ALU.mult), reads=[A, rs], writes=[xk[kc]])
            for kc in range(16):
                S.op("act", lambda e, kc=kc: e.activation(
                    out=hb[:, kc, :tn], in_=xt[:, kc, :tn], func=AF.Identity,
                    bias=self.mod(l, jshift, kc, wh), scale=1.0), reads=[xk[kc], self.MOD], writes=[hk[kc]])
                if want_f32:
                    S.op("act", lambda e, kc=kc: e.activation(
                        out=xt[:, kc, :tn], in_=xt[:, kc, :tn], func=AF.Identity,
                        bias=self.mod(l, jshift, kc, wh), scale=1.0), reads=[self.MOD], writes=[xk[kc]])
            for whole, parts in ((xt, xk), (hb, hk)):
                for pb in parts:
                    for k_, v_ in pb.w.items():
                        if whole.buf.w.get(k_, 0) < v_:
                            whole.buf.w[k_] = v_
                    for k_, v_ in pb.r.items():
                        if whole.buf.r.get(k_, 0) < v_:
                            whole.buf.r[k_] = v_
            consume(tb, t0, tn, hb, hf)

    def linear(self, es, AT, W, Wap, K, c0, c1, mode, epilogue, tblocks=None, SW=1024):
        S = self.S
        KCn = K // 128
        es = ExitStack()
        wts = [self.sb(es, f"lw{i}", [128, KCn, SW], BF16) for i in range(2)]
        ats = [self.sb(es, f"la{i}", [128, KCn, 512], BF16) for i in range(2)]
        ATv = AT.h.rearrange("(kc p) t -> p kc t", p=128)
        Wv = Wap.rearrange("(kc p) n -> p kc n", p=128)
        tbl = list(enumerate(TBLOCKS)) if tblocks is None else tblocks
        wi = 0
        ai = 0
        for cs in range(c0, c1, SW):
            ncol = min(SW, c1 - cs)
            wt = wts[wi % 2]
            wi += 1
            for kc in range(KCn):
                for h0 in range(0, ncol, 512):
                    hn = min(512, ncol - h0)
                    S.dma("pool", wt[:, kc, h0:h0 + hn], Wv[:, kc, cs + h0:cs + h0 + hn], reads=[W], writes=[wt])
            for tb, (t0, tn) in tbl:
                at = ats[ai % 2]
                ai += 1
                S.dma("sp", at[:, :, :tn], ATv[:, :, t0:t0 + tn], reads=[AT.b(tb)], writes=[at])
                if mode == "fm":
                    for sub in range(ncol // 128):
                        ps = self.psum()
                        for kc in range(KCn):
                            S.op("pe", lambda e, kc=kc, sub=sub, ps=ps, wt=wt, at=at, tn=tn: e.matmul(
                                ps[:, :tn], lhsT=wt[:, kc, sub * 128:(sub + 1) * 128], rhs=at[:, kc, :tn],
                                start=(kc == 0), stop=(kc == KCn - 1)), reads=[wt, at], writes=[ps])
                        epilogue(ps, cs + sub * 128, tb, t0, tn)
                else:
                    for s_ in range(tn // 128):
                        for h0 in range(0, ncol, 512):
                            hn = min(512, ncol - h0)
                            ps = self.psum()
                            for kc in range(KCn):
                                S.op("pe", lambda e, kc=kc, s_=s_, ps=ps, wt=wt, at=at, hn=hn, h0=h0: e.matmul(
                                    ps[:, :hn], lhsT=at[:, kc, s_ * 128:(s_ + 1) * 128], rhs=wt[:, kc, h0:h0 + hn],
                                    start=(kc == 0), stop=(kc == KCn - 1)), reads=[wt, at], writes=[ps])
                            epilogue(ps, cs + h0, hn, tb, t0 + s_ * 128)
        S.barrier()
        es.close()

    def even_proj(self, l, e, w_in, HT, XA, YG, QK, VV, cos_d, sin_d):
        S = self.S
        with ExitStack() as es:
            cosS = self.sb(es, "cosS", [128, SEQ], F32)
            sinS = self.sb(es, "sinS", [128, SEQ], F32)
            S.dma("sp", cosS[:], cos_d[:], reads=[cos_d], writes=[cosS])
            S.dma("sp", sinS[:], sin_d[:], reads=[sin_d], writes=[sinS])
            st_f = [self.sb(es, f"stf{i}", [128, 512], F32) for i in range(2)]
            st_g = [self.sb(es, f"stg{i}", [128, 512], F32) for i in range(2)]
            st_b = [self.sb(es, f"stb{i}", [128, 512], BF16) for i in range(2)]
            st_o = [self.sb(es, f"sto{i}", [128, 512], BF16) for i in range(2)]
            cnt = [0]
            R = self.CB[:, 4, :]

            def epi(ps, col, tb, t0, tn):
                i = cnt[0] % 2
                cnt[0] += 1
                sf, sg, sbb, so = st_f[i], st_g[i], st_b[i], st_o[i]
                if 'noepi' in DBG:
                    S.op("act", lambda e_: e_.copy(out=sf[:, :tn], in_=ps[:, :tn]), reads=[ps], writes=[sf])
                    return
                if ('onlyxa' in DBG and col >= 1024) or ('onlyya' in DBG and not (1024 <= col < 2048)) or ('onlyqk' in DBG and col < 2048):
                    S.op("act", lambda e_: e_.copy(out=sf[:, :tn], in_=ps[:, :tn]), reads=[ps], writes=[sf])
                    return
                if col < 1024:
                    S.op("act", lambda e_: e_.copy(out=sf[:, :tn], in_=ps[:, :tn]), reads=[ps], writes=[sf])
                    S.dma("pool", XA[col:col + 128, t0:t0 + tn], sf[:, :tn], reads=[sf], writes=[XA.b((col // 128, tb))])
                elif col < 2048:
                    c = col - 1024
                    S.op("act", lambda e_: e_.activation(out=sf[:, :tn], in_=ps[:, :tn], func=AF.Square),
                         reads=[ps], writes=[sf])
                    S.op("dve", lambda e_: e_.tensor_scalar(out=sf[:, :tn], in0=sf[:, :tn], scalar1=0.044715,
                                                            scalar2=1.0, op0=ALU.mult, op1=ALU.add),
                         reads=[sf], writes=[sf])
                    S.op("dve", lambda e_: e_.tensor_tensor(out=sf[:, :tn], in0=ps[:, :tn], in1=sf[:, :tn], op=ALU.mult),
                         reads=[ps, sf], writes=[sf])
                    S.op("act", lambda e_: e_.activation(out=sg[:, :tn], in_=sf[:, :tn], func=AF.Sigmoid,
                                                         scale=1.5957691216057308), reads=[sf], writes=[sg])
                    S.op("dve", lambda e_: e_.tensor_tensor(out=so[:, :tn], in0=ps[:, :tn], in1=sg[:, :tn], op=ALU.mult),
                         reads=[ps, sg], writes=[so])
                    S.dma("pool", YG[c:c + 128, t0:t0 + tn], so[:, :tn], reads=[so], writes=[YG.b((c // 128, tb))])
                else:
                    c = col - 2048
                    if tb == 0:
                        S.op("act", lambda e_: e_.copy(out=so[:, :tn], in_=ps[:, :tn]), reads=[ps], writes=[so])
                    else:
                        p0 = t0 - CTX
                        S.op("act", lambda e_: e_.copy(out=sbb[:, :tn], in_=ps[:, :tn]), reads=[ps], writes=[sbb])
                        ps2 = self.psum()
                        S.op("pe", lambda e_: e_.matmul(ps2[:, :tn], lhsT=R, rhs=sbb[:, :tn], start=True, stop=True),
                             reads=[sbb, self.CB], writes=[ps2])
                        S.op("dve", lambda e_: e_.tensor_tensor(out=sf[:, :tn], in0=ps[:, :tn], in1=cosS[:, p0:p0 + tn],
                                                                op=ALU.mult), reads=[ps, cosS], writes=[sf])
                        S.op("dve", lambda e_: e_.tensor_tensor(out=sg[:, :tn], in0=ps2[:, :tn], in1=sinS[:, p0:p0 + tn],
                                                                op=ALU.mult), reads=[ps2, sinS], writes=[sg])
                        S.op("pool", lambda e_: e_.tensor_tensor(out=so[:, :tn], in0=sf[:, :tn], in1=sg[:, :tn], op=ALU.add),
                             reads=[sf, sg], writes=[so])
                    S.dma("pool", QK[c:c + 128, t0:t0 + tn], so[:, :tn], reads=[so], writes=[QK.b((c // 128, tb))])

            self.linear(es, HT, w_in, w_in[:], D, 0, 3328, "fm", epi)

            def epi_v(ps, cs, ncol, tb, tok0):
                i = cnt[0] % 2
                cnt[0] += 1
                so = st_o[i]
                S.op("act", lambda e_: e_.copy(out=so[:, :ncol], in_=ps[:, :ncol]), reads=[ps], writes=[so])
                S.dma("pool", VV[tok0:tok0 + 128, :], so[:, :ncol], reads=[so], writes=[VV.b(tok0 // 128)])

            if 'nov' not in DBG:
                self.linear(es, HT, w_in, w_in[:], D, 3328, 3584, "tm", epi_v)
            S.barrier()

    def even_lru(self, e, raw_d, ixw_d, XA, YG, MIXT):
        S = self.S
        with ExitStack() as es:
            cn = self.sb(es, "cn", [128, 16], F32)
            cn2 = self.sb(es, "cn2", [128, 16], F32)
            S.op("act", lambda e_: e_.activation(out=cn[:], in_=self.vcol(f"ev_lam{e}", 0, 16), func=AF.Exp, scale=-1.0),
                 reads=[self.V], writes=[cn])
            S.op("act", lambda e_: e_.activation(out=cn[:], in_=cn[:], func=AF.Ln, bias=1.0, scale=1.0),
                 reads=[cn], writes=[cn])
            S.op("dve", lambda e_: e_.tensor_scalar(out=cn2[:], in0=cn[:], scalar1=-16.0, scalar2=None, op0=ALU.mult),
                 reads=[cn], writes=[cn2])
            S.op("dve", lambda e_: e_.tensor_scalar(out=cn[:], in0=cn[:], scalar1=-8.0, scalar2=None, op0=ALU.mult),
                 reads=[cn], writes=[cn])
            gw = self.sb(es, "gw", [128, 2, 2, 8, 128], BF16)
            for gi, wd in enumerate((raw_d, ixw_d)):
                for d in range(2):
                    S.dma("pool", gw[:, gi, d, :, :], wd[e, d].rearrange("h i j -> i h j"), reads=[wd], writes=[gw])
            B = [self.sb(es, f"lru{i}", [128, T], F32) for i in range(7)]
            xab = self.sb(es, "xab", [128, T], BF16)
            ygt = self.sb(es, "ygt", [128, T], BF16)
            outb = self.sb(es, "outb", [128, T], BF16)
            xp, xa, Rb, Ib, Ab, HF, HB = B
            segs = [(0, CTX), (CTX, T)]
            for h in range(8):
                S.dma("sp", xp[:], XA[h * 128:(h + 1) * 128, :], reads=[XA.b((h, tb)) for tb in range(9)], writes=[xp])
                S.dma("sp", ygt[:], YG[h * 128:(h + 1) * 128, :], reads=[YG.b((h, tb)) for tb in range(9)], writes=[ygt])
                cw = lambda j: self.vcol(f"ev_cw{e}", j * 8 + h, 1)
                for (a0, a1) in segs:
                    S.op("dve", lambda e_, a0=a0, a1=a1: e_.tensor_scalar(
                        out=xa[:, a0:a1], in0=xp[:, a0:a1], scalar1=cw(2), scalar2=self.vcol(f"ev_cb{e}", h, 1),
                        op0=ALU.mult, op1=ALU.add), reads=[xp, self.V], writes=[xa])
                    for j, off in ((0, -2), (1, -1), (3, 1)):
                        lo = max(a0, a0 - off)
                        hi = min(a1, a1 - off)
                        S.op("dve", lambda e_, j=j, off=off, lo=lo, hi=hi: e_.scalar_tensor_tensor(
                            out=xa[:, lo:hi], in0=xp[:, lo + off:hi + off], scalar=cw(j), in1=xa[:, lo:hi],
                            op0=ALU.mult, op1=ALU.add), reads=[xp, self.V], writes=[xa])
                S.op("pool", lambda e_: e_.tensor_copy(out=xab[:], in_=xa[:]), reads=[xa], writes=[xab])
                for d in range(2):
                    col = d * 8 + h
                    for gi, dst, bname in ((0, Rb, f"ev_rab{e}"), (1, Ib, f"ev_ixb{e}")):
                        for (t0, tn) in TBLOCKS:
                            ps = self.psum()
                            S.op("pe", lambda e_, ps=ps, gi=gi, t0=t0, tn=tn: e_.matmul(
                                ps[:, :tn], lhsT=gw[:, gi, d, h, :], rhs=xab[:, t0:t0 + tn], start=True, stop=True),
                                reads=[gw, xab], writes=[ps])
                            S.op("act", lambda e_, ps=ps, dst=dst, t0=t0, tn=tn, bname=bname: e_.activation(
                                out=dst[:, t0:t0 + tn], in_=ps[:, :tn], func=AF.Sigmoid,
                                bias=self.vcol(bname, col, 1), scale=1.0), reads=[ps, self.V], writes=[dst])
                    S.op("act", lambda e_: e_.activation(out=Ab[:], in_=Rb[:], func=AF.Exp, scale=cn[:, col:col + 1]),
                         reads=[Rb, cn], writes=[Ab])
                    S.op("act", lambda e_: e_.activation(out=xp[:], in_=Rb[:], func=AF.Exp, scale=cn2[:, col:col + 1]),
                         reads=[Rb, cn2], writes=[xp])
                    S.op("dve", lambda e_: e_.tensor_scalar(out=xp[:], in0=xp[:], scalar1=-1.0, scalar2=1.0,
                                                            op0=ALU.mult, op1=ALU.add), reads=[xp], writes=[xp])
                    S.op("act", lambda e_: e_.activation(out=xp[:], in_=xp[:], func=AF.Sqrt), reads=[xp], writes=[xp])
                    S.op("pool", lambda e_: e_.tensor_tensor(out=Ib[:], in0=Ib[:], in1=xa[:], op=ALU.mult),
                         reads=[xa], writes=[Ib])
                    S.op("pool", lambda e_: e_.tensor_tensor(out=Ib[:], in0=Ib[:], in1=xp[:], op=ALU.mult),
                         reads=[xp], writes=[Ib])
                    if d == 0:
                        S.op("dve", lambda e_: e_.tensor_tensor_scan(
                            out=HF[:], data0=Ab[:], data1=Ib[:], initial=0.0, op0=ALU.mult, op1=ALU.add),
                            reads=[Ab, Ib], writes=[HF])
                    else:
                        S.op("dve", lambda e_: e_.tensor_tensor_scan(
                            out=HB[:, 0:CTX][:, ::-1], data0=Ab[:, 0:CTX][:, ::-1], data1=Ib[:, 0:CTX][:, ::-1],
                            initial=0.0, op0=ALU.mult, op1=ALU.add), reads=[Ab, Ib], writes=[HB])
                        S.op("dve", lambda e_: e_.tensor_tensor_scan(
                            out=HB[:, CTX:T][:, ::-1], data0=Ab[:, CTX:T][:, ::-1], data1=Ib[:, CTX:T][:, ::-1],
                            initial=HB[:, 0:1], op0=ALU.mult, op1=ALU.add), reads=[Ab, Ib], writes=[HB])
                S.op("pool", lambda e_: e_.tensor_tensor(out=HF[:], in0=HF[:], in1=HB[:], op=ALU.add),
                     reads=[HB], writes=[HF])
                S.op("pool", lambda e_: e_.tensor_tensor(out=outb[:], in0=HF[:], in1=ygt[:], op=ALU.mult),
                     reads=[HF, ygt], writes=[outb])
                S.dma("sp", MIXT[h * 128:(h + 1) * 128, :], outb[:], reads=[outb],
                      writes=[MIXT.b((h, tb)) for tb in range(9)])
            S.barrier()

    def even_attn(self, e, QK, VV, MIXT):
        S = self.S
        scale = 128.0 ** -0.5
        with ExitStack() as es:
            SE = self.sb(es, "SE", [128, 8], F32)
            S.op("act", lambda e_: e_.activation(out=SE[:], in_=self.vcol(f"ev_sink{e}", 0, 8), func=AF.Exp),
                 reads=[self.V], writes=[SE])
            MK = self.sb(es, "MK", [128, 2, 4, 128], BF16)
            for mi in range(2):
                for g in range(4):
                    S.op("pool", lambda e_, mi=mi, g=g: e_.tensor_copy(out=MK[:, mi, g, :], in_=self.CB[:, 2 + mi, :]),
                         reads=[self.CB], writes=[MK])
            KT = self.sb(es, "KT", [128, T], BF16)
            Vt = self.sb(es, "Vt", [128, 34, 128], BF16)
            QT = self.sb(es, "QT", [128, 4, T], BF16)
            OUT = self.sb(es, "OUT", [128, 4, T], BF16)
            ESB = [self.sb(es, f"esb{i}", [128, 5, 512], BF16) for i in range(2)]
            TMP = [self.sb(es, f"atmp{i}", [128, 512], F32) for i in range(2)]
            allqk = lambda c: [QK.b((c, tb)) for tb in range(9)]
            for kv in range(2):
                S.dma("sp", KT[:], QK[(8 + kv) * 128:(9 + kv) * 128, :], reads=allqk(8 + kv), writes=[KT])
                S.dma("sp", Vt[:], VV[:, kv * 128:(kv + 1) * 128].rearrange("(n p) d -> p n d", p=128),
                      reads=[VV.b(i) for i in range(34)], writes=[Vt])
                for g in range(4):
                    S.dma("sp", QT[:, g, :], QK[(kv * 4 + g) * 128:(kv * 4 + g + 1) * 128, :],
                          reads=allqk(kv * 4 + g), writes=[QT])
                for qb in range(34):
                    t0 = qb * 128
                    if qb < 2:
                        kts = [(0, None), (1, None)]
                    else:
                        kts = [(0, None), (1, None)]
                        if qb - 2 >= 1:
                            kts.append((qb - 1, 0))
                        kts.append((qb, None))
                        if qb - 2 <= 30:
                            kts.append((qb + 1, 1))
                    esb = ESB[qb % 2]
                    tmp = TMP[qb % 2]
                    nk = len(kts)
                    for i, (kt, mk) in enumerate(kts):
                        ps = self.psum()
                        S.op("pe", lambda e_, ps=ps, kt=kt: e_.matmul(
                            ps[:, 0:512].rearrange("p (g q) -> p g q", g=4), lhsT=KT[:, kt * 128:(kt + 1) * 128],
                            rhs=QT[:, :, t0:t0 + 128], start=True, stop=True), reads=[KT, QT], writes=[ps])
                        S.op("act", lambda e_, ps=ps, i=i: e_.activation(out=esb[:, i, :], in_=ps[:, 0:512], func=AF.Exp,
                                                                       scale=scale), reads=[ps], writes=[esb])
                        if mk is not None:
                            S.op("pool", lambda e_, i=i, mk=mk: e_.tensor_tensor(
                                out=esb[:, i, :], in0=esb[:, i, :], in1=MK[:, mk].rearrange("p g q -> p (g q)"),
                                op=ALU.mult), reads=[MK], writes=[esb])
                    psd = self.psum()
                    for i in range(nk):
                        S.op("pe", lambda e_, i=i: e_.matmul(psd[:, 0:512], lhsT=self.ONESB, rhs=esb[:, i, :],
                                                             start=(i == 0), stop=(i == nk - 1)),
                             reads=[esb, self.CB], writes=[psd])
                    pso = self.psum()
                    for i, (kt, mk) in enumerate(kts):
                        S.op("pe", lambda e_, i=i, kt=kt: e_.matmul(pso[:, 0:512], lhsT=Vt[:, kt, :], rhs=esb[:, i, :],
                                                                     start=(i == 0), stop=(i == nk - 1)),
                             reads=[esb, Vt], writes=[pso])
                    for g in range(4):
                        S.op("dve", lambda e_, g=g: e_.tensor_scalar(
                            out=tmp[:, g * 128:(g + 1) * 128], in0=psd[:, g * 128:(g + 1) * 128],
                            scalar1=SE[:, kv * 4 + g:kv * 4 + g + 1], scalar2=None, op0=ALU.add),
                            reads=[psd, SE], writes=[tmp])
                    S.op("dve", lambda e_: e_.reciprocal(out=tmp[:], in_=tmp[:]), reads=[tmp], writes=[tmp])
                    S.op("dve", lambda e_: e_.tensor_tensor(
                        out=OUT[:, :, t0:t0 + 128], in0=pso[:, 0:512].rearrange("p (g q) -> p g q", g=4),
                        in1=tmp[:].rearrange("p (g q) -> p g q", g=4), op=ALU.mult), reads=[pso, tmp], writes=[OUT])
                for g in range(4):
                    hh = 8 + kv * 4 + g
                    S.dma("sp", MIXT[hh * 128:(hh + 1) * 128, :], OUT[:, g, :], reads=[OUT],
                          writes=[MIXT.b((hh, tb)) for tb in range(9)])
            S.barrier()

    def out_proj_residual(self, l, jgate, MIXT, w_out, XT):
        S = self.S
        with ExitStack() as es:
            xr = [self.sb(es, f"xr{i}", [128, 512], F32) for i in range(3)]
            cnt = [0]

            def epi(ps, col, tb, t0, tn):
                x = xr[cnt[0] % 3]
                cnt[0] += 1
                kc = col // 128
                wh = 1 if tb == 0 else 0
                S.dma("pool", x[:, :tn], XT[col:col + 128, t0:t0 + tn], reads=[XT.b((kc, tb))], writes=[x])
                S.op("dve", lambda e_: e_.scalar_tensor_tensor(
                    out=x[:, :tn], in0=ps[:, :tn], scalar=self.mod(l, jgate, kc, wh), in1=x[:, :tn],
                    op0=ALU.mult, op1=ALU.add), reads=[ps, self.MOD], writes=[x])
                S.dma("pool", XT[col:col + 128, t0:t0 + tn], x[:, :tn], reads=[x], writes=[XT.b((kc, tb))])

            for tb in range(9):
                agg = MIXT.b(tb)
                for c in range(16):
                    sb_ = MIXT.b((c, tb))
                    for k, v in sb_.w.items():
                        if agg.w.get(k, 0) < v:
                            agg.w[k] = v
            self.linear(es, MIXT, w_out, w_out[:], D, 0, D, "fm", epi)
            S.barrier()

    def build(self, layers=range(DEPTH), do_mixer=True, do_moe=True, do_final=True, stop=99):
        S = self.S
        self.setup()
        xT_in = self.dram_in("xT", [D, T])
        XT = self.dram("XT", [D, T], F32)
        for kc in range(16):
            S.dma("sp", XT[kc * 128:(kc + 1) * 128, :], xT_in[kc * 128:(kc + 1) * 128, :], reads=[xT_in], writes=[XT])
        S.barrier()
        OUT = self.dram_out("out", [SEQ, D]) if do_final else None
        ada_w = {l: self.dram_in(f"ada_w{l}", [D, 6 * D]) for l in layers}
        self.cos_d = self.dram_in("cosT", [128, SEQ])
        self.sin_d = self.dram_in("sinT", [128, SEQ])
        HT = self.dram("HT", [D, T], BF16)
        MIXT = self.dram("MIXT", [D, T], BF16)
        self.XT, self.HT, self.MIXT = XT, HT, MIXT
        evs = sorted({l // 2 for l in layers if l % 2 == 0})
        ods = sorted({l // 2 for l in layers if l % 2 == 1})
        if do_mixer and evs:
            ev_w_in = {e: self.dram_in(f"ev_w_in{e}", [D, 3584]) for e in evs}
            ev_w_out = {e: self.dram_in(f"ev_w_out{e}", [D, D]) for e in evs}
            ev_ra = self.dram_in("ev_ra_w", [2, 2, 8, 128, 128])
            ev_ix = self.dram_in("ev_ix_w", [2, 2, 8, 128, 128])
            XA = self.dram("XA", [1024, T], F32)
            YG = self.dram("YG", [1024, T], BF16)
            QK = self.dram("QK", [1280, T], BF16)
            VV = self.dram("VV", [T, 256], BF16)
        if do_mixer and ods:
            self.odd_decl(ods)
        if do_moe:
            self.moe_decl(layers)
        if stop >= 1:
            self.phase_mod(ada_w, layers)
        for l in layers:
            if stop < 2:
                break
            if do_mixer:
                with ExitStack() as es:
                    def consume(tb, t0, tn, hb, hf):
                        S.dma("sp", HT.h.rearrange("(kc p) t -> p kc t", p=128)[:, :, t0:t0 + tn], hb[:, :, :tn],
                              reads=[hb], writes=[HT.b(tb)])
                    self.phase_norm(XT, l, 0, f"nmix{l}", consume, es)
                    S.barrier()
                if l % 2 == 0:
                    e = l // 2
                    if stop >= 3:
                        self.even_proj(l, e, ev_w_in[e], HT, XA, YG, QK, VV, self.cos_d, self.sin_d)
                    if stop >= 4:
                        self.even_lru(e, ev_ra, ev_ix, XA, YG, MIXT)
                    if stop >= 5:
                        self.even_attn(e, QK, VV, MIXT)
                    if stop >= 6:
                        self.out_proj_residual(l, 2, MIXT, ev_w_out[e], XT)
                else:
                    self.odd_mixer(l, l // 2)
            if do_moe:
                self.moe_layer(l)
        if do_final:
            self.final_norm(XT, OUT)
        S.barrier()
        self.es.close()
        return self.nc

    def moe_decl(self, layers):
        self.moe_r = {l: self.dram_in(f"moe_r{l}", [D, NE]) for l in layers}
        self.moe_w = {}
        for l in layers:
            for hh in range(2):
                self.moe_w[(l, "g", hh)] = self.dram_in(f"wg{l}_{hh}", [8, D, DEXP])
                self.moe_w[(l, "u", hh)] = self.dram_in(f"wu{l}_{hh}", [8, D, DEXP])
                self.moe_w[(l, "d", hh)] = self.dram_in(f"wd{l}_{hh}", [8, DEXP, D])
        self.Hrow = self.dram("Hrow", [T, D], BF16)
        self.Yacc = self.dram("Yacc", [T, D], F32)

    def moe_layer(self, l):
        S = self.S
        XT, Hrow, Yacc = self.XT, self.Hrow, self.Yacc
        with ExitStack() as eso:
            IDXT = self.sb(eso, "IDXT", [128, 5, 16], U32)
            GT = self.sb(eso, "GT", [128, 5, 16], F32)
            eso2 = ExitStack()
            AFFT = self.sb(eso2, "AFFT", [16, T], F32)
            zt = self.sb(eso2, "zt", [128, 2048], F32)
            S.op("pool", lambda e_: e_.memset(zt[:], 0.0), writes=[zt])
            for i in range(34):
                S.dma("sp", Yacc[i * 128:(i + 1) * 128, :], zt[:], reads=[zt], writes=[Yacc.b(i)])
            with ExitStack() as es:
                WR = self.sb(es, "WR", [128, 16, 16], F32)
                S.dma("sp", WR[:], self.moe_r[l].h.rearrange("(kc p) e -> p kc e", p=128), reads=[self.moe_r[l]], writes=[WR])
                hrow = [self.sb(es, f"hrow{i}", [128, 2048], BF16) for i in range(2)]
                sm = [self.sb(es, f"smx{i}", [128, 4], F32) for i in range(2)]
                ex = [self.sb(es, f"ex{i}", [128, 16], F32) for i in range(2)]
                cnt = [0]

                def consume(tb, t0, tn, hb, hf):
                    for s_ in range(tn // 128):
                        i = cnt[0] % 2
                        cnt[0] += 1
                        tok0 = t0 + s_ * 128
                        ps = self.psum()
                        for kc in range(16):
                            S.op("pe", lambda e_, kc=kc, ps=ps: e_.matmul(
                                ps[:, 0:16], lhsT=hf[:, kc, s_ * 128:(s_ + 1) * 128], rhs=WR[:, kc, :],
                                start=(kc == 0), stop=(kc == 15)), reads=[hf, WR], writes=[ps])
                        m, x_ = sm[i], ex[i]
                        S.op("dve", lambda e_, ps=ps: e_.tensor_reduce(out=m[:, 0:1], in_=ps[:, 0:16], axis=AX.X, op=ALU.max,
                                                                     negate=True), reads=[ps], writes=[m])
                        S.op("act", lambda e_, ps=ps: e_.activation(out=x_[:], in_=ps[:, 0:16], func=AF.Exp, bias=m[:, 0:1],
                                                                    scale=1.0, accum_out=m[:, 1:2]), reads=[ps, m], writes=[x_, m])
                        S.op("dve", lambda e_: e_.reciprocal(out=m[:, 2:3], in_=m[:, 1:2]), reads=[m], writes=[m])
                        S.op("dve", lambda e_: e_.tensor_scalar(out=x_[:], in0=x_[:], scalar1=m[:, 2:3], scalar2=None,
                                                                op0=ALU.mult), reads=[m], writes=[x_])
                        ps2 = self.psum()
                        S.op("pe", lambda e_, ps2=ps2: e_.transpose(out=ps2[0:16, 0:128], in_=x_[:], identity=self.IDF[:]),
                             reads=[x_, self.IDF], writes=[ps2])
                        S.op("act", lambda e_, ps2=ps2: e_.copy(out=AFFT[:, tok0:tok0 + 128], in_=ps2[0:16, 0:128]),
                             reads=[ps2], writes=[AFFT])
                        hr = hrow[i]
                        for kg in range(4):
                            ps3 = self.psum()
                            for kk in range(4):
                                kc = kg * 4 + kk
                                S.op("pe", lambda e_, kc=kc, kk=kk, ps3=ps3: e_.transpose(
                                    out=ps3[:, kk * 128:(kk + 1) * 128], in_=hf[:, kc, s_ * 128:(s_ + 1) * 128],
                                    identity=self.IDF[:]), reads=[hf, self.IDF], writes=[ps3])
                            eng = "act" if kg % 2 == 0 else "dve"
                            if eng == "act":
                                S.op("act", lambda e_, ps3=ps3, kg=kg: e_.copy(out=hr[:, kg * 512:(kg + 1) * 512], in_=ps3[:, 0:512]),
                                     reads=[ps3], writes=[hr])
                            else:
                                S.op("dve", lambda e_, ps3=ps3, kg=kg: e_.tensor_copy(out=hr[:, kg * 512:(kg + 1) * 512], in_=ps3[:, 0:512]),
                                     reads=[ps3], writes=[hr])
                        S.dma("sp", Hrow[tok0:tok0 + 128, :], hr[:], reads=[hr], writes=[Hrow.b(tok0 // 128)])

                self.phase_norm(XT, l, 3, f"nffn{l}", consume, es, want_f32=True)
                S.barrier()
            with ExitStack() as es:
                work = self.sb(es, "work", [16, SEQ], F32)
                VAL = self.sb(es, "VAL", [16, 544], F32)
                IDXu = self.sb(es, "IDXu", [16, 544], U32)
                IDXf = self.sb(es, "IDXf", [16, 544], F32)
                for (c0, n, cap, s0, toff) in ((0, CTX, 32, 512, 0), (CTX, SEQ, 512, 0, CTX)):
                    S.op("act", lambda e_: e_.copy(out=work[:, :n], in_=AFFT[:, c0:c0 + n]), reads=[AFFT], writes=[work])
                    for it in range(cap // 8):
                        sl = slice(s0 + it * 8, s0 + it * 8 + 8)
                        S.op("dve", lambda e_, sl=sl: e_.max(out=VAL[:, sl], in_=work[:, :n]), reads=[work], writes=[VAL])
                        S.op("dve", lambda e_, sl=sl: e_.max_index(out=IDXu[:, sl], in_max=VAL[:, sl], in_values=work[:, :n]),
                             reads=[work, VAL], writes=[IDXu])
                        S.op("dve", lambda e_, sl=sl: e_.match_replace(out=work[:, :n], in_to_replace=VAL[:, sl],
                                                                       in_values=work[:, :n], imm_value=-1.0),
                             reads=[VAL], writes=[work])
                    S.op("dve", lambda e_: e_.tensor_copy(out=IDXf[:, s0:s0 + cap], in_=IDXu[:, s0:s0 + cap]),
                         reads=[IDXu], writes=[IDXf])
                    if toff:
                        S.op("dve", lambda e_: e_.tensor_scalar(out=IDXf[:, s0:s0 + cap], in0=IDXf[:, s0:s0 + cap],
                                                                scalar1=float(toff), scalar2=None, op0=ALU.add),
                             reads=[IDXf], writes=[IDXf])
                S.op("pool", lambda e_: e_.memset(IDXT[:], 0), writes=[IDXT])
                S.op("pool", lambda e_: e_.memset(GT[:], 0.0), writes=[GT])
                for st in range(5):
                    ns = 128 if st < 4 else 32
                    for src, dst in ((IDXf, IDXT), (VAL, GT)):
                        ps = self.psum()
                        S.op("pe", lambda e_, ps=ps, src=src: e_.transpose(
                            out=ps[0:ns, 0:16], in_=src[:, st * 128:st * 128 + ns], identity=self.IDF[0:16, 0:16]),
                            reads=[src, self.IDF], writes=[ps])
                        S.op("dve", lambda e_, ps=ps, dst=dst: e_.tensor_copy(out=dst[0:ns, st, :], in_=ps[0:ns, 0:16]),
                             reads=[ps], writes=[dst])
                S.barrier()
            eso2.close()
            with ExitStack() as es:
                FG = 512
                NG = DEXP // FG
                wgt = [self.sb(es, f"wgt{i}", [128, 16, FG], BF16) for i in range(2)]
                wut = [self.sb(es, f"wut{i}", [128, 16, FG], BF16) for i in range(2)]
                wdt = [self.sb(es, f"wdt{i}", [128, FG // 128, D], BF16) for i in range(2)]
                xs = [self.sb(es, f"xs{i}", [128, D], BF16) for i in range(2)]
                xsT = self.sb(es, "xsT", [128, 16, 544], BF16)
                hidT = self.sb(es, "hidT", [128, FG // 128, 544], BF16)
                sg = [self.sb(es, f"sg{i}", [128, 512], F32) for i in range(2)]
                yacc = self.sb(es, "yacc", [128, 5, D], F32)
                wi = 0
                xi = 0
                si = 0
                for ex_ in range(NE):
                    Wg = self.moe_w[(l, "g", ex_ // 8)]
                    Wu = self.moe_w[(l, "u", ex_ // 8)]
                    Wd = self.moe_w[(l, "d", ex_ // 8)]
                    el = ex_ % 8
                    for st in range(5):
                        ns = 128 if st < 4 else 32
                        x = xs[xi % 2]
                        xi += 1
                        S.dma("pool", None, None, reads=[Hrow.b(i) for i in range(34)] + [IDXT], writes=[x],
                              fn=lambda e_, x=x, ns=ns, st=st: e_.indirect_dma_start(
                                  out=x[0:ns, :], out_offset=None, in_=Hrow[:, :],
                                  in_offset=bass.IndirectOffsetOnAxis(ap=IDXT[0:ns, st, ex_:ex_ + 1], axis=0)))
                        for kg in range(2):
                            ps = self.psum()
                            psb = ps.h.bitcast(BF16)
                            for kk in range(8):
                                kc = kg * 8 + kk
                                S.op("pe", lambda e_, kc=kc, kk=kk, psb=psb, x=x, ns=ns: e_.transpose(
                                    out=psb[:, kk * 128:kk * 128 + ns], in_=x[0:ns, kc * 128:(kc + 1) * 128],
                                    identity=self.IDB[0:ns, 0:ns]), reads=[x, self.CB], writes=[ps])
                            S.op("act" if kg == 0 else "dve",
                                 (lambda e_, psb=psb, kg=kg, st=st, ns=ns: e_.copy(
                                     out=xsT[:, kg * 8:(kg + 1) * 8, st * 128:st * 128 + ns],
                                     in_=psb[:, 0:1024].rearrange("p (k s) -> p k s", k=8)[:, :, 0:ns])) if kg == 0 else
                                 (lambda e_, psb=psb, kg=kg, st=st, ns=ns: e_.tensor_copy(
                                     out=xsT[:, kg * 8:(kg + 1) * 8, st * 128:st * 128 + ns],
                                     in_=psb[:, 0:1024].rearrange("p (k s) -> p k s", k=8)[:, :, 0:ns])),
                                 reads=[ps], writes=[xsT])
                    for g in range(NG):
                        wg_, wu_, wd_ = wgt[wi % 2], wut[wi % 2], wdt[wi % 2]
                        wi += 1
                        f0 = g * FG
                        S.dma("pool", wg_[:], Wg[el, :, f0:f0 + FG].rearrange("(kc p) f -> p kc f", p=128), reads=[Wg], writes=[wg_])
                        S.dma("pool", wu_[:], Wu[el, :, f0:f0 + FG].rearrange("(kc p) f -> p kc f", p=128), reads=[Wu], writes=[wu_])
                        S.dma("pool", wd_[:], Wd[el, f0:f0 + FG, :].rearrange("(fc p) d -> p fc d", p=128), reads=[Wd], writes=[wd_])
                        for fc in range(FG // 128):
                            for (c0, ns) in ((0, 512), (512, 32)):
                                psg = self.psum()
                                psu = self.psum()
                                for kc in range(16):
                                    S.op("pe", lambda e_, kc=kc, psg=psg, c0=c0, ns=ns, fc=fc, wg_=wg_: e_.matmul(
                                        psg[:, 0:ns], lhsT=wg_[:, kc, fc * 128:(fc + 1) * 128], rhs=xsT[:, kc, c0:c0 + ns],
                                        start=(kc == 0), stop=(kc == 15)), reads=[wg_, xsT], writes=[psg])
                                for kc in range(16):
                                    S.op("pe", lambda e_, kc=kc, psu=psu, c0=c0, ns=ns, fc=fc, wu_=wu_: e_.matmul(
                                        psu[:, 0:ns], lhsT=wu_[:, kc, fc * 128:(fc + 1) * 128], rhs=xsT[:, kc, c0:c0 + ns],
                                        start=(kc == 0), stop=(kc == 15)), reads=[wu_, xsT], writes=[psu])
                                s_ = sg[si % 2]
                                si += 1
                                S.op("act", lambda e_, psg=psg, s_=s_, ns=ns: e_.activation(out=s_[:, 0:ns], in_=psg[:, 0:ns], func=AF.Silu),
                                     reads=[psg], writes=[s_])
                                S.op("dve", lambda e_, psu=psu, s_=s_, ns=ns, c0=c0, fc=fc: e_.tensor_tensor(
                                    out=hidT[:, fc, c0:c0 + ns], in0=psu[:, 0:ns], in1=s_[:, 0:ns], op=ALU.mult),
                                    reads=[psu, s_], writes=[hidT])
                        for st in range(5):
                            ns = 128 if st < 4 else 32
                            for dc in range(4):
                                psd = self.psum()
                                nf = FG // 128
                                for fc in range(nf):
                                    S.op("pe", lambda e_, fc=fc, psd=psd, st=st, ns=ns, dc=dc, wd_=wd_: e_.matmul(
                                        psd[0:ns, 0:512], lhsT=hidT[:, fc, st * 128:st * 128 + ns],
                                        rhs=wd_[:, fc, dc * 512:(dc + 1) * 512], start=(fc == 0), stop=(fc == nf - 1)),
                                        reads=[hidT, wd_], writes=[psd])
                                gsc = GT[0:ns, st, ex_:ex_ + 1]
                                if g == 0:
                                    S.op("act", lambda e_, psd=psd, st=st, ns=ns, dc=dc, gsc=gsc: e_.activation(
                                        out=yacc[0:ns, st, dc * 512:(dc + 1) * 512], in_=psd[0:ns, 0:512], func=AF.Copy, scale=gsc),
                                        reads=[psd, GT], writes=[yacc])
                                else:
                                    S.op("dve", lambda e_, psd=psd, st=st, ns=ns, dc=dc, gsc=gsc: e_.scalar_tensor_tensor(
                                        out=yacc[0:ns, st, dc * 512:(dc + 1) * 512], in0=psd[0:ns, 0:512], scalar=gsc,
                                        in1=yacc[0:ns, st, dc * 512:(dc + 1) * 512], op0=ALU.mult, op1=ALU.add),
                                        reads=[psd, GT], writes=[yacc])
                    for st in range(5):
                        ns = 128 if st < 4 else 32
                        S.dma("pool", None, None, reads=[yacc, IDXT], writes=[Yacc],
                              fn=lambda e_, ns=ns, st=st: e_.indirect_dma_start(
                                  out=Yacc[:, :], out_offset=bass.IndirectOffsetOnAxis(ap=IDXT[0:ns, st, ex_:ex_ + 1], axis=0),
                                  in_=yacc[0:ns, st, :], in_offset=None, compute_op=ALU.add))
                S.barrier()
            with ExitStack() as es:
                yt = [self.sb(es, f"yt{i}", [128, D], F32) for i in range(2)]
                xr = [self.sb(es, f"mxr{i}", [128, 16, 128], F32) for i in range(2)]
                XTv = XT.h.rearrange("(kc p) t -> p kc t", p=128)
                for i in range(34):
                    y = yt[i % 2]
                    x = xr[i % 2]
                    wh = 1 if i < 2 else 0
                    S.dma("sp", y[:], Yacc[i * 128:(i + 1) * 128, :], reads=[Yacc], writes=[y])
                    S.dma("sp", x[:], XTv[:, :, i * 128:(i + 1) * 128], reads=[XT.b(("m5", i))], writes=[x])
                    for kg in range(4):
                        ps = self.psum()
                        for kk in range(4):
                            kc = kg * 4 + kk
                            S.op("pe", lambda e_, kc=kc, kk=kk, ps=ps, y=y: e_.transpose(
                                out=ps[:, kk * 128:(kk + 1) * 128], in_=y[:, kc * 128:(kc + 1) * 128], identity=self.IDF[:]),
                                reads=[y, self.IDF], writes=[ps])
                        for kk in range(4):
                            kc = kg * 4 + kk
                            S.op("dve", lambda e_, kc=kc, kk=kk, ps=ps, x=x: e_.scalar_tensor_tensor(
                                out=x[:, kc, :], in0=ps[:, kk * 128:(kk + 1) * 128], scalar=self.mod(l, 5, kc, wh),
                                in1=x[:, kc, :], op0=ALU.mult, op1=ALU.add), reads=[ps, self.MOD], writes=[x])
                    S.dma("sp", XTv[:, :, i * 128:(i + 1) * 128], x[:], reads=[x], writes=[XT.b(("m5", i))])
                S.barrier()

    def final_norm(self, XT, OUT):
        S = self.S
        with ExitStack() as es:
            xts = [self.sb(es, f"fx{i}", [128, 16, 512], F32) for i in range(2)]
            sq = self.sb(es, "fsq", [128, 16, 512], BF16)
            rs = self.sb(es, "frs", [128, 512], F32)
            ot = [self.sb(es, f"fo{i}", [128, D], F32) for i in range(2)]
            XTv = XT.h.rearrange("(kc p) t -> p kc t", p=128)
            oi = 0
            for tb, (t0, tn) in list(enumerate(TBLOCKS))[1:]:
                xt = xts[tb % 2]
                S.dma("sp", xt[:], XTv[:, :, t0:t0 + tn], reads=[XT], writes=[xt])
                S.op("act", lambda e: e.activation(out=sq[:], in_=xt[:], func=AF.Square), reads=[xt], writes=[sq])
                acc = self.psum()
                for kc in range(16):
                    S.op("pe", lambda e: e.matmul(acc[:, :], lhsT=self.ONESB, rhs=sq[:, kc, :], start=(kc == 0), stop=(kc == 15)),
                         reads=[sq, self.CB], writes=[acc])
                S.op("act", lambda e: e.activation(out=rs[:], in_=acc[:, :], func=AF.Sqrt, bias=self.epsT[:, 0:1], scale=1.0 / D),
                     reads=[acc, self.epsT], writes=[rs])
                S.op("dve", lambda e: e.reciprocal(out=rs[:], in_=rs[:]), reads=[rs], writes=[rs])
                for kc in range(16):
                    S.op("dve", lambda e: e.scalar_tensor_tensor(out=xt[:, kc, :], in0=xt[:, kc, :], scalar=self.vcol("fnorm", kc, 1),
                                                                 in1=rs[:], op0=ALU.mult, op1=ALU.mult), reads=[rs, self.V], writes=[xt])
                for s_ in range(4):
                    o = ot[oi % 2]
                    oi += 1
                    for kg in range(4):
                        ps = self.psum()
                        for kk in range(4):
                            kc = kg * 4 + kk
                            S.op("pe", lambda e: e.transpose(out=ps[:, kk * 128:(kk + 1) * 128], in_=xt[:, kc, s_ * 128:(s_ + 1) * 128],
                                                             identity=self.IDF[:]), reads=[xt, self.IDF], writes=[ps])
                        if kg % 2 == 0:
                            S.op("act", lambda e: e.copy(out=o[:, kg * 512:(kg + 1) * 512], in_=ps[:, 0:512]), reads=[ps], writes=[o])
                        else:
                            S.op("dve", lambda e: e.tensor_copy(out=o[:, kg * 512:(kg + 1) * 512], in_=ps[:, 0:512]), reads=[ps], writes=[o])
                    r0 = t0 - CTX + s_ * 128
                    S.dma("sp", OUT[r0:r0 + 128, :], o[:], reads=[o], writes=[OUT.b(r0)])
            S.barrier()

    def odd_decl(self, ods):
        self.od_w_in = {o: self.dram_in(f"od_w_in{o}", [D, 6176]) for o in ods}
        self.od_w_out = {o: self.dram_in(f"od_w_out{o}", [D, D]) for o in ods}
        self.hnorm_d = {o: self.dram_in(f"hnorm_bc{o}", [128, D]) for o in ods}
        self.cf_d = self.dram_in("cf", [128, 5, 128])
        self.QKP = self.dram("QKP", [D, T], F32)
        self.QS = self.dram("QS", [D, T], BF16)
        self.VM = self.dram("VM", [T, D], BF16)
        self.OS = self.dram("OS", [T, D], BF16)
        self.GG = self.dram("GG", [T, 32], F32)
        self.HS = self.dram("HS", [T, D], F32)

    def odd_mixer(self, l, o, stop=99):
        S = self.S
        HT, MIXT, XT = self.HT, self.MIXT, self.XT
        w_in = self.od_w_in[o]
        QKP, QS, VM, OS, GG, HS = self.QKP, self.QS, self.VM, self.OS, self.GG, self.HS
        with ExitStack() as es:
            st_f = [self.sb(es, f"ostf{i}", [128, 512], F32) for i in range(2)]
            st_b = [self.sb(es, f"ostb{i}", [128, 512], BF16) for i in range(2)]
            cnt = [0]

            def epi_qk(ps, col, tb, t0, tn):
                sf = st_f[cnt[0] % 2]
                cnt[0] += 1
                S.op("act", lambda e: e.copy(out=sf[:, :tn], in_=ps[:, :tn]), reads=[ps], writes=[sf])
                S.dma("pool", QKP[col:col + 128, t0:t0 + tn], sf[:, :tn], reads=[sf], writes=[QKP.b((col, tb))])

            self.linear(es, HT, w_in, w_in[:], D, 0, 2048, "fm", epi_qk)

            def epi_tm(ps, cs, ncol, tb, tok0):
                i = cnt[0] % 2
                cnt[0] += 1
                if cs < 4096:
                    sb_ = st_b[i]
                    S.op("act", lambda e: e.copy(out=sb_[:, :ncol], in_=ps[:, :ncol]), reads=[ps], writes=[sb_])
                    S.dma("pool", VM[tok0:tok0 + 128, cs - 2048:cs - 2048 + ncol], sb_[:, :ncol], reads=[sb_], writes=[VM.b((cs, tok0))])
                elif cs < 6144:
                    sb_ = st_b[i]
                    S.op("act", lambda e: e.activation(out=sb_[:, :ncol], in_=ps[:, :ncol], func=AF.Sigmoid), reads=[ps], writes=[sb_])
                    S.dma("pool", OS[tok0:tok0 + 128, cs - 4096:cs - 4096 + ncol], sb_[:, :ncol], reads=[sb_], writes=[OS.b((cs, tok0))])
                else:
                    sf = st_f[i]
                    S.op("act", lambda e: e.copy(out=sf[:, :ncol], in_=ps[:, :ncol]), reads=[ps], writes=[sf])
                    S.dma("pool", GG[tok0:tok0 + 128, :], sf[:, :ncol], reads=[sf], writes=[GG.b(tok0)])

            self.linear(es, HT, w_in, w_in[:], D, 2048, 6176, "tm", epi_tm)
            S.barrier()
        if stop < 2:
            return
        with ExitStack() as es:
            xp = [self.sb(es, f"oxp{i}", [128, T], F32) for i in range(2)]
            xa = [self.sb(es, f"oxa{i}", [128, T], F32) for i in range(2)]
            xo = [self.sb(es, f"oxo{i}", [128, T], BF16) for i in range(2)]
            segs = [(0, CTX), (CTX, T)]
            for c in range(16):
                p_, a_, o_ = xp[c % 2], xa[c % 2], xo[c % 2]
                S.dma("sp", p_[:], QKP[c * 128:(c + 1) * 128, :], reads=[QKP], writes=[p_])
                cw = lambda j: self.vcol(f"od_cw{o}", j * 16 + c, 1)
                for (a0, a1) in segs:
                    S.op("dve", lambda e: e.tensor_scalar(out=a_[:, a0:a1], in0=p_[:, a0:a1], scalar1=cw(2),
                                                          scalar2=self.vcol(f"od_cb{o}", c, 1), op0=ALU.mult, op1=ALU.add),
                         reads=[p_, self.V], writes=[a_])
                    for j, off in ((0, -2), (1, -1), (3, 1)):
                        lo = max(a0, a0 - off)
                        hi = min(a1, a1 - off)
                        S.op("dve", lambda e: e.scalar_tensor_tensor(out=a_[:, lo:hi], in0=p_[:, lo + off:hi + off], scalar=cw(j),
                                                                     in1=a_[:, lo:hi], op0=ALU.mult, op1=ALU.add),
                             reads=[p_, self.V], writes=[a_])
                if c < 8:
                    S.op("act", lambda e: e.activation(out=a_[:], in_=a_[:], func=AF.Silu), reads=[a_], writes=[a_])
                    S.op("act", lambda e: e.activation(out=o_[:], in_=a_[:], func=AF.Copy, scale=128.0 ** -0.5),
                         reads=[a_], writes=[o_])
                else:
                    S.op("act", lambda e: e.activation(out=o_[:], in_=a_[:], func=AF.Silu), reads=[a_], writes=[o_])
                S.dma("sp", QS[c * 128:(c + 1) * 128, :], o_[:], reads=[o_], writes=[QS.b(c)])
            S.barrier()
        if stop < 3:
            return
        with ExitStack() as es:
            CF = self.sb(es, "CF", [128, 5, 128], F32)
            S.dma("sp", CF[:], self.cf_d[:], reads=[self.cf_d], writes=[CF])
            NCH = 34
            GGt = self.sb(es, "GGt", [128, NCH, 32], F32)
            S.dma("sp", GGt[:], GG.h.rearrange("(n p) g -> p n g", p=128), reads=[GG], writes=[GGt])
            gbb = self.vcol(f"od_gb{o}", 0, 32)
            for n in range(NCH):
                S.op("pool", lambda e: e.tensor_tensor(out=GGt[:, n, :], in0=GGt[:, n, :], in1=gbb, op=ALU.add),
                     reads=[self.V], writes=[GGt])
            GG4 = GGt[:].rearrange("p n (ty h) -> p n ty h", ty=4)
            LF = self.sb(es, "LF", [128, NCH, 2, 8], F32)
            for d in range(2):
                S.op("act", lambda e: e.activation(out=LF[:, :, d, :], in_=GG4[:, :, 2 * d + 1, :], func=AF.Exp, scale=-1.0),
                     reads=[GGt], writes=[LF])
            S.op("act", lambda e: e.activation(out=LF[:], in_=LF[:], func=AF.Ln, bias=1.0, scale=1.0), reads=[LF], writes=[LF])
            S.op("dve", lambda e: e.tensor_scalar(out=LF[:], in0=LF[:], scalar1=-1.0, scalar2=None, op0=ALU.mult),
                 reads=[LF], writes=[LF])
            Bt = self.sb(es, "Bt", [128, NCH, 2, 8], F32)
            Ct = self.sb(es, "Ct", [128, NCH, 2, 8], F32)
            EBt = self.sb(es, "EBt", [128, NCH, 2, 8], F32)
            for n in range(NCH):
                ps = self.psum()
                for d in range(2):
                    S.op("pe", lambda e: e.matmul(ps[:, d * 8:(d + 1) * 8], lhsT=CF[:, d, :], rhs=LF[:, n, d, :], start=True, stop=True),
                         reads=[CF, LF], writes=[ps])
                S.op("dve", lambda e: e.tensor_copy(out=Bt[:, n].rearrange("p d h -> p (d h)"), in_=ps[:, 0:16]),
                     reads=[ps], writes=[Bt])
            for d in range(2):
                S.op("dve", lambda e: e.tensor_tensor(out=Ct[:, :, d, :], in0=GG4[:, :, 2 * d, :], in1=Bt[:, :, d, :], op=ALU.subtract),
                     reads=[GGt, Bt], writes=[Ct])
            S.op("act", lambda e: e.activation(out=EBt[:], in_=Bt[:], func=AF.Exp), reads=[Bt], writes=[EBt])
            qT = self.sb(es, "qT", [128, T], BF16)
            kT = self.sb(es, "kT", [128, T], BF16)
            Va = self.sb(es, "Va", [128, NCH, 257], BF16)
            Hs = self.sb(es, "Hs", [128, NCH, 256], F32)
            ktm = self.sb(es, "ktm", [128, NCH, 128], BF16)
            Cf = [self.sb(es, f"Cf{d}", [128, 257], F32) for d in range(2)]
            Cb = [self.sb(es, f"Cb{d}", [128, 257], BF16) for d in range(2)]
            LFB = [self.sb(es, f"LFB{i}", [128, 128], F32) for i in range(3)]
            BBm = [self.sb(es, f"BBm{i}", [128, 128], F32) for i in range(3)]
            Dm = [self.sb(es, f"Dm{i}", [128, 128], F32) for i in range(3)]
            SD = [self.sb(es, f"SD{i}", [128, 128], BF16) for i in range(3)]
            ku = [self.sb(es, f"ku{i}", [128, 128], BF16) for i in range(3)]
            tin = [self.sb(es, f"tin{i}", [128, 257], F32) for i in range(2)]
            tot = [self.sb(es, f"tot{i}", [128, 257], F32) for i in range(2)]
            sc = [self.sb(es, f"sc{i}", [128, 8], F32) for i in range(3)]
            order_f = list(range(NCH))
            order_b = [1, 0] + list(range(NCH - 1, 1, -1))
            ui = 0
            for hd in range(8):
                S.dma("sp", qT[:], QS[hd * 128:(hd + 1) * 128, :], reads=[QS], writes=[qT])
                S.dma("sp", kT[:], QS[1024 + hd * 128:1024 + (hd + 1) * 128, :], reads=[QS], writes=[kT])
                S.dma("sp", Va[:, :, 0:256], VM[:, hd * 256:(hd + 1) * 256].rearrange("(n p) d -> p n d", p=128), reads=[VM], writes=[Va])
                S.op("pool", lambda e: e.memset(Va[:, :, 256:257], 1.0), writes=[Va])
                S.op("pool", lambda e: e.memset(Hs[:], 0.0), writes=[Hs])
                for n0 in range(0, NCH, 8):
                    nn = min(8, NCH - n0)
                    ps = self.psum()
                    psb = ps.h.bitcast(BF16)
                    for j in range(nn):
                        S.op("pe", lambda e: e.transpose(out=psb[:, j * 128:(j + 1) * 128], in_=kT[:, (n0 + j) * 128:(n0 + j + 1) * 128],
                                                         identity=self.IDB), reads=[kT, self.CB], writes=[ps])
                    S.op("act", lambda e: e.copy(out=ktm[:, n0:n0 + nn, :].rearrange("p n d -> p (n d)"), in_=psb[:, 0:nn * 128]),
                         reads=[ps], writes=[ktm])
                for d in range(2):
                    S.op("pool", lambda e: e.memset(Cf[d][:], 0.0), writes=[Cf[d]])
                    S.op("pool", lambda e: e.memset(Cb[d][:], 0.0), writes=[Cb[d]])
                units = []
                for step in range(NCH):
                    for d in range(2):
                        units.append((order_f[step] if d == 0 else order_b[step], d))
                NB3 = 3
                live = {}

                def stageA(ux):
                    n, d = units[ux]
                    i = ux % NB3
                    tsl = slice(n * 128, (n + 1) * 128)
                    last = 127 if d == 0 else 0
                    lfb, bbm, dm, sd, ku_, sc_ = LFB[i], BBm[i], Dm[i], SD[i], ku[i], sc[i]
                    S.op("act", lambda e: e.activation(out=lfb[:], in_=CF[:, 4, :], func=AF.Copy, scale=LF[:, n, d, hd:hd + 1]),
                         reads=[CF, LF], writes=[lfb])
                    psB = self.psum()
                    S.op("pe", lambda e: e.matmul(psB[:, 0:128], lhsT=lfb[:], rhs=CF[:, d, :], start=True, stop=True),
                         reads=[lfb, CF], writes=[psB])
                    S.op("dve", lambda e: e.tensor_tensor(out=bbm[:], in0=psB[:, 0:128], in1=CF[:, 2 + d, :], op=ALU.add),
                         reads=[psB, CF], writes=[bbm])
                    S.op("act", lambda e: e.copy(out=sc_[:, 0:1], in_=psB[:, last:last + 1]), reads=[psB], writes=[sc_])
                    S.op("act", lambda e: e.activation(out=dm[:], in_=bbm[:], func=AF.Exp, bias=Ct[:, n, d, hd:hd + 1], scale=1.0),
                         reads=[bbm, Ct], writes=[dm])
                    S.op("act", lambda e: e.activation(out=sc_[:, 1:2], in_=Ct[:, n, d, hd:hd + 1], func=AF.Exp, bias=sc_[:, 0:1], scale=1.0),
                         reads=[Ct, sc_], writes=[sc_])
                    S.op("act", lambda e: e.activation(out=sc_[:, 2:3], in_=sc_[:, 0:1], func=AF.Exp), reads=[sc_], writes=[sc_])
                    psS = self.psum()
                    S.op("pe", lambda e: e.matmul(psS[:, 0:128], lhsT=kT[:, tsl], rhs=qT[:, tsl], start=True, stop=True),
                         reads=[kT, qT], writes=[psS])
                    S.op("dve", lambda e: e.tensor_tensor(out=sd[:], in0=psS[:, 0:128], in1=dm[:], op=ALU.mult),
                         reads=[psS, dm], writes=[sd])
                    psI = self.psum()
                    S.op("pe", lambda e: e.matmul(psI[:, 0:257], lhsT=sd[:], rhs=Va[:, n, :], start=True, stop=True),
                         reads=[sd, Va], writes=[psI])
                    S.op("act", lambda e: e.activation(out=ku_[:], in_=ktm[:, n, :], func=AF.Copy, scale=sc_[:, 1:2]),
                         reads=[ktm, sc_], writes=[ku_])
                    psU = self.psum()
                    S.op("pe", lambda e: e.matmul(psU[:, 0:257], lhsT=ku_[:], rhs=Va[:, n, :], start=True, stop=True),
                         reads=[ku_, Va], writes=[psU])
                    live[ux] = (psI, psU)

                def stageB(ux):
                    n, d = units[ux]
                    i = ux % NB3
                    tsl = slice(n * 128, (n + 1) * 128)
                    tin_, tot_, sc_ = tin[i % 2], tot[i % 2], sc[i]
                    psI, psU = live.pop(ux)
                    psC = self.psum()
                    S.op("pe", lambda e: e.matmul(psC[:, 0:257], lhsT=qT[:, tsl], rhs=Cb[d][:], start=True, stop=True),
                         reads=[qT, Cb[d]], writes=[psC])
                    S.op("act", lambda e: e.activation(out=tin_[:], in_=psC[:, 0:257], func=AF.Copy, scale=EBt[:, n, d, hd:hd + 1]),
                         reads=[psC, EBt], writes=[tin_])
                    S.op("dve", lambda e: e.scalar_tensor_tensor(out=Cf[d][:], in0=Cf[d][:], scalar=sc_[:, 2:3], in1=psU[:, 0:257],
                                                                 op0=ALU.mult, op1=ALU.add), reads=[psU, sc_], writes=[Cf[d]])
                    S.op("act", lambda e: e.copy(out=Cb[d][:], in_=Cf[d][:]), reads=[Cf[d]], writes=[Cb[d]])
                    S.op("dve", lambda e: e.tensor_tensor(out=tot_[:], in0=psI[:, 0:257], in1=tin_[:], op=ALU.add),
                         reads=[psI, tin_], writes=[tot_])
                    S.op("act", lambda e: e.activation(out=sc_[:, 3:4], in_=tot_[:, 256:257], func=AF.Abs), reads=[tot_], writes=[sc_])
                    S.op("dve", lambda e: e.tensor_scalar(out=sc_[:, 3:4], in0=sc_[:, 3:4], scalar1=1.0, scalar2=None,
                                                          op0=ALU.max), reads=[sc_], writes=[sc_])
                    S.op("dve", lambda e: e.reciprocal(out=sc_[:, 4:5], in_=sc_[:, 3:4]), reads=[sc_], writes=[sc_])
                    S.op("dve", lambda e: e.scalar_tensor_tensor(out=Hs[:, n, :], in0=tot_[:, 0:256], scalar=sc_[:, 4:5], in1=Hs[:, n, :],
                                                                 op0=ALU.mult, op1=ALU.add), reads=[tot_, sc_], writes=[Hs])

                stageA(0)
                for ux in range(len(units)):
                    if ux + 1 < len(units):
                        stageA(ux + 1)
                    stageB(ux)
                S.dma("sp", HS[:, hd * 256:(hd + 1) * 256].rearrange("(n p) d -> p n d", p=128), Hs[:], reads=[Hs], writes=[HS.b(hd)])
            S.barrier()
        if stop < 4:
            return
        with ExitStack() as es:
            hn = self.sb(es, "hn", [128, D], F32)
            S.dma("sp", hn[:], self.hnorm_d[o][:], reads=[self.hnorm_d[o]], writes=[hn])
            hsT = [self.sb(es, f"hsT{i}", [128, D], F32) for i in range(2)]
            osT = [self.sb(es, f"osT{i}", [128, D], BF16) for i in range(2)]
            w2 = [self.sb(es, f"w2{i}", [128, D], F32) for i in range(2)]
            hb_ = [self.sb(es, f"hbo{i}", [128, D], BF16) for i in range(2)]
            junk = self.sb(es, "junk", [128, 256], F32)
            ss = [self.sb(es, f"oss{i}", [128, 8], F32) for i in range(2)]
            mo = [self.sb(es, f"mo{i}", [128, 16, 128], BF16) for i in range(2)]
            MIXv = MIXT.h.rearrange("(kc p) t -> p kc t", p=128)
            for i in range(34):
                h_, o_, w_, hb2, s_, m_ = hsT[i % 2], osT[i % 2], w2[i % 2], hb_[i % 2], ss[i % 2], mo[i % 2]
                S.dma("sp", h_[:], HS[i * 128:(i + 1) * 128, :], reads=[HS], writes=[h_])
                S.dma("sp", o_[:], OS[i * 128:(i + 1) * 128, :], reads=[OS], writes=[o_])
                S.op("pool", lambda e: e.tensor_tensor(out=w_[:], in0=o_[:], in1=hn[:], op=ALU.mult), reads=[o_, hn], writes=[w_])
                for hd in range(8):
                    S.op("act", lambda e: e.activation(out=junk[:], in_=h_[:, hd * 256:(hd + 1) * 256], func=AF.Square,
                                                       accum_out=s_[:, hd:hd + 1]), reads=[h_], writes=[junk, s_])
                S.op("act", lambda e: e.activation(out=s_[:], in_=s_[:], func=AF.Sqrt, bias=self.epsT[:, 0:1], scale=1.0 / 256),
                     reads=[s_, self.epsT], writes=[s_])
                S.op("dve", lambda e: e.reciprocal(out=s_[:], in_=s_[:]), reads=[s_], writes=[s_])
                for hd in range(8):
                    S.op("dve", lambda e: e.scalar_tensor_tensor(out=hb2[:, hd * 256:(hd + 1) * 256], in0=h_[:, hd * 256:(hd + 1) * 256],
                                                                 scalar=s_[:, hd:hd + 1], in1=w_[:, hd * 256:(hd + 1) * 256],
                                                                 op0=ALU.mult, op1=ALU.mult), reads=[h_, s_, w_], writes=[hb2])
                for kg in range(2):
                    ps = self.psum()
                    psb = ps.h.bitcast(BF16)
                    for kk in range(8):
                        kc = kg * 8 + kk
                        S.op("pe", lambda e: e.transpose(out=psb[:, kk * 128:(kk + 1) * 128], in_=hb2[:, kc * 128:(kc + 1) * 128],
                                                         identity=self.IDB), reads=[hb2, self.CB], writes=[ps])
                    if kg == 0:
                        S.op("act", lambda e: e.copy(out=m_[:, 0:8, :].rearrange("p k t -> p (k t)"), in_=psb[:, 0:1024]), reads=[ps], writes=[m_])
                    else:
                        S.op("dve", lambda e: e.tensor_copy(out=m_[:, 8:16, :].rearrange("p k t -> p (k t)"), in_=psb[:, 0:1024]), reads=[ps], writes=[m_])
                S.dma("sp", MIXv[:, :, i * 128:(i + 1) * 128], m_[:], reads=[m_], writes=[MIXT.b(("o4", i))])
            S.barrier()
        if stop < 5:
            return
        self.out_proj_residual(l, 2, MIXT, self.od_w_out[o], XT)


N_CORES = 4


def kernel(**inp):
    inp = {k: np.asarray(v) for k, v in inp.items()}
    P = Prog()
    nc = P.build()
    cs = make_consts()
    shared = {"cb": cs["cb"], "ident_f": cs["ident_f"], "cosT": cs["cosT"], "sinT": cs["sinT"], "cf": cs["cf"],
              "ev_ra_w": np.ascontiguousarray(inp["ev_ra_w"], dtype=np.float32),
              "ev_ix_w": np.ascontiguousarray(inp["ev_ix_w"], dtype=np.float32)}
    for l in range(DEPTH):
        shared[f"ada_w{l}"] = np.ascontiguousarray(inp["ada_w"][l], dtype=np.float32)
        shared[f"moe_r{l}"] = np.ascontiguousarray(inp["moe_router"][l], dtype=np.float32)
        for hh in range(2):
            shared[f"wg{l}_{hh}"] = np.ascontiguousarray(inp["moe_w_gate"][l, hh * 8:(hh + 1) * 8], dtype=np.float32)
            shared[f"wu{l}_{hh}"] = np.ascontiguousarray(inp["moe_w_up"][l, hh * 8:(hh + 1) * 8], dtype=np.float32)
            shared[f"wd{l}_{hh}"] = np.ascontiguousarray(inp["moe_w_down"][l, hh * 8:(hh + 1) * 8], dtype=np.float32)
    for e in range(2):
        shared[f"ev_w_in{e}"] = np.ascontiguousarray(inp["ev_w_in"][e], dtype=np.float32)
        shared[f"ev_w_out{e}"] = np.ascontiguousarray(inp["ev_w_out"][e], dtype=np.float32)
        shared[f"od_w_in{e}"] = np.ascontiguousarray(inp["od_w_in"][e], dtype=np.float32)
        shared[f"od_w_out{e}"] = np.ascontiguousarray(inp["od_w_out"][e], dtype=np.float32)
        shared[f"hnorm_bc{e}"] = np.ascontiguousarray(
            np.broadcast_to(np.asarray(inp["od_hnorm_w"][e], np.float32)[None, :], (128, D)))
    in_maps = []
    for b in range(N_CORES):
        m = dict(shared)
        m["xT"] = np.ascontiguousarray(np.concatenate([inp["ctx"][b], inp["x"][b]], axis=0).T.astype(np.float32))
        m["vecs"] = pack_vecs(inp, b)
        in_maps.append(m)
    res = run_bass_kernel_spmd(nc, in_maps, core_ids=list(range(N_CORES)))
    return np.stack([np.asarray(res.results[b]["out"], dtype=np.float32) for b in range(N_CORES)], axis=0)
```

```python
import numpy as np
import concourse.bass as bass
import concourse.mybir as mybir
from concourse.bass_utils import run_bass_kernel_spmd
from contextlib import ExitStack

F32 = mybir.dt.float32
BF16 = mybir.dt.bfloat16
U32 = mybir.dt.uint32
I32 = mybir.dt.int32
ALU = mybir.AluOpType
AF = mybir.ActivationFunctionType
AX = mybir.AxisListType

D = 2048
KC = 16
CTX = 256
SEQ = 4096
T = CTX + SEQ
DEPTH = 4
NE = 16
DEXP = 1536
EPS = 1e-6
NQ = 6
import os
DBG = set(os.environ.get('K_DBG', '').split(','))
SAME_SYNC = os.environ.get("K_SAME", "1") == "1"


class Buf:
    __slots__ = ("w", "r", "excl")

    def __init__(self):
        self.w = {}
        self.r = {}
        self.excl = False


class TT:
    def __init__(self, h):
        self.h = h
        self.buf = Buf()
        self.sub = {}

    def __getitem__(self, idx):
        return self.h[idx]

    def b(self, key=None):
        if key is None:
            return self.buf
        s = self.sub.get(key)
        if s is None:
            s = self.sub[key] = Buf()
        return s


def _bufs(lst):
    out = []
    for x in lst:
        if isinstance(x, TT):
            out.append(x.buf)
        elif isinstance(x, Buf):
            out.append(x)
        elif isinstance(x, (list, tuple)):
            out.extend(_bufs(x))
        else:
            raise TypeError(type(x))
    return out


class Sched:
    def __init__(self, nc, es):
        self.nc = nc
        self.es = es
        self.engs = {"pe": nc.tensor, "act": nc.scalar, "dve": nc.vector, "pool": nc.gpsimd, "sp": nc.sync}
        self.semh = {}
        self.ecnt = {}
        for k in ("pe", "act", "dve", "pool"):
            self.semh["E_" + k] = es.enter_context(nc.semaphore("sem_" + k))
            self.ecnt[k] = 0
        self.rings = {}
        for q in ("sp", "act", "pool"):
            self.rings[q] = {"n": 0, "val": [0] * NQ}
            for i in range(NQ):
                self.semh[f"D_{q}_{i}"] = es.enter_context(nc.semaphore(f"dq_{q}_{i}"))
        self.waited = {k: {} for k in self.engs}
        self.n_ops = 0

    def _deps(self, reads, writes):
        deps = {}
        for b in reads:
            for k, v in b.w.items():
                if deps.get(k, 0) < v:
                    deps[k] = v
        for b in writes:
            for k, v in b.w.items():
                if deps.get(k, 0) < v:
                    deps[k] = v
            for k, v in b.r.items():
                if deps.get(k, 0) < v:
                    deps[k] = v
        return deps

    def _wait(self, engname, deps, skip=None):
        e = self.engs[engname]
        wd = self.waited[engname]
        for k, v in deps.items():
            if k == skip:
                continue
            if wd.get(k, 0) < v:
                e.wait_ge(self.semh[k], v)
                wd[k] = v

    def _mark(self, key, v, reads, writes):
        for b in writes:
            b.w = {key: v}
            b.r = {}
        for b in reads:
            if b.r.get(key, 0) < v:
                b.r[key] = v

    def op(self, engname, fn, reads=(), writes=()):
        reads = _bufs(reads)
        writes = _bufs(writes)
        for b in reads:
            if b.excl and b not in writes:
                writes.append(b)
        deps = self._deps(reads, writes)
        key = "E_" + engname
        skip = key if (engname == "pe" or not SAME_SYNC) else None
        self._wait(engname, deps, skip)
        ins = fn(self.engs[engname])
        self.ecnt[engname] += 1
        v = self.ecnt[engname]
        ins.then_inc(self.semh[key], 1)
        self._mark(key, v, [b for b in reads if b not in writes], writes)
        self.n_ops += 1
        return ins

    def dma(self, q, out, in_, reads=(), writes=(), fn=None, **kw):
        reads = _bufs(reads)
        writes = _bufs(writes)
        ring = self.rings[q]
        i = ring["n"] % NQ
        ring["n"] += 1
        key = f"D_{q}_{i}"
        deps = self._deps(reads, writes)
        prev = ring["val"][i]
        if prev and deps.get(key, 0) < prev:
            deps[key] = prev
        self._wait(q, deps)
        e = self.engs[q]
        if fn is not None:
            ins = fn(e)
        else:
            ins = e.dma_start(out=out, in_=in_, **kw)
        ins.then_inc(self.semh[key], 16)
        v = prev + 16
        ring["val"][i] = v
        self._mark(key, v, [b for b in reads if b not in writes], writes)
        self.n_ops += 1
        return ins

    def barrier(self):
        deps = {}
        for k in ("pe", "act", "dve", "pool"):
            if self.ecnt[k]:
                deps["E_" + k] = self.ecnt[k]
        for q, ring in self.rings.items():
            for i in range(NQ):
                if ring["val"][i]:
                    deps[f"D_{q}_{i}"] = ring["val"][i]
        for e in self.engs:
            self._wait(e, deps)


def _vec_layout():
    off = {}
    n = 0

    def add(name, cols):
        nonlocal n
        off[name] = n
        n += cols

    add("c", 16)
    add("cctx", 16)
    for l in range(DEPTH):
        add(f"ada_b{l}", 96)
        add(f"nmix{l}", 16)
        add(f"nffn{l}", 16)
    add("fnorm", 16)
    for e in range(2):
        add(f"ev_cw{e}", 32)
        add(f"ev_cb{e}", 8)
        add(f"ev_rab{e}", 16)
        add(f"ev_ixb{e}", 16)
        add(f"ev_lam{e}", 16)
        add(f"ev_sink{e}", 8)
    for o in range(2):
        add(f"od_cw{o}", 64)
        add(f"od_cb{o}", 16)
        add(f"od_gb{o}", 32)
    return off, n


VOFF, NV = _vec_layout()


def _pm(v):
    v = np.asarray(v, np.float32).reshape(-1, 128)
    return np.ascontiguousarray(v.T)


def pack_vecs(inp, b):
    V = np.zeros((128, NV), np.float32)

    def put(name, arr):
        arr = np.asarray(arr, np.float32)
        V[: arr.shape[0], VOFF[name]: VOFF[name] + arr.shape[1]] = arr

    put("c", _pm(inp["c"][b]))
    put("cctx", _pm(inp["c_ctx"]))
    for l in range(DEPTH):
        put(f"ada_b{l}", _pm(inp["ada_b"][l]))
        put(f"nmix{l}", _pm(inp["norm_mix_w"][l]))
        put(f"nffn{l}", _pm(inp["norm_ffn_w"][l]))
    put("fnorm", _pm(inp["final_norm_w"]))
    for e in range(2):
        put(f"ev_cw{e}", np.concatenate([_pm(inp["ev_conv_w"][e, j]) for j in range(4)], axis=1))
        put(f"ev_cb{e}", _pm(inp["ev_conv_b"][e]))
        put(f"ev_rab{e}", np.concatenate([_pm(inp["ev_ra_b"][e, d].reshape(-1)) for d in range(2)], axis=1))
        put(f"ev_ixb{e}", np.concatenate([_pm(inp["ev_ix_b"][e, d].reshape(-1)) for d in range(2)], axis=1))
        put(f"ev_lam{e}", np.concatenate([_pm(inp["ev_lambda"][e, d]) for d in range(2)], axis=1))
        put(f"ev_sink{e}", np.broadcast_to(np.asarray(inp["ev_sink"][e], np.float32)[None, :], (128, 8)))
    for o in range(2):
        put(f"od_cw{o}", np.concatenate([_pm(inp["od_conv_w"][o, j]) for j in range(4)], axis=1))
        put(f"od_cb{o}", _pm(inp["od_conv_b"][o]))
        put(f"od_gb{o}", np.broadcast_to(np.asarray(inp["od_gate_b"][o], np.float32).reshape(1, 32), (128, 32)))
    return V


def make_consts():
    import ml_dtypes
    bf = ml_dtypes.bfloat16
    c = {}
    c["ident_f"] = np.eye(128, dtype=np.float32)
    cb = np.zeros((128, 6, 128), np.float32)
    cb[:, 0, :] = np.eye(128)
    cb[:, 1, :] = 1.0
    j = np.arange(128)[:, None]
    i = np.arange(128)[None, :]
    cb[:, 2, :] = (j >= i)
    cb[:, 3, :] = (j <= i)
    R = np.zeros((128, 128), np.float32)
    for d in range(128):
        p = d + 32 if (d % 64) < 32 else d - 32
        R[p, d] = 1.0
    cb[:, 4, :] = R
    c["cb"] = cb.astype(bf)
    pairs = 32
    inv = np.power(10000.0, -np.arange(pairs, dtype=np.float32) / pairs).astype(np.float32)
    t = np.arange(SEQ)
    row = (t // 64).astype(np.float32)
    col = (t % 64).astype(np.float32)
    ra = (row[:, None] * inv).astype(np.float32)
    ca = (col[:, None] * inv).astype(np.float32)
    cosT = np.zeros((128, SEQ), np.float32)
    sinT = np.zeros((128, SEQ), np.float32)
    cosT[0:32] = np.cos(ra).T
    cosT[32:64] = np.cos(ra).T
    cosT[64:96] = np.cos(ca).T
    cosT[96:128] = np.cos(ca).T
    sinT[0:32] = -np.sin(ra).T
    sinT[32:64] = np.sin(ra).T
    sinT[64:96] = -np.sin(ca).T
    sinT[96:128] = np.sin(ca).T
    c["cosT"] = cosT
    c["sinT"] = sinT
    cf = np.zeros((128, 5, 128), np.float32)
    cf[:, 0, :] = (j <= i)
    cf[:, 1, :] = (j >= i)
    cf[:, 2, :] = np.where(j <= i, 0.0, -30000.0)
    cf[:, 3, :] = np.where(j >= i, 0.0, -30000.0)
    cf[:, 4, :] = 1.0
    c["cf"] = cf
    return c


TBLOCKS = [(0, 256)] + [(256 + 512 * i, 512) for i in range(8)]


class Prog:
    def __init__(self, dbg_in=(), dbg_out=()):
        self.nc = bass.Bass("TRN2", target_bir_lowering=False)
        self.es = ExitStack()
        self.S = Sched(self.nc, self.es)
        self.dbg_in = set(dbg_in)
        self.dbg_out = set(dbg_out)
        self.ext = {}
        nc = self.nc
        self.ps = [TT(self.es.enter_context(nc.psum_tensor(f"ps{i}", [128, 512], F32))) for i in range(8)]
        for p_ in self.ps:
            p_.buf.excl = True
        self.psi = 0
        self.uid = 0

    def dram_in(self, name, shape, dtype=F32):
        t = TT(self.nc.dram_tensor(name, list(shape), dtype, kind="ExternalInput").ap())
        self.ext[name] = t
        return t

    def dram_out(self, name, shape, dtype=F32):
        t = TT(self.nc.dram_tensor(name, list(shape), dtype, kind="ExternalOutput").ap())
        self.ext[name] = t
        return t

    def dram(self, name, shape, dtype):
        kind = "ExternalInput" if name in self.dbg_in else ("ExternalOutput" if name in self.dbg_out else "Internal")
        t = TT(self.nc.dram_tensor(name, list(shape), dtype, kind=kind).ap())
        self.ext[name] = t
        return t

    def sb(self, es, name, shape, dtype):
        self.uid += 1
        return TT(es.enter_context(self.nc.sbuf_tensor(f"{name}_{self.uid}", list(shape), dtype)))

    def psum(self):
        p = self.ps[self.psi % 8]
        self.psi += 1
        return p

    def setup(self):
        S = self.S
        es = self.es
        self.vecs_d = self.dram_in("vecs", [128, NV])
        self.cb_d = self.dram_in("cb", [128, 6, 128], BF16)
        self.identf_d = self.dram_in("ident_f", [128, 128])
        self.V = self.sb(es, "V", [128, NV], F32)
        self.CB = self.sb(es, "CB", [128, 6, 128], BF16)
        self.IDF = self.sb(es, "IDF", [128, 128], F32)
        self.MOD = self.sb(es, "MOD", [128, DEPTH, 96, 2], F32)
        self.epsT = self.sb(es, "epsT", [128, 1], F32)
        S.dma("sp", self.V[:], self.vecs_d[:], reads=[self.vecs_d], writes=[self.V])
        S.dma("sp", self.CB[:], self.cb_d[:], reads=[self.cb_d], writes=[self.CB])
        S.dma("sp", self.IDF[:], self.identf_d[:], reads=[self.identf_d], writes=[self.IDF])
        S.op("dve", lambda e: e.memset(self.epsT[:], EPS), writes=[self.epsT])
        self.IDB = self.CB[:, 0, :]
        self.ONESB = self.CB[:, 1, :]

    def vcol(self, name, c0=0, n=1):
        o = VOFF[name] + c0
        return self.V[:, o:o + n]

    def phase_mod(self, ada_w, layers):
        S = self.S
        with ExitStack() as es:
            s2 = self.sb(es, "s2", [128, 16, 2], F32)
            S.op("act", lambda e: e.activation(out=s2[:, :, 0], in_=self.vcol("c", 0, 16), func=AF.Silu),
                 reads=[self.V], writes=[s2])
            S.op("act", lambda e: e.activation(out=s2[:, :, 1], in_=self.vcol("cctx", 0, 16), func=AF.Silu),
                 reads=[self.V], writes=[s2])
            wt = [self.sb(es, f"adaw{i}", [128, 16, 1024], F32) for i in range(2)]
            it = 0
            for l in layers:
                W = ada_w[l]
                acc = self.psum()
                for g in range(12):
                    w = wt[it % 2]
                    it += 1
                    for kc in range(16):
                        S.dma("sp", w[:, kc, :], W[kc * 128:(kc + 1) * 128, g * 1024:(g + 1) * 1024],
                              reads=[W], writes=[w])
                    for sub in range(8):
                        col = (g * 8 + sub) * 2
                        for kc in range(16):
                            S.op("pe", lambda e, kc=kc, sub=sub, col=col, w=w: e.matmul(
                                acc[:, col:col + 2], lhsT=w[:, kc, sub * 128:(sub + 1) * 128], rhs=s2[:, kc, :],
                                start=(kc == 0), stop=(kc == 15)), reads=[w, s2], writes=[acc])
                accv = acc[:, 0:192].rearrange("p (n w) -> p n w", w=2)
                for wh in range(2):
                    S.op("dve", lambda e, wh=wh, l=l, accv=accv: e.tensor_tensor(
                        out=self.MOD[:, l, :, wh], in0=accv[:, :, wh], in1=self.vcol(f"ada_b{l}", 0, 96), op=ALU.add),
                        reads=[acc, self.V], writes=[self.MOD])
            S.barrier()

    def mod(self, l, j, kc, wh):
        return self.MOD[:, l, j * 16 + kc, wh:wh + 1]

    def phase_norm(self, XT, l, jshift, nwname, consume, es, want_f32=False):
        S = self.S
        A = self.sb(es, "A", [128, 16, 2], F32)
        for wh in range(2):
            S.op("dve", lambda e, wh=wh: e.scalar_tensor_tensor(
                out=A[:, :, wh], in0=self.MOD[:, l, (jshift + 1) * 16:(jshift + 2) * 16, wh], scalar=1.0,
                in1=self.vcol(nwname, 0, 16), op0=ALU.add, op1=ALU.mult), reads=[self.MOD, self.V], writes=[A])
        xts = [self.sb(es, f"xt{i}", [128, 16, 512], F32) for i in range(2)]
        sqs = [self.sb(es, f"sq{i}", [128, 16, 512], BF16) for i in range(1)]
        rstd = [self.sb(es, f"rstd{i}", [128, 512], F32) for i in range(2)]
        hbs = [self.sb(es, f"hb{i}", [128, 16, 512], BF16) for i in range(2)]
        XTv = XT.h.rearrange("(kc p) t -> p kc t", p=128)
        for tb, (t0, tn) in enumerate(TBLOCKS):
            wh = 1 if tb == 0 else 0
            xt = xts[tb % 2]
            sq = sqs[0]
            rs = rstd[tb % 2]
            hb = hbs[tb % 2]
            xk = [xt.b(kc) for kc in range(16)]
            hk = [hb.b(kc) for kc in range(16)]
            for whole, parts in ((xt, xk), (hb, hk)):
                for pb in parts:
                    for k_, v_ in whole.buf.w.items():
                        if pb.w.get(k_, 0) < v_:
                            pb.w[k_] = v_
                    for k_, v_ in whole.buf.r.items():
                        if pb.r.get(k_, 0) < v_:
                            pb.r[k_] = v_
            S.dma("sp", xt[:, :, :tn], XTv[:, :, t0:t0 + tn], reads=[XT.b((kc, tb)) for kc in range(16)], writes=[xt] + xk)
            S.op("act", lambda e: e.activation(out=sq[:, :, :tn], in_=xt[:, :, :tn], func=AF.Square),
                 reads=[xt] + xk, writes=[sq])
            acc = self.psum()
            for kc in range(16):
                S.op("pe", lambda e, kc=kc: e.matmul(acc[:, :tn], lhsT=self.ONESB, rhs=sq[:, kc, :tn],
                                                      start=(kc == 0), stop=(kc == 15)),
                     reads=[sq, self.CB], writes=[acc])
            S.op("act", lambda e: e.activation(out=rs[:, :tn], in_=acc[:, :tn], func=AF.Sqrt,
                                               bias=self.epsT[:, 0:1], scale=1.0 / D),
                 reads=[acc, self.epsT], writes=[rs])
            S.op("dve", lambda e: e.reciprocal(out=rs[:, :tn], in_=rs[:, :tn]), reads=[rs], writes=[rs])
            hf = xt if want_f32 else None
            for kc in range(16):
                S.op("dve", lambda e, kc=kc: e.scalar_tensor_tensor(
                    out=xt[:, kc, :tn], in0=xt[:, kc, :tn], scalar=A[:, kc, wh:wh + 1], in1=rs[:, :tn],
                    op0=ALU.mult, op1=ALU.mult), reads=[A, rs], writes=[xk[kc]])
            for kc in range(16):
                S.op("act", lambda e, kc=kc: e.activation(
                    out=hb[:, kc, :tn], in_=xt[:, kc, :tn], func=AF.Identity,
                    bias=self.mod(l, jshift, kc, wh), scale=1.0), reads=[xk[kc], self.MOD], writes=[hk[kc]])
                if want_f32:
                    S.op("act", lambda e, kc=kc: e.activation(
                        out=xt[:, kc, :tn], in_=xt[:, kc, :tn], func=AF.Identity,
                        bias=self.mod(l, jshift, kc, wh), scale=1.0), reads=[self.MOD], writes=[xk[kc]])
            for whole, parts in ((xt, xk), (hb, hk)):
                for pb in parts:
                    for k_, v_ in pb.w.items():
                        if whole.buf.w.get(k_, 0) < v_:
                            whole.buf.w[k_] = v_
                    for k_, v_ in pb.r.items():
                        if whole.buf.r.get(k_, 0) < v_:
                            whole.buf.r[k_] = v_
            consume(tb, t0, tn, hb, hf)

    def linear(self, es, AT, W, Wap, K, c0, c1, mode, epilogue, tblocks=None, SW=1024):
        S = self.S
        KCn = K // 128
        es = ExitStack()
        wts = [self.sb(es, f"lw{i}", [128, KCn, SW], BF16) for i in range(2)]
        ats = [self.sb(es, f"la{i}", [128, KCn, 512], BF16) for i in range(2)]
        ATv = AT.h.rearrange("(kc p) t -> p kc t", p=128)
        Wv = Wap.rearrange("(kc p) n -> p kc n", p=128)
        tbl = list(enumerate(TBLOCKS)) if tblocks is None else tblocks
        wi = 0
        ai = 0
        for cs in range(c0, c1, SW):
            ncol = min(SW, c1 - cs)
            wt = wts[wi % 2]
            wi += 1
            for kc in range(KCn):
                for h0 in range(0, ncol, 512):
                    hn = min(512, ncol - h0)
                    S.dma("pool", wt[:, kc, h0:h0 + hn], Wv[:, kc, cs + h0:cs + h0 + hn], reads=[W], writes=[wt])
            for tb, (t0, tn) in tbl:
                at = ats[ai % 2]
                ai += 1
                S.dma("sp", at[:, :, :tn], ATv[:, :, t0:t0 + tn], reads=[AT.b(tb)], writes=[at])
                if mode == "fm":
                    for sub in range(ncol // 128):
                        ps = self.psum()
                        for kc in range(KCn):
                            S.op("pe", lambda e, kc=kc, sub=sub, ps=ps, wt=wt, at=at, tn=tn: e.matmul(
                                ps[:, :tn], lhsT=wt[:, kc, sub * 128:(sub + 1) * 128], rhs=at[:, kc, :tn],
                                start=(kc == 0), stop=(kc == KCn - 1)), reads=[wt, at], writes=[ps])
                        epilogue(ps, cs + sub * 128, tb, t0, tn)
                else:
                    for s_ in range(tn // 128):
                        for h0 in range(0, ncol, 512):
                            hn = min(512, ncol - h0)
                            ps = self.psum()
                            for kc in range(KCn):
                                S.op("pe", lambda e, kc=kc, s_=s_, ps=ps, wt=wt, at=at, hn=hn, h0=h0: e.matmul(
                                    ps[:, :hn], lhsT=at[:, kc, s_ * 128:(s_ + 1) * 128], rhs=wt[:, kc, h0:h0 + hn],
                                    start=(kc == 0), stop=(kc == KCn - 1)), reads=[wt, at], writes=[ps])
                            epilogue(ps, cs + h0, hn, tb, t0 + s_ * 128)
        S.barrier()
        es.close()

    def even_proj(self, l, e, w_in, HT, XA, YG, QK, VV, cos_d, sin_d):
        S = self.S
        with ExitStack() as es:
            cosS = self.sb(es, "cosS", [128, SEQ], F32)
            sinS = self.sb(es, "sinS", [128, SEQ], F32)
            S.dma("sp", cosS[:], cos_d[:], reads=[cos_d], writes=[cosS])
            S.dma("sp", sinS[:], sin_d[:], reads=[sin_d], writes=[sinS])
            st_f = [self.sb(es, f"stf{i}", [128, 512], F32) for i in range(2)]
            st_g = [self.sb(es, f"stg{i}", [128, 512], F32) for i in range(2)]
            st_b = [self.sb(es, f"stb{i}", [128, 512], BF16) for i in range(2)]
            st_o = [self.sb(es, f"sto{i}", [128, 512], BF16) for i in range(2)]
            cnt = [0]
            R = self.CB[:, 4, :]

            def epi(ps, col, tb, t0, tn):
                i = cnt[0] % 2
                cnt[0] += 1
                sf, sg, sbb, so = st_f[i], st_g[i], st_b[i], st_o[i]
                if 'noepi' in DBG:
                    S.op("act", lambda e_: e_.copy(out=sf[:, :tn], in_=ps[:, :tn]), reads=[ps], writes=[sf])
                    return
                if ('onlyxa' in DBG and col >= 1024) or ('onlyya' in DBG and not (1024 <= col < 2048)) or ('onlyqk' in DBG and col < 2048):
                    S.op("act", lambda e_: e_.copy(out=sf[:, :tn], in_=ps[:, :tn]), reads=[ps], writes=[sf])
                    return
                if col < 1024:
                    S.op("act", lambda e_: e_.copy(out=sf[:, :tn], in_=ps[:, :tn]), reads=[ps], writes=[sf])
                    S.dma("pool", XA[col:col + 128, t0:t0 + tn], sf[:, :tn], reads=[sf], writes=[XA.b((col // 128, tb))])
                elif col < 2048:
                    c = col - 1024
                    S.op("act", lambda e_: e_.activation(out=sf[:, :tn], in_=ps[:, :tn], func=AF.Square),
                         reads=[ps], writes=[sf])
                    S.op("dve", lambda e_: e_.tensor_scalar(out=sf[:, :tn], in0=sf[:, :tn], scalar1=0.044715,
                                                            scalar2=1.0, op0=ALU.mult, op1=ALU.add),
                         reads=[sf], writes=[sf])
                    S.op("dve", lambda e_: e_.tensor_tensor(out=sf[:, :tn], in0=ps[:, :tn], in1=sf[:, :tn], op=ALU.mult),
                         reads=[ps, sf], writes=[sf])
                    S.op("act", lambda e_: e_.activation(out=sg[:, :tn], in_=sf[:, :tn], func=AF.Sigmoid,
                                                         scale=1.5957691216057308), reads=[sf], writes=[sg])
                    S.op("dve", lambda e_: e_.tensor_tensor(out=so[:, :tn], in0=ps[:, :tn], in1=sg[:, :tn], op=ALU.mult),
                         reads=[ps, sg], writes=[so])
                    S.dma("pool", YG[c:c + 128, t0:t0 + tn], so[:, :tn], reads=[so], writes=[YG.b((c // 128, tb))])
                else:
                    c = col - 2048
                    if tb == 0:
                        S.op("act", lambda e_: e_.copy(out=so[:, :tn], in_=ps[:, :tn]), reads=[ps], writes=[so])
                    else:
                        p0 = t0 - CTX
                        S.op("act", lambda e_: e_.copy(out=sbb[:, :tn], in_=ps[:, :tn]), reads=[ps], writes=[sbb])
                        ps2 = self.psum()
                        S.op("pe", lambda e_: e_.matmul(ps2[:, :tn], lhsT=R, rhs=sbb[:, :tn], start=True, stop=True),
                             reads=[sbb, self.CB], writes=[ps2])
                        S.op("dve", lambda e_: e_.tensor_tensor(out=sf[:, :tn], in0=ps[:, :tn], in1=cosS[:, p0:p0 + tn],
                                                                op=ALU.mult), reads=[ps, cosS], writes=[sf])
                        S.op("dve", lambda e_: e_.tensor_tensor(out=sg[:, :tn], in0=ps2[:, :tn], in1=sinS[:, p0:p0 + tn],
                                                                op=ALU.mult), reads=[ps2, sinS], writes=[sg])
                        S.op("pool", lambda e_: e_.tensor_tensor(out=so[:, :tn], in0=sf[:, :tn], in1=sg[:, :tn], op=ALU.add),
                             reads=[sf, sg], writes=[so])
                    S.dma("pool", QK[c:c + 128, t0:t0 + tn], so[:, :tn], reads=[so], writes=[QK.b((c // 128, tb))])

            self.linear(es, HT, w_in, w_in[:], D, 0, 3328, "fm", epi)

            def epi_v(ps, cs, ncol, tb, tok0):
                i = cnt[0] % 2
                cnt[0] += 1
                so = st_o[i]
                S.op("act", lambda e_: e_.copy(out=so[:, :ncol], in_=ps[:, :ncol]), reads=[ps], writes=[so])
                S.dma("pool", VV[tok0:tok0 + 128, :], so[:, :ncol], reads=[so], writes=[VV.b(tok0 // 128)])

            if 'nov' not in DBG:
                self.linear(es, HT, w_in, w_in[:], D, 3328, 3584, "tm", epi_v)
            S.barrier()

    def even_lru(self, e, raw_d, ixw_d, XA, YG, MIXT):
        S = self.S
        with ExitStack() as es:
            cn = self.sb(es, "cn", [128, 16], F32)
            cn2 = self.sb(es, "cn2", [128, 16], F32)
            S.op("act", lambda e_: e_.activation(out=cn[:], in_=self.vcol(f"ev_lam{e}", 0, 16), func=AF.Exp, scale=-1.0),
                 reads=[self.V], writes=[cn])
            S.op("act", lambda e_: e_.activation(out=cn[:], in_=cn[:], func=AF.Ln, bias=1.0, scale=1.0),
                 reads=[cn], writes=[cn])
            S.op("dve", lambda e_: e_.tensor_scalar(out=cn2[:], in0=cn[:], scalar1=-16.0, scalar2=None, op0=ALU.mult),
                 reads=[cn], writes=[cn2])
            S.op("dve", lambda e_: e_.tensor_scalar(out=cn[:], in0=cn[:], scalar1=-8.0, scalar2=None, op0=ALU.mult),
                 reads=[cn], writes=[cn])
            gw = self.sb(es, "gw", [128, 2, 2, 8, 128], BF16)
            for gi, wd in enumerate((raw_d, ixw_d)):
                for d in range(2):
                    S.dma("pool", gw[:, gi, d, :, :], wd[e, d].rearrange("h i j -> i h j"), reads=[wd], writes=[gw])
            B = [self.sb(es, f"lru{i}", [128, T], F32) for i in range(7)]
            xab = self.sb(es, "xab", [128, T], BF16)
            ygt = self.sb(es, "ygt", [128, T], BF16)
            outb = self.sb(es, "outb", [128, T], BF16)
            xp, xa, Rb, Ib, Ab, HF, HB = B
            segs = [(0, CTX), (CTX, T)]
            for h in range(8):
                S.dma("sp", xp[:], XA[h * 128:(h + 1) * 128, :], reads=[XA.b((h, tb)) for tb in range(9)], writes=[xp])
                S.dma("sp", ygt[:], YG[h * 128:(h + 1) * 128, :], reads=[YG.b((h, tb)) for tb in range(9)], writes=[ygt])
                cw = lambda j: self.vcol(f"ev_cw{e}", j * 8 + h, 1)
                for (a0, a1) in segs:
                    S.op("dve", lambda e_, a0=a0, a1=a1: e_.tensor_scalar(
                        out=xa[:, a0:a1], in0=xp[:, a0:a1], scalar1=cw(2), scalar2=self.vcol(f"ev_cb{e}", h, 1),
                        op0=ALU.mult, op1=ALU.add), reads=[xp, self.V], writes=[xa])
                    for j, off in ((0, -2), (1, -1), (3, 1)):
                        lo = max(a0, a0 - off)
                        hi = min(a1, a1 - off)
                        S.op("dve", lambda e_, j=j, off=off, lo=lo, hi=hi: e_.scalar_tensor_tensor(
                            out=xa[:, lo:hi], in0=xp[:, lo + off:hi + off], scalar=cw(j), in1=xa[:, lo:hi],
                            op0=ALU.mult, op1=ALU.add), reads=[xp, self.V], writes=[xa])
                S.op("pool", lambda e_: e_.tensor_copy(out=xab[:], in_=xa[:]), reads=[xa], writes=[xab])
                for d in range(2):
                    col = d * 8 + h
                    for gi, dst, bname in ((0, Rb, f"ev_rab{e}"), (1, Ib, f"ev_ixb{e}")):
                        for (t0, tn) in TBLOCKS:
                            ps = self.psum()
                            S.op("pe", lambda e_, ps=ps, gi=gi, t0=t0, tn=tn: e_.matmul(
                                ps[:, :tn], lhsT=gw[:, gi, d, h, :], rhs=xab[:, t0:t0 + tn], start=True, stop=True),
                                reads=[gw, xab], writes=[ps])
                            S.op("act", lambda e_, ps=ps, dst=dst, t0=t0, tn=tn, bname=bname: e_.activation(
                                out=dst[:, t0:t0 + tn], in_=ps[:, :tn], func=AF.Sigmoid,
                                bias=self.vcol(bname, col, 1), scale=1.0), reads=[ps, self.V], writes=[dst])
                    S.op("act", lambda e_: e_.activation(out=Ab[:], in_=Rb[:], func=AF.Exp, scale=cn[:, col:col + 1]),
                         reads=[Rb, cn], writes=[Ab])
                    S.op("act", lambda e_: e_.activation(out=xp[:], in_=Rb[:], func=AF.Exp, scale=cn2[:, col:col + 1]),
                         reads=[Rb, cn2], writes=[xp])
                    S.op("dve", lambda e_: e_.tensor_scalar(out=xp[:], in0=xp[:], scalar1=-1.0, scalar2=1.0,
                                                            op0=ALU.mult, op1=ALU.add), reads=[xp], writes=[xp])
                    S.op("act", lambda e_: e_.activation(out=xp[:], in_=xp[:], func=AF.Sqrt), reads=[xp], writes=[xp])
                    S.op("pool", lambda e_: e_.tensor_tensor(out=Ib[:], in0=Ib[:], in1=xa[:], op=ALU.mult),
                         reads=[xa], writes=[Ib])
                    S.op("pool", lambda e_: e_.tensor_tensor(out=Ib[:], in0=Ib[:], in1=xp[:], op=ALU.mult),
                         reads=[xp], writes=[Ib])
                    if d == 0:
                        S.op("dve", lambda e_: e_.tensor_tensor_scan(
                            out=HF[:], data0=Ab[:], data1=Ib[:], initial=0.0, op0=ALU.mult, op1=ALU.add),
                            reads=[Ab, Ib], writes=[HF])
                    else:
                        S.op("dve", lambda e_: e_.tensor_tensor_scan(
                            out=HB[:, 0:CTX][:, ::-1], data0=Ab[:, 0:CTX][:, ::-1], data1=Ib[:, 0:CTX][:, ::-1],
                            initial=0.0, op0=ALU.mult, op1=ALU.add), reads=[Ab, Ib], writes=[HB])
                        S.op("dve", lambda e_: e_.tensor_tensor_scan(
                            out=HB[:, CTX:T][:, ::-1], data0=Ab[:, CTX:T][:, ::-1], data1=Ib[:, CTX:T][:, ::-1],
                            initial=HB[:, 0:1], op0=ALU.mult, op1=ALU.add), reads=[Ab, Ib], writes=[HB])
                S.op("pool", lambda e_: e_.tensor_tensor(out=HF[:], in0=HF[:], in1=HB[:], op=ALU.add),
                     reads=[HB], writes=[HF])
                S.op("pool", lambda e_: e_.tensor_tensor(out=outb[:], in0=HF[:], in1=ygt[:], op=ALU.mult),
                     reads=[HF, ygt], writes=[outb])
                S.dma("sp", MIXT[h * 128:(h + 1) * 128, :], outb[:], reads=[outb],
                      writes=[MIXT.b((h, tb)) for tb in range(9)])
            S.barrier()

    def even_attn(self, e, QK, VV, MIXT):
        S = self.S
        scale = 128.0 ** -0.5
        with ExitStack() as es:
            SE = self.sb(es, "SE", [128, 8], F32)
            S.op("act", lambda e_: e_.activation(out=SE[:], in_=self.vcol(f"ev_sink{e}", 0, 8), func=AF.Exp),
                 reads=[self.V], writes=[SE])
            MK = self.sb(es, "MK", [128, 2, 4, 128], BF16)
            for mi in range(2):
                for g in range(4):
                    S.op("pool", lambda e_, mi=mi, g=g: e_.tensor_copy(out=MK[:, mi, g, :], in_=self.CB[:, 2 + mi, :]),
                         reads=[self.CB], writes=[MK])
            KT = self.sb(es, "KT", [128, T], BF16)
            Vt = self.sb(es, "Vt", [128, 34, 128], BF16)
            QT = self.sb(es, "QT", [128, 4, T], BF16)
            OUT = self.sb(es, "OUT", [128, 4, T], BF16)
            ESB = [self.sb(es, f"esb{i}", [128, 5, 512], BF16) for i in range(2)]
            TMP = [self.sb(es, f"atmp{i}", [128, 512], F32) for i in range(2)]
            allqk = lambda c: [QK.b((c, tb)) for tb in range(9)]
            for kv in range(2):
                S.dma("sp", KT[:], QK[(8 + kv) * 128:(9 + kv) * 128, :], reads=allqk(8 + kv), writes=[KT])
                S.dma("sp", Vt[:], VV[:, kv * 128:(kv + 1) * 128].rearrange("(n p) d -> p n d", p=128),
                      reads=[VV.b(i) for i in range(34)], writes=[Vt])
                for g in range(4):
                    S.dma("sp", QT[:, g, :], QK[(kv * 4 + g) * 128:(kv * 4 + g + 1) * 128, :],
                          reads=allqk(kv * 4 + g), writes=[QT])
                for qb in range(34):
                    t0 = qb * 128
                    if qb < 2:
                        kts = [(0, None), (1, None)]
                    else:
                        kts = [(0, None), (1, None)]
                        if qb - 2 >= 1:
                            kts.append((qb - 1, 0))
                        kts.append((qb, None))
                        if qb - 2 <= 30:
                            kts.append((qb + 1, 1))
                    esb = ESB[qb % 2]
                    tmp = TMP[qb % 2]
                    nk = len(kts)
                    for i, (kt, mk) in enumerate(kts):
                        ps = self.psum()
                        S.op("pe", lambda e_, ps=ps, kt=kt: e_.matmul(
                            ps[:, 0:512].rearrange("p (g q) -> p g q", g=4), lhsT=KT[:, kt * 128:(kt + 1) * 128],
                            rhs=QT[:, :, t0:t0 + 128], start=True, stop=True), reads=[KT, QT], writes=[ps])
                        S.op("act", lambda e_, ps=ps, i=i: e_.activation(out=esb[:, i, :], in_=ps[:, 0:512], func=AF.Exp,
                                                                       scale=scale), reads=[ps], writes=[esb])
                        if mk is not None:
                            S.op("pool", lambda e_, i=i, mk=mk: e_.tensor_tensor(
                                out=esb[:, i, :], in0=esb[:, i, :], in1=MK[:, mk].rearrange("p g q -> p (g q)"),
                                op=ALU.mult), reads=[MK], writes=[esb])
                    psd = self.psum()
                    for i in range(nk):
                        S.op("pe", lambda e_, i=i: e_.matmul(psd[:, 0:512], lhsT=self.ONESB, rhs=esb[:, i, :],
                                                             start=(i == 0), stop=(i == nk - 1)),
                             reads=[esb, self.CB], writes=[psd])
                    pso = self.psum()
                    for i, (kt, mk) in enumerate(kts):
                        S.op("pe", lambda e_, i=i, kt=kt: e_.matmul(pso[:, 0:512], lhsT=Vt[:, kt, :], rhs=esb[:, i, :],
                                                                     start=(i == 0), stop=(i == nk - 1)),
                             reads=[esb, Vt], writes=[pso])
                    for g in range(4):
                        S.op("dve", lambda e_, g=g: e_.tensor_scalar(
                            out=tmp[:, g * 128:(g + 1) * 128], in0=psd[:, g * 128:(g + 1) * 128],
                            scalar1=SE[:, kv * 4 + g:kv * 4 + g + 1], scalar2=None, op0=ALU.add),
                            reads=[psd, SE], writes=[tmp])
                    S.op("dve", lambda e_: e_.reciprocal(out=tmp[:], in_=tmp[:]), reads=[tmp], writes=[tmp])
                    S.op("dve", lambda e_: e_.tensor_tensor(
                        out=OUT[:, :, t0:t0 + 128], in0=pso[:, 0:512].rearrange("p (g q) -> p g q", g=4),
                        in1=tmp[:].rearrange("p (g q) -> p g q", g=4), op=ALU.mult), reads=[pso, tmp], writes=[OUT])
                for g in range(4):
                    hh = 8 + kv * 4 + g
                    S.dma("sp", MIXT[hh * 128:(hh + 1) * 128, :], OUT[:, g, :], reads=[OUT],
                          writes=[MIXT.b((hh, tb)) for tb in range(9)])
            S.barrier()

    def out_proj_residual(self, l, jgate, MIXT, w_out, XT):
        S = self.S
        with ExitStack() as es:
            xr = [self.sb(es, f"xr{i}", [128, 512], F32) for i in range(3)]
            cnt = [0]

            def epi(ps, col, tb, t0, tn):
                x = xr[cnt[0] % 3]
                cnt[0] += 1
                kc = col // 128
                wh = 1 if tb == 0 else 0
                S.dma("pool", x[:, :tn], XT[col:col + 128, t0:t0 + tn], reads=[XT.b((kc, tb))], writes=[x])
                S.op("dve", lambda e_: e_.scalar_tensor_tensor(
                    out=x[:, :tn], in0=ps[:, :tn], scalar=self.mod(l, jgate, kc, wh), in1=x[:, :tn],
                    op0=ALU.mult, op1=ALU.add), reads=[ps, self.MOD], writes=[x])
                S.dma("pool", XT[col:col + 128, t0:t0 + tn], x[:, :tn], reads=[x], writes=[XT.b((kc, tb))])

            for tb in range(9):
                agg = MIXT.b(tb)
                for c in range(16):
                    sb_ = MIXT.b((c, tb))
                    for k, v in sb_.w.items():
                        if agg.w.get(k, 0) < v:
                            agg.w[k] = v
            self.linear(es, MIXT, w_out, w_out[:], D, 0, D, "fm", epi)
            S.barrier()

    def build(self, layers=range(DEPTH), do_mixer=True, do_moe=True, do_final=True, stop=99):
        S = self.S
        self.setup()
        xT_in = self.dram_in("xT", [D, T])
        XT = self.dram("XT", [D, T], F32)
        for kc in range(16):
            S.dma("sp", XT[kc * 128:(kc + 1) * 128, :], xT_in[kc * 128:(kc + 1) * 128, :], reads=[xT_in], writes=[XT])
        S.barrier()
        OUT = self.dram_out("out", [SEQ, D]) if do_final else None
        ada_w = {l: self.dram_in(f"ada_w{l}", [D, 6 * D]) for l in layers}
        self.cos_d = self.dram_in("cosT", [128, SEQ])
        self.sin_d = self.dram_in("sinT", [128, SEQ])
        HT = self.dram("HT", [D, T], BF16)
        MIXT = self.dram("MIXT", [D, T], BF16)
        self.XT, self.HT, self.MIXT = XT, HT, MIXT
        evs = sorted({l // 2 for l in layers if l % 2 == 0})
        ods = sorted({l // 2 for l in layers if l % 2 == 1})
        if do_mixer and evs:
            ev_w_in = {e: self.dram_in(f"ev_w_in{e}", [D, 3584]) for e in evs}
            ev_w_out = {e: self.dram_in(f"ev_w_out{e}", [D, D]) for e in evs}
            ev_ra = self.dram_in("ev_ra_w", [2, 2, 8, 128, 128])
            ev_ix = self.dram_in("ev_ix_w", [2, 2, 8, 128, 128])
            XA = self.dram("XA", [1024, T], F32)
            YG = self.dram("YG", [1024, T], BF16)
            QK = self.dram("QK", [1280, T], BF16)
            VV = self.dram("VV", [T, 256], BF16)
        if do_mixer and ods:
            self.odd_decl(ods)
        if do_moe:
            self.moe_decl(layers)
        if stop >= 1:
            self.phase_mod(ada_w, layers)
        for l in layers:
            if stop < 2:
                break
            if do_mixer:
                with ExitStack() as es:
                    def consume(tb, t0, tn, hb, hf):
                        S.dma("sp", HT.h.rearrange("(kc p) t -> p kc t", p=128)[:, :, t0:t0 + tn], hb[:, :, :tn],
                              reads=[hb], writes=[HT.b(tb)])
                    self.phase_norm(XT, l, 0, f"nmix{l}", consume, es)
                    S.barrier()
                if l % 2 == 0:
                    e = l // 2
                    if stop >= 3:
                        self.even_proj(l, e, ev_w_in[e], HT, XA, YG, QK, VV, self.cos_d, self.sin_d)
                    if stop >= 4:
                        self.even_lru(e, ev_ra, ev_ix, XA, YG, MIXT)
                    if stop >= 5:
                        self.even_attn(e, QK, VV, MIXT)
                    if stop >= 6:
                        self.out_proj_residual(l, 2, MIXT, ev_w_out[e], XT)
                else:
                    self.odd_mixer(l, l // 2)
            if do_moe:
                self.moe_layer(l)
        if do_final:
            self.final_norm(XT, OUT)
        S.barrier()
        self.es.close()
        return self.nc

    def moe_decl(self, layers):
        self.moe_r = {l: self.dram_in(f"moe_r{l}", [D, NE]) for l in layers}
        self.moe_w = {}
        for l in layers:
            for hh in range(2):
                self.moe_w[(l, "g", hh)] = self.dram_in(f"wg{l}_{hh}", [8, D, DEXP])
                self.moe_w[(l, "u", hh)] = self.dram_in(f"wu{l}_{hh}", [8, D, DEXP])
                self.moe_w[(l, "d", hh)] = self.dram_in(f"wd{l}_{hh}", [8, DEXP, D])
        self.Hrow = self.dram("Hrow", [T, D], BF16)
        self.Yacc = self.dram("Yacc", [T, D], F32)

    def moe_layer(self, l):
        S = self.S
        XT, Hrow, Yacc = self.XT, self.Hrow, self.Yacc
        with ExitStack() as eso:
            IDXT = self.sb(eso, "IDXT", [128, 5, 16], U32)
            GT = self.sb(eso, "GT", [128, 5, 16], F32)
            eso2 = ExitStack()
            AFFT = self.sb(eso2, "AFFT", [16, T], F32)
            zt = self.sb(eso2, "zt", [128, 2048], F32)
            S.op("pool", lambda e_: e_.memset(zt[:], 0.0), writes=[zt])
            for i in range(34):
                S.dma("sp", Yacc[i * 128:(i + 1) * 128, :], zt[:], reads=[zt], writes=[Yacc.b(i)])
            with ExitStack() as es:
                WR = self.sb(es, "WR", [128, 16, 16], F32)
                S.dma("sp", WR[:], self.moe_r[l].h.rearrange("(kc p) e -> p kc e", p=128), reads=[self.moe_r[l]], writes=[WR])
                hrow = [self.sb(es, f"hrow{i}", [128, 2048], BF16) for i in range(2)]
                sm = [self.sb(es, f"smx{i}", [128, 4], F32) for i in range(2)]
                ex = [self.sb(es, f"ex{i}", [128, 16], F32) for i in range(2)]
                cnt = [0]

                def consume(tb, t0, tn, hb, hf):
                    for s_ in range(tn // 128):
                        i = cnt[0] % 2
                        cnt[0] += 1
                        tok0 = t0 + s_ * 128
                        ps = self.psum()
                        for kc in range(16):
                            S.op("pe", lambda e_, kc=kc, ps=ps: e_.matmul(
                                ps[:, 0:16], lhsT=hf[:, kc, s_ * 128:(s_ + 1) * 128], rhs=WR[:, kc, :],
                                start=(kc == 0), stop=(kc == 15)), reads=[hf, WR], writes=[ps])
                        m, x_ = sm[i], ex[i]
                        S.op("dve", lambda e_, ps=ps: e_.tensor_reduce(out=m[:, 0:1], in_=ps[:, 0:16], axis=AX.X, op=ALU.max,
                                                                     negate=True), reads=[ps], writes=[m])
                        S.op("act", lambda e_, ps=ps: e_.activation(out=x_[:], in_=ps[:, 0:16], func=AF.Exp, bias=m[:, 0:1],
                                                                    scale=1.0, accum_out=m[:, 1:2]), reads=[ps, m], writes=[x_, m])
                        S.op("dve", lambda e_: e_.reciprocal(out=m[:, 2:3], in_=m[:, 1:2]), reads=[m], writes=[m])
                        S.op("dve", lambda e_: e_.tensor_scalar(out=x_[:], in0=x_[:], scalar1=m[:, 2:3], scalar2=None,
                                                                op0=ALU.mult), reads=[m], writes=[x_])
                        ps2 = self.psum()
                        S.op("pe", lambda e_, ps2=ps2: e_.transpose(out=ps2[0:16, 0:128], in_=x_[:], identity=self.IDF[:]),
                             reads=[x_, self.IDF], writes=[ps2])
                        S.op("act", lambda e_, ps2=ps2: e_.copy(out=AFFT[:, tok0:tok0 + 128], in_=ps2[0:16, 0:128]),
                             reads=[ps2], writes=[AFFT])
                        hr = hrow[i]
                        for kg in range(4):
                            ps3 = self.psum()
                            for kk in range(4):
                                kc = kg * 4 + kk
                                S.op("pe", lambda e_, kc=kc, kk=kk, ps3=ps3: e_.transpose(
                                    out=ps3[:, kk * 128:(kk + 1) * 128], in_=hf[:, kc, s_ * 128:(s_ + 1) * 128],
                                    identity=self.IDF[:]), reads=[hf, self.IDF], writes=[ps3])
                            eng = "act" if kg % 2 == 0 else "dve"
                            if eng == "act":
                                S.op("act", lambda e_, ps3=ps3, kg=kg: e_.copy(out=hr[:, kg * 512:(kg + 1) * 512], in_=ps3[:, 0:512]),
                                     reads=[ps3], writes=[hr])
                            else:
                                S.op("dve", lambda e_, ps3=ps3, kg=kg: e_.tensor_copy(out=hr[:, kg * 512:(kg + 1) * 512], in_=ps3[:, 0:512]),
                                     reads=[ps3], writes=[hr])
                        S.dma("sp", Hrow[tok0:tok0 + 128, :], hr[:], reads=[hr], writes=[Hrow.b(tok0 // 128)])

                self.phase_norm(XT, l, 3, f"nffn{l}", consume, es, want_f32=True)
                S.barrier()
            with ExitStack() as es:
                work = self.sb(es, "work", [16, SEQ], F32)
                VAL = self.sb(es, "VAL", [16, 544], F32)
                IDXu = self.sb(es, "IDXu", [16, 544], U32)
                IDXf = self.sb(es, "IDXf", [16, 544], F32)
                for (c0, n, cap, s0, toff) in ((0, CTX, 32, 512, 0), (CTX, SEQ, 512, 0, CTX)):
                    S.op("act", lambda e_: e_.copy(out=work[:, :n], in_=AFFT[:, c0:c0 + n]), reads=[AFFT], writes=[work])
                    for it in range(cap // 8):
                        sl = slice(s0 + it * 8, s0 + it * 8 + 8)
                        S.op("dve", lambda e_, sl=sl: e_.max(out=VAL[:, sl], in_=work[:, :n]), reads=[work], writes=[VAL])
                        S.op("dve", lambda e_, sl=sl: e_.max_index(out=IDXu[:, sl], in_max=VAL[:, sl], in_values=work[:, :n]),
                             reads=[work, VAL], writes=[IDXu])
                        S.op("dve", lambda e_, sl=sl: e_.match_replace(out=work[:, :n], in_to_replace=VAL[:, sl],
                                                                       in_values=work[:, :n], imm_value=-1.0),
                             reads=[VAL], writes=[work])
                    S.op("dve", lambda e_: e_.tensor_copy(out=IDXf[:, s0:s0 + cap], in_=IDXu[:, s0:s0 + cap]),
                         reads=[IDXu], writes=[IDXf])
                    if toff:
                        S.op("dve", lambda e_: e_.tensor_scalar(out=IDXf[:, s0:s0 + cap], in0=IDXf[:, s0:s0 + cap],
                                                                scalar1=float(toff), scalar2=None, op0=ALU.add),
                             reads=[IDXf], writes=[IDXf])
                S.op("pool", lambda e_: e_.memset(IDXT[:], 0), writes=[IDXT])
                S.op("pool", lambda e_: e_.memset(GT[:], 0.0), writes=[GT])
                for st in range(5):
                    ns = 128 if st < 4 else 32
                    for src, dst in ((IDXf, IDXT), (VAL, GT)):
                        ps = self.psum()
                        S.op("pe", lambda e_, ps=ps, src=src: e_.transpose(
                            out=ps[0:ns, 0:16], in_=src[:, st * 128:st * 128 + ns], identity=self.IDF[0:16, 0:16]),
                            reads=[src, self.IDF], writes=[ps])
                        S.op("dve", lambda e_, ps=ps, dst=dst: e_.tensor_copy(out=dst[0:ns, st, :], in_=ps[0:ns, 0:16]),
                             reads=[ps], writes=[dst])
                S.barrier()
            eso2.close()
            with ExitStack() as es:
                FG = 512
                NG = DEXP // FG
                wgt = [self.sb(es, f"wgt{i}", [128, 16, FG], BF16) for i in range(2)]
                wut = [self.sb(es, f"wut{i}", [128, 16, FG], BF16) for i in range(2)]
                wdt = [self.sb(es, f"wdt{i}", [128, FG // 128, D], BF16) for i in range(2)]
                xs = [self.sb(es, f"xs{i}", [128, D], BF16) for i in range(5)]
                xsT = self.sb(es, "xsT", [128, 16, 544], BF16)
                hidT = self.sb(es, "hidT", [128, FG // 128, 544], BF16)
                sg = [self.sb(es, f"sg{i}", [128, 512], F32) for i in range(2)]
                yacc = self.sb(es, "yacc", [128, 5, D], F32)
                wi = 0
                xi = 0
                si = 0
                def issue_gathers(ex_):
                    for st in range(5):
                        ns = 128 if st < 4 else 32
                        x = xs[st]
                        S.dma("pool", None, None, reads=[Hrow.b(i) for i in range(34)] + [IDXT], writes=[x],
                              fn=lambda e_, x=x, ns=ns, st=st: e_.indirect_dma_start(
                                  out=x[0:ns, :], out_offset=None, in_=Hrow[:, :],
                                  in_offset=bass.IndirectOffsetOnAxis(ap=IDXT[0:ns, st, ex_:ex_ + 1], axis=0)))

                wstate = {"wi": 0}
                issued = {}

                def issue_w(ex_, g):
                    Wg = self.moe_w[(l, "g", ex_ // 8)]
                    Wu = self.moe_w[(l, "u", ex_ // 8)]
                    Wd = self.moe_w[(l, "d", ex_ // 8)]
                    el = ex_ % 8
                    k = wstate["wi"] % 2
                    wstate["wi"] += 1
                    wg_, wu_, wd_ = wgt[k], wut[k], wdt[k]
                    f0 = g * FG
                    S.dma("pool", wg_[:], Wg[el, :, f0:f0 + FG].rearrange("(kc p) f -> p kc f", p=128), reads=[Wg], writes=[wg_])
                    S.dma("pool", wu_[:], Wu[el, :, f0:f0 + FG].rearrange("(kc p) f -> p kc f", p=128), reads=[Wu], writes=[wu_])
                    S.dma("pool", wd_[:], Wd[el, f0:f0 + FG, :].rearrange("(fc p) d -> p fc d", p=128), reads=[Wd], writes=[wd_])
                    issued[(ex_, g)] = (wg_, wu_, wd_)

                issue_gathers(0)
                issue_w(0, 0)
                for ex_ in range(NE):
                    for st in range(5):
                        ns = 128 if st < 4 else 32
                        x = xs[st]
                        for kg in range(2):
                            ps = self.psum()
                            psb = ps.h.bitcast(BF16)
                            for kk in range(8):
                                kc = kg * 8 + kk
                                S.op("pe", lambda e_, kc=kc, kk=kk, psb=psb, x=x, ns=ns: e_.transpose(
                                    out=psb[:, kk * 128:kk * 128 + ns], in_=x[0:ns, kc * 128:(kc + 1) * 128],
                                    identity=self.IDB[0:ns, 0:ns]), reads=[x, self.CB], writes=[ps])
                            S.op("act" if kg == 0 else "dve",
                                 (lambda e_, psb=psb, kg=kg, st=st, ns=ns: e_.copy(
                                     out=xsT[:, kg * 8:(kg + 1) * 8, st * 128:st * 128 + ns],
                                     in_=psb[:, 0:1024].rearrange("p (k s) -> p k s", k=8)[:, :, 0:ns])) if kg == 0 else
                                 (lambda e_, psb=psb, kg=kg, st=st, ns=ns: e_.tensor_copy(
                                     out=xsT[:, kg * 8:(kg + 1) * 8, st * 128:st * 128 + ns],
                                     in_=psb[:, 0:1024].rearrange("p (k s) -> p k s", k=8)[:, :, 0:ns])),
                                 reads=[ps], writes=[xsT])
                    for g in range(NG):
                        if g + 1 < NG:
                            issue_w(ex_, g + 1)
                        elif ex_ + 1 < NE:
                            issue_gathers(ex_ + 1)
                            issue_w(ex_ + 1, 0)
                        wg_, wu_, wd_ = issued.pop((ex_, g))
                        for fc in range(FG // 128):
                            for (c0, ns) in ((0, 512), (512, 32)):
                                psg = self.psum()
                                psu = self.psum()
                                for kc in range(16):
                                    S.op("pe", lambda e_, kc=kc, psg=psg, c0=c0, ns=ns, fc=fc, wg_=wg_: e_.matmul(
                                        psg[:, 0:ns], lhsT=wg_[:, kc, fc * 128:(fc + 1) * 128], rhs=xsT[:, kc, c0:c0 + ns],
                                        start=(kc == 0), stop=(kc == 15)), reads=[wg_, xsT], writes=[psg])
                                for kc in range(16):
                                    S.op("pe", lambda e_, kc=kc, psu=psu, c0=c0, ns=ns, fc=fc, wu_=wu_: e_.matmul(
                                        psu[:, 0:ns], lhsT=wu_[:, kc, fc * 128:(fc + 1) * 128], rhs=xsT[:, kc, c0:c0 + ns],
                                        start=(kc == 0), stop=(kc == 15)), reads=[wu_, xsT], writes=[psu])
                                s_ = sg[si % 2]
                                si += 1
                                S.op("act", lambda e_, psg=psg, s_=s_, ns=ns: e_.activation(out=s_[:, 0:ns], in_=psg[:, 0:ns], func=AF.Silu),
                                     reads=[psg], writes=[s_])
                                S.op("dve", lambda e_, psu=psu, s_=s_, ns=ns, c0=c0, fc=fc: e_.tensor_tensor(
                                    out=hidT[:, fc, c0:c0 + ns], in0=psu[:, 0:ns], in1=s_[:, 0:ns], op=ALU.mult),
                                    reads=[psu, s_], writes=[hidT])
                        for st in range(5):
                            ns = 128 if st < 4 else 32
                            for dc in range(4):
                                psd = self.psum()
                                nf = FG // 128
                                for fc in range(nf):
                                    S.op("pe", lambda e_, fc=fc, psd=psd, st=st, ns=ns, dc=dc, wd_=wd_: e_.matmul(
                                        psd[0:ns, 0:512], lhsT=hidT[:, fc, st * 128:st * 128 + ns],
                                        rhs=wd_[:, fc, dc * 512:(dc + 1) * 512], start=(fc == 0), stop=(fc == nf - 1)),
                                        reads=[hidT, wd_], writes=[psd])
                                gsc = GT[0:ns, st, ex_:ex_ + 1]
                                if g == 0:
                                    S.op("act", lambda e_, psd=psd, st=st, ns=ns, dc=dc, gsc=gsc: e_.activation(
                                        out=yacc[0:ns, st, dc * 512:(dc + 1) * 512], in_=psd[0:ns, 0:512], func=AF.Copy, scale=gsc),
                                        reads=[psd, GT], writes=[yacc])
                                else:
                                    S.op("dve", lambda e_, psd=psd, st=st, ns=ns, dc=dc, gsc=gsc: e_.scalar_tensor_tensor(
                                        out=yacc[0:ns, st, dc * 512:(dc + 1) * 512], in0=psd[0:ns, 0:512], scalar=gsc,
                                        in1=yacc[0:ns, st, dc * 512:(dc + 1) * 512], op0=ALU.mult, op1=ALU.add),
                                        reads=[psd, GT], writes=[yacc])
                    for st in range(5):
                        ns = 128 if st < 4 else 32
                        S.dma("pool", None, None, reads=[yacc, IDXT], writes=[Yacc],
                              fn=lambda e_, ns=ns, st=st: e_.indirect_dma_start(
                                  out=Yacc[:, :], out_offset=bass.IndirectOffsetOnAxis(ap=IDXT[0:ns, st, ex_:ex_ + 1], axis=0),
                                  in_=yacc[0:ns, st, :], in_offset=None, compute_op=ALU.add))
                S.barrier()
            with ExitStack() as es:
                yt = [self.sb(es, f"yt{i}", [128, D], F32) for i in range(2)]
                xr = [self.sb(es, f"mxr{i}", [128, 16, 128], F32) for i in range(2)]
                XTv = XT.h.rearrange("(kc p) t -> p kc t", p=128)
                for i in range(34):
                    y = yt[i % 2]
                    x = xr[i % 2]
                    wh = 1 if i < 2 else 0
                    S.dma("sp", y[:], Yacc[i * 128:(i + 1) * 128, :], reads=[Yacc], writes=[y])
                    S.dma("sp", x[:], XTv[:, :, i * 128:(i + 1) * 128], reads=[XT.b(("m5", i))], writes=[x])
                    for kg in range(4):
                        ps = self.psum()
                        for kk in range(4):
                            kc = kg * 4 + kk
                            S.op("pe", lambda e_, kc=kc, kk=kk, ps=ps, y=y: e_.transpose(
                                out=ps[:, kk * 128:(kk + 1) * 128], in_=y[:, kc * 128:(kc + 1) * 128], identity=self.IDF[:]),
                                reads=[y, self.IDF], writes=[ps])
                        for kk in range(4):
                            kc = kg * 4 + kk
                            S.op("dve", lambda e_, kc=kc, kk=kk, ps=ps, x=x: e_.scalar_tensor_tensor(
                                out=x[:, kc, :], in0=ps[:, kk * 128:(kk + 1) * 128], scalar=self.mod(l, 5, kc, wh),
                                in1=x[:, kc, :], op0=ALU.mult, op1=ALU.add), reads=[ps, self.MOD], writes=[x])
                    S.dma("sp", XTv[:, :, i * 128:(i + 1) * 128], x[:], reads=[x], writes=[XT.b(("m5", i))])
                S.barrier()

    def final_norm(self, XT, OUT):
        S = self.S
        with ExitStack() as es:
            xts = [self.sb(es, f"fx{i}", [128, 16, 512], F32) for i in range(2)]
            sq = self.sb(es, "fsq", [128, 16, 512], BF16)
            rs = self.sb(es, "frs", [128, 512], F32)
            ot = [self.sb(es, f"fo{i}", [128, D], F32) for i in range(2)]
            XTv = XT.h.rearrange("(kc p) t -> p kc t", p=128)
            oi = 0
            for tb, (t0, tn) in list(enumerate(TBLOCKS))[1:]:
                xt = xts[tb % 2]
                S.dma("sp", xt[:], XTv[:, :, t0:t0 + tn], reads=[XT], writes=[xt])
                S.op("act", lambda e: e.activation(out=sq[:], in_=xt[:], func=AF.Square), reads=[xt], writes=[sq])
                acc = self.psum()
                for kc in range(16):
                    S.op("pe", lambda e: e.matmul(acc[:, :], lhsT=self.ONESB, rhs=sq[:, kc, :], start=(kc == 0), stop=(kc == 15)),
                         reads=[sq, self.CB], writes=[acc])
                S.op("act", lambda e: e.activation(out=rs[:], in_=acc[:, :], func=AF.Sqrt, bias=self.epsT[:, 0:1], scale=1.0 / D),
                     reads=[acc, self.epsT], writes=[rs])
                S.op("dve", lambda e: e.reciprocal(out=rs[:], in_=rs[:]), reads=[rs], writes=[rs])
                for kc in range(16):
                    S.op("dve", lambda e: e.scalar_tensor_tensor(out=xt[:, kc, :], in0=xt[:, kc, :], scalar=self.vcol("fnorm", kc, 1),
                                                                 in1=rs[:], op0=ALU.mult, op1=ALU.mult), reads=[rs, self.V], writes=[xt])
                for s_ in range(4):
                    o = ot[oi % 2]
                    oi += 1
                    for kg in range(4):
                        ps = self.psum()
                        for kk in range(4):
                            kc = kg * 4 + kk
                            S.op("pe", lambda e: e.transpose(out=ps[:, kk * 128:(kk + 1) * 128], in_=xt[:, kc, s_ * 128:(s_ + 1) * 128],
                                                             identity=self.IDF[:]), reads=[xt, self.IDF], writes=[ps])
                        if kg % 2 == 0:
                            S.op("act", lambda e: e.copy(out=o[:, kg * 512:(kg + 1) * 512], in_=ps[:, 0:512]), reads=[ps], writes=[o])
                        else:
                            S.op("dve", lambda e: e.tensor_copy(out=o[:, kg * 512:(kg + 1) * 512], in_=ps[:, 0:512]), reads=[ps], writes=[o])
                    r0 = t0 - CTX + s_ * 128
                    S.dma("sp", OUT[r0:r0 + 128, :], o[:], reads=[o], writes=[OUT.b(r0)])
            S.barrier()

    def odd_decl(self, ods):
        self.od_w_in = {o: self.dram_in(f"od_w_in{o}", [D, 6176]) for o in ods}
        self.od_w_out = {o: self.dram_in(f"od_w_out{o}", [D, D]) for o in ods}
        self.hnorm_d = {o: self.dram_in(f"hnorm_bc{o}", [128, D]) for o in ods}
        self.cf_d = self.dram_in("cf", [128, 5, 128])
        self.QKP = self.dram("QKP", [D, T], F32)
        self.QS = self.dram("QS", [D, T], BF16)
        self.VM = self.dram("VM", [T, D], BF16)
        self.OS = self.dram("OS", [T, D], BF16)
        self.GG = self.dram("GG", [T, 32], F32)
        self.HS = self.dram("HS", [T, D], F32)

    def odd_mixer(self, l, o, stop=99):
        S = self.S
        HT, MIXT, XT = self.HT, self.MIXT, self.XT
        w_in = self.od_w_in[o]
        QKP, QS, VM, OS, GG, HS = self.QKP, self.QS, self.VM, self.OS, self.GG, self.HS
        with ExitStack() as es:
            st_f = [self.sb(es, f"ostf{i}", [128, 512], F32) for i in range(2)]
            st_b = [self.sb(es, f"ostb{i}", [128, 512], BF16) for i in range(2)]
            cnt = [0]

            def epi_qk(ps, col, tb, t0, tn):
                sf = st_f[cnt[0] % 2]
                cnt[0] += 1
                S.op("act", lambda e: e.copy(out=sf[:, :tn], in_=ps[:, :tn]), reads=[ps], writes=[sf])
                S.dma("pool", QKP[col:col + 128, t0:t0 + tn], sf[:, :tn], reads=[sf], writes=[QKP.b((col, tb))])

            self.linear(es, HT, w_in, w_in[:], D, 0, 2048, "fm", epi_qk)

            def epi_tm(ps, cs, ncol, tb, tok0):
                i = cnt[0] % 2
                cnt[0] += 1
                if cs < 4096:
                    sb_ = st_b[i]
                    S.op("act", lambda e: e.copy(out=sb_[:, :ncol], in_=ps[:, :ncol]), reads=[ps], writes=[sb_])
                    S.dma("pool", VM[tok0:tok0 + 128, cs - 2048:cs - 2048 + ncol], sb_[:, :ncol], reads=[sb_], writes=[VM.b((cs, tok0))])
                elif cs < 6144:
                    sb_ = st_b[i]
                    S.op("act", lambda e: e.activation(out=sb_[:, :ncol], in_=ps[:, :ncol], func=AF.Sigmoid), reads=[ps], writes=[sb_])
                    S.dma("pool", OS[tok0:tok0 + 128, cs - 4096:cs - 4096 + ncol], sb_[:, :ncol], reads=[sb_], writes=[OS.b((cs, tok0))])
                else:
                    sf = st_f[i]
                    S.op("act", lambda e: e.copy(out=sf[:, :ncol], in_=ps[:, :ncol]), reads=[ps], writes=[sf])
                    S.dma("pool", GG[tok0:tok0 + 128, :], sf[:, :ncol], reads=[sf], writes=[GG.b(tok0)])

            self.linear(es, HT, w_in, w_in[:], D, 2048, 6176, "tm", epi_tm)
            S.barrier()
        if stop < 2:
            return
        with ExitStack() as es:
            xp = [self.sb(es, f"oxp{i}", [128, T], F32) for i in range(2)]
            xa = [self.sb(es, f"oxa{i}", [128, T], F32) for i in range(2)]
            xo = [self.sb(es, f"oxo{i}", [128, T], BF16) for i in range(2)]
            segs = [(0, CTX), (CTX, T)]
            for c in range(16):
                p_, a_, o_ = xp[c % 2], xa[c % 2], xo[c % 2]
                S.dma("sp", p_[:], QKP[c * 128:(c + 1) * 128, :], reads=[QKP], writes=[p_])
                cw = lambda j: self.vcol(f"od_cw{o}", j * 16 + c, 1)
                for (a0, a1) in segs:
                    S.op("dve", lambda e: e.tensor_scalar(out=a_[:, a0:a1], in0=p_[:, a0:a1], scalar1=cw(2),
                                                          scalar2=self.vcol(f"od_cb{o}", c, 1), op0=ALU.mult, op1=ALU.add),
                         reads=[p_, self.V], writes=[a_])
                    for j, off in ((0, -2), (1, -1), (3, 1)):
                        lo = max(a0, a0 - off)
                        hi = min(a1, a1 - off)
                        S.op("dve", lambda e: e.scalar_tensor_tensor(out=a_[:, lo:hi], in0=p_[:, lo + off:hi + off], scalar=cw(j),
                                                                     in1=a_[:, lo:hi], op0=ALU.mult, op1=ALU.add),
                             reads=[p_, self.V], writes=[a_])
                if c < 8:
                    S.op("act", lambda e: e.activation(out=a_[:], in_=a_[:], func=AF.Silu), reads=[a_], writes=[a_])
                    S.op("act", lambda e: e.activation(out=o_[:], in_=a_[:], func=AF.Copy, scale=128.0 ** -0.5),
                         reads=[a_], writes=[o_])
                else:
                    S.op("act", lambda e: e.activation(out=o_[:], in_=a_[:], func=AF.Silu), reads=[a_], writes=[o_])
                S.dma("sp", QS[c * 128:(c + 1) * 128, :], o_[:], reads=[o_], writes=[QS.b(c)])
            S.barrier()
        if stop < 3:
            return
        with ExitStack() as es:
            CF = self.sb(es, "CF", [128, 5, 128], F32)
            S.dma("sp", CF[:], self.cf_d[:], reads=[self.cf_d], writes=[CF])
            NCH = 34
            GGt = self.sb(es, "GGt", [128, NCH, 32], F32)
            S.dma("sp", GGt[:], GG.h.rearrange("(n p) g -> p n g", p=128), reads=[GG], writes=[GGt])
            gbb = self.vcol(f"od_gb{o}", 0, 32)
            for n in range(NCH):
                S.op("pool", lambda e: e.tensor_tensor(out=GGt[:, n, :], in0=GGt[:, n, :], in1=gbb, op=ALU.add),
                     reads=[self.V], writes=[GGt])
            GG4 = GGt[:].rearrange("p n (ty h) -> p n ty h", ty=4)
            LF = self.sb(es, "LF", [128, NCH, 2, 8], F32)
            for d in range(2):
                S.op("act", lambda e: e.activation(out=LF[:, :, d, :], in_=GG4[:, :, 2 * d + 1, :], func=AF.Exp, scale=-1.0),
                     reads=[GGt], writes=[LF])
            S.op("act", lambda e: e.activation(out=LF[:], in_=LF[:], func=AF.Ln, bias=1.0, scale=1.0), reads=[LF], writes=[LF])
            S.op("dve", lambda e: e.tensor_scalar(out=LF[:], in0=LF[:], scalar1=-1.0, scalar2=None, op0=ALU.mult),
                 reads=[LF], writes=[LF])
            Bt = self.sb(es, "Bt", [128, NCH, 2, 8], F32)
            Ct = self.sb(es, "Ct", [128, NCH, 2, 8], F32)
            EBt = self.sb(es, "EBt", [128, NCH, 2, 8], F32)
            for n in range(NCH):
                ps = self.psum()
                for d in range(2):
                    S.op("pe", lambda e: e.matmul(ps[:, d * 8:(d + 1) * 8], lhsT=CF[:, d, :], rhs=LF[:, n, d, :], start=True, stop=True),
                         reads=[CF, LF], writes=[ps])
                S.op("dve", lambda e: e.tensor_copy(out=Bt[:, n].rearrange("p d h -> p (d h)"), in_=ps[:, 0:16]),
                     reads=[ps], writes=[Bt])
            for d in range(2):
                S.op("dve", lambda e: e.tensor_tensor(out=Ct[:, :, d, :], in0=GG4[:, :, 2 * d, :], in1=Bt[:, :, d, :], op=ALU.subtract),
                     reads=[GGt, Bt], writes=[Ct])
            S.op("act", lambda e: e.activation(out=EBt[:], in_=Bt[:], func=AF.Exp), reads=[Bt], writes=[EBt])
            qT = self.sb(es, "qT", [128, T], BF16)
            kT = self.sb(es, "kT", [128, T], BF16)
            Va = self.sb(es, "Va", [128, NCH, 257], BF16)
            Hs = self.sb(es, "Hs", [128, NCH, 256], F32)
            ktm = self.sb(es, "ktm", [128, NCH, 128], BF16)
            Cf = [self.sb(es, f"Cf{d}", [128, 257], F32) for d in range(2)]
            Cb = [self.sb(es, f"Cb{d}", [128, 257], BF16) for d in range(2)]
            LFB = [self.sb(es, f"LFB{i}", [128, 128], F32) for i in range(3)]
            BBm = [self.sb(es, f"BBm{i}", [128, 128], F32) for i in range(3)]
            Dm = [self.sb(es, f"Dm{i}", [128, 128], F32) for i in range(3)]
            SD = [self.sb(es, f"SD{i}", [128, 128], BF16) for i in range(3)]
            ku = [self.sb(es, f"ku{i}", [128, 128], BF16) for i in range(3)]
            tin = [self.sb(es, f"tin{i}", [128, 257], F32) for i in range(2)]
            tot = [self.sb(es, f"tot{i}", [128, 257], F32) for i in range(2)]
            sc = [self.sb(es, f"sc{i}", [128, 8], F32) for i in range(3)]
            order_f = list(range(NCH))
            order_b = [1, 0] + list(range(NCH - 1, 1, -1))
            ui = 0
            for hd in range(8):
                S.dma("sp", qT[:], QS[hd * 128:(hd + 1) * 128, :], reads=[QS], writes=[qT])
                S.dma("sp", kT[:], QS[1024 + hd * 128:1024 + (hd + 1) * 128, :], reads=[QS], writes=[kT])
                S.dma("sp", Va[:, :, 0:256], VM[:, hd * 256:(hd + 1) * 256].rearrange("(n p) d -> p n d", p=128), reads=[VM], writes=[Va])
                S.op("pool", lambda e: e.memset(Va[:, :, 256:257], 1.0), writes=[Va])
                S.op("pool", lambda e: e.memset(Hs[:], 0.0), writes=[Hs])
                for n0 in range(0, NCH, 8):
                    nn = min(8, NCH - n0)
                    ps = self.psum()
                    psb = ps.h.bitcast(BF16)
                    for j in range(nn):
                        S.op("pe", lambda e: e.transpose(out=psb[:, j * 128:(j + 1) * 128], in_=kT[:, (n0 + j) * 128:(n0 + j + 1) * 128],
                                                         identity=self.IDB), reads=[kT, self.CB], writes=[ps])
                    S.op("act", lambda e: e.copy(out=ktm[:, n0:n0 + nn, :].rearrange("p n d -> p (n d)"), in_=psb[:, 0:nn * 128]),
                         reads=[ps], writes=[ktm])
                for d in range(2):
                    S.op("pool", lambda e: e.memset(Cf[d][:], 0.0), writes=[Cf[d]])
                    S.op("pool", lambda e: e.memset(Cb[d][:], 0.0), writes=[Cb[d]])
                units = []
                for step in range(NCH):
                    for d in range(2):
                        units.append((order_f[step] if d == 0 else order_b[step], d))
                NB3 = 3
                live = {}

                def stageA(ux):
                    n, d = units[ux]
                    i = ux % NB3
                    tsl = slice(n * 128, (n + 1) * 128)
                    last = 127 if d == 0 else 0
                    lfb, bbm, dm, sd, ku_, sc_ = LFB[i], BBm[i], Dm[i], SD[i], ku[i], sc[i]
                    S.op("act", lambda e: e.activation(out=lfb[:], in_=CF[:, 4, :], func=AF.Copy, scale=LF[:, n, d, hd:hd + 1]),
                         reads=[CF, LF], writes=[lfb])
                    psB = self.psum()
                    S.op("pe", lambda e: e.matmul(psB[:, 0:128], lhsT=lfb[:], rhs=CF[:, d, :], start=True, stop=True),
                         reads=[lfb, CF], writes=[psB])
                    S.op("dve", lambda e: e.tensor_tensor(out=bbm[:], in0=psB[:, 0:128], in1=CF[:, 2 + d, :], op=ALU.add),
                         reads=[psB, CF], writes=[bbm])
                    S.op("act", lambda e: e.copy(out=sc_[:, 0:1], in_=psB[:, last:last + 1]), reads=[psB], writes=[sc_])
                    S.op("act", lambda e: e.activation(out=dm[:], in_=bbm[:], func=AF.Exp, bias=Ct[:, n, d, hd:hd + 1], scale=1.0),
                         reads=[bbm, Ct], writes=[dm])
                    S.op("act", lambda e: e.activation(out=sc_[:, 1:2], in_=Ct[:, n, d, hd:hd + 1], func=AF.Exp, bias=sc_[:, 0:1], scale=1.0),
                         reads=[Ct, sc_], writes=[sc_])
                    S.op("act", lambda e: e.activation(out=sc_[:, 2:3], in_=sc_[:, 0:1], func=AF.Exp), reads=[sc_], writes=[sc_])
                    psS = self.psum()
                    S.op("pe", lambda e: e.matmul(psS[:, 0:128], lhsT=kT[:, tsl], rhs=qT[:, tsl], start=True, stop=True),
                         reads=[kT, qT], writes=[psS])
                    S.op("dve", lambda e: e.tensor_tensor(out=sd[:], in0=psS[:, 0:128], in1=dm[:], op=ALU.mult),
                         reads=[psS, dm], writes=[sd])
                    psI = self.psum()
                    S.op("pe", lambda e: e.matmul(psI[:, 0:257], lhsT=sd[:], rhs=Va[:, n, :], start=True, stop=True),
                         reads=[sd, Va], writes=[psI])
                    S.op("act", lambda e: e.activation(out=ku_[:], in_=ktm[:, n, :], func=AF.Copy, scale=sc_[:, 1:2]),
                         reads=[ktm, sc_], writes=[ku_])
                    psU = self.psum()
                    S.op("pe", lambda e: e.matmul(psU[:, 0:257], lhsT=ku_[:], rhs=Va[:, n, :], start=True, stop=True),
                         reads=[ku_, Va], writes=[psU])
                    live[ux] = (psI, psU)

                def stageB(ux):
                    n, d = units[ux]
                    i = ux % NB3
                    tsl = slice(n * 128, (n + 1) * 128)
                    tin_, tot_, sc_ = tin[i % 2], tot[i % 2], sc[i]
                    psI, psU = live.pop(ux)
                    psC = self.psum()
                    S.op("pe", lambda e: e.matmul(psC[:, 0:257], lhsT=qT[:, tsl], rhs=Cb[d][:], start=True, stop=True),
                         reads=[qT, Cb[d]], writes=[psC])
                    S.op("act", lambda e: e.activation(out=tin_[:], in_=psC[:, 0:257], func=AF.Copy, scale=EBt[:, n, d, hd:hd + 1]),
                         reads=[psC, EBt], writes=[tin_])
                    S.op("dve", lambda e: e.scalar_tensor_tensor(out=Cf[d][:], in0=Cf[d][:], scalar=sc_[:, 2:3], in1=psU[:, 0:257],
                                                                 op0=ALU.mult, op1=ALU.add), reads=[psU, sc_], writes=[Cf[d]])
                    S.op("act", lambda e: e.copy(out=Cb[d][:], in_=Cf[d][:]), reads=[Cf[d]], writes=[Cb[d]])
                    S.op("dve", lambda e: e.tensor_tensor(out=tot_[:], in0=psI[:, 0:257], in1=tin_[:], op=ALU.add),
                         reads=[psI, tin_], writes=[tot_])
                    S.op("act", lambda e: e.activation(out=sc_[:, 3:4], in_=tot_[:, 256:257], func=AF.Abs), reads=[tot_], writes=[sc_])
                    S.op("dve", lambda e: e.tensor_scalar(out=sc_[:, 3:4], in0=sc_[:, 3:4], scalar1=1.0, scalar2=None,
                                                          op0=ALU.max), reads=[sc_], writes=[sc_])
                    S.op("dve", lambda e: e.reciprocal(out=sc_[:, 4:5], in_=sc_[:, 3:4]), reads=[sc_], writes=[sc_])
                    S.op("dve", lambda e: e.scalar_tensor_tensor(out=Hs[:, n, :], in0=tot_[:, 0:256], scalar=sc_[:, 4:5], in1=Hs[:, n, :],
                                                                 op0=ALU.mult, op1=ALU.add), reads=[tot_, sc_], writes=[Hs])

                stageA(0)
                for ux in range(len(units)):
                    if ux + 1 < len(units):
                        stageA(ux + 1)
                    stageB(ux)
                S.dma("sp", HS[:, hd * 256:(hd + 1) * 256].rearrange("(n p) d -> p n d", p=128), Hs[:], reads=[Hs], writes=[HS.b(hd)])
            S.barrier()
        if stop < 4:
            return
        with ExitStack() as es:
            hn = self.sb(es, "hn", [128, D], F32)
            S.dma("sp", hn[:], self.hnorm_d[o][:], reads=[self.hnorm_d[o]], writes=[hn])
            hsT = [self.sb(es, f"hsT{i}", [128, D], F32) for i in range(2)]
            osT = [self.sb(es, f"osT{i}", [128, D], BF16) for i in range(2)]
            w2 = [self.sb(es, f"w2{i}", [128, D], F32) for i in range(2)]
            hb_ = [self.sb(es, f"hbo{i}", [128, D], BF16) for i in range(2)]
            junk = self.sb(es, "junk", [128, 256], F32)
            ss = [self.sb(es, f"oss{i}", [128, 8], F32) for i in range(2)]
            mo = [self.sb(es, f"mo{i}", [128, 16, 128], BF16) for i in range(2)]
            MIXv = MIXT.h.rearrange("(kc p) t -> p kc t", p=128)
            for i in range(34):
                h_, o_, w_, hb2, s_, m_ = hsT[i % 2], osT[i % 2], w2[i % 2], hb_[i % 2], ss[i % 2], mo[i % 2]
                S.dma("sp", h_[:], HS[i * 128:(i + 1) * 128, :], reads=[HS], writes=[h_])
                S.dma("sp", o_[:], OS[i * 128:(i + 1) * 128, :], reads=[OS], writes=[o_])
                S.op("pool", lambda e: e.tensor_tensor(out=w_[:], in0=o_[:], in1=hn[:], op=ALU.mult), reads=[o_, hn], writes=[w_])
                for hd in range(8):
                    S.op("act", lambda e: e.activation(out=junk[:], in_=h_[:, hd * 256:(hd + 1) * 256], func=AF.Square,
                                                       accum_out=s_[:, hd:hd + 1]), reads=[h_], writes=[junk, s_])
                S.op("act", lambda e: e.activation(out=s_[:], in_=s_[:], func=AF.Sqrt, bias=self.epsT[:, 0:1], scale=1.0 / 256),
                     reads=[s_, self.epsT], writes=[s_])
                S.op("dve", lambda e: e.reciprocal(out=s_[:], in_=s_[:]), reads=[s_], writes=[s_])
                for hd in range(8):
                    S.op("dve", lambda e: e.scalar_tensor_tensor(out=hb2[:, hd * 256:(hd + 1) * 256], in0=h_[:, hd * 256:(hd + 1) * 256],
                                                                 scalar=s_[:, hd:hd + 1], in1=w_[:, hd * 256:(hd + 1) * 256],
                                                                 op0=ALU.mult, op1=ALU.mult), reads=[h_, s_, w_], writes=[hb2])
                for kg in range(2):
                    ps = self.psum()
                    psb = ps.h.bitcast(BF16)
                    for kk in range(8):
                        kc = kg * 8 + kk
                        S.op("pe", lambda e: e.transpose(out=psb[:, kk * 128:(kk + 1) * 128], in_=hb2[:, kc * 128:(kc + 1) * 128],
                                                         identity=self.IDB), reads=[hb2, self.CB], writes=[ps])
                    if kg == 0:
                        S.op("act", lambda e: e.copy(out=m_[:, 0:8, :].rearrange("p k t -> p (k t)"), in_=psb[:, 0:1024]), reads=[ps], writes=[m_])
                    else:
                        S.op("dve", lambda e: e.tensor_copy(out=m_[:, 8:16, :].rearrange("p k t -> p (k t)"), in_=psb[:, 0:1024]), reads=[ps], writes=[m_])
                S.dma("sp", MIXv[:, :, i * 128:(i + 1) * 128], m_[:], reads=[m_], writes=[MIXT.b(("o4", i))])
            S.barrier()
        if stop < 5:
            return
        self.out_proj_residual(l, 2, MIXT, self.od_w_out[o], XT)


N_CORES = 4


def kernel(**inp):
    inp = {k: np.asarray(v) for k, v in inp.items()}
    P = Prog()
    nc = P.build()
    cs = make_consts()
    shared = {"cb": cs["cb"], "ident_f": cs["ident_f"], "cosT": cs["cosT"], "sinT": cs["sinT"], "cf": cs["cf"],
              "ev_ra_w": np.ascontiguousarray(inp["ev_ra_w"], dtype=np.float32),
              "ev_ix_w": np.ascontiguousarray(inp["ev_ix_w"], dtype=np.float32)}
    for l in range(DEPTH):
        shared[f"ada_w{l}"] = np.ascontiguousarray(inp["ada_w"][l], dtype=np.float32)
        shared[f"moe_r{l}"] = np.ascontiguousarray(inp["moe_router"][l], dtype=np.float32)
        for hh in range(2):
            shared[f"wg{l}_{hh}"] = np.ascontiguousarray(inp["moe_w_gate"][l, hh * 8:(hh + 1) * 8], dtype=np.float32)
            shared[f"wu{l}_{hh}"] = np.ascontiguousarray(inp["moe_w_up"][l, hh * 8:(hh + 1) * 8], dtype=np.float32)
            shared[f"wd{l}_{hh}"] = np.ascontiguousarray(inp["moe_w_down"][l, hh * 8:(hh + 1) * 8], dtype=np.float32)
    for e in range(2):
        shared[f"ev_w_in{e}"] = np.ascontiguousarray(inp["ev_w_in"][e], dtype=np.float32)
        shared[f"ev_w_out{e}"] = np.ascontiguousarray(inp["ev_w_out"][e], dtype=np.float32)
        shared[f"od_w_in{e}"] = np.ascontiguousarray(inp["od_w_in"][e], dtype=np.float32)
        shared[f"od_w_out{e}"] = np.ascontiguousarray(inp["od_w_out"][e], dtype=np.float32)
        shared[f"hnorm_bc{e}"] = np.ascontiguousarray(
            np.broadcast_to(np.asarray(inp["od_hnorm_w"][e], np.float32)[None, :], (128, D)))
    in_maps = []
    for b in range(N_CORES):
        m = dict(shared)
        m["xT"] = np.ascontiguousarray(np.concatenate([inp["ctx"][b], inp["x"][b]], axis=0).T.astype(np.float32))
        m["vecs"] = pack_vecs(inp, b)
        in_maps.append(m)
    res = run_bass_kernel_spmd(nc, in_maps, core_ids=list(range(N_CORES)))
    return np.stack([np.asarray(res.results[b]["out"], dtype=np.float32) for b in range(N_CORES)], axis=0)
```
